# Optimizing a Trainium2 kernel written in Bass

```python
import math
import jax, jax.numpy as jnp
from jax import lax
import numpy as np

D_MODEL = 1024
BATCH = 8
SEQ = 2048
DEPTH = 2

MIX_WIDTH = D_MODEL
HEAD_DIM = 64
ATTN_WIDTH = D_MODEL // 2
N_Q_HEADS = ATTN_WIDTH // HEAD_DIM
N_KV_HEADS = N_Q_HEADS // 4
KV_WIDTH = N_KV_HEADS * HEAD_DIM
WINDOW = 128
BLOCK = 128
ROPE_THETA = 500000.0
ROPE_DIM = HEAD_DIM // 4
CONV_WIDTH = D_MODEL // 4
CONV_K = 3
POOL_WIDTH = D_MODEL // 4
POOL_WINDOWS = (2, 4, 8, 16)
POOL_GROUP = POOL_WIDTH // len(POOL_WINDOWS)
IN_WIDTH = ATTN_WIDTH + 2 * KV_WIDTH + 3 * CONV_WIDTH + POOL_WIDTH
IN_SPLITS = (ATTN_WIDTH,
             ATTN_WIDTH + KV_WIDTH,
             ATTN_WIDTH + 2 * KV_WIDTH,
             ATTN_WIDTH + 2 * KV_WIDTH + CONV_WIDTH,
             ATTN_WIDTH + 2 * KV_WIDTH + 2 * CONV_WIDTH,
             ATTN_WIDTH + 2 * KV_WIDTH + 3 * CONV_WIDTH)
PEER_HEADS = 8
PEER_NKEYS = 128
PEER_EXPERTS = PEER_NKEYS * PEER_NKEYS
PEER_QDIM = 256
PEER_TOPK = 16
PEER_CHUNK = 128
EPS = 1e-6
MASK_VALUE = -1e30

kernel_name = "hybrid_conv_swa_pool_peer_encoder"


def rms_norm(t, g):
    tf = t.astype(jnp.float32)
    y = tf * lax.rsqrt(jnp.mean(tf * tf, axis=-1, keepdims=True) + EPS)
    return (y * g.astype(jnp.float32)).astype(t.dtype)


def rope_tables(positions):
    inv = ROPE_THETA ** (-jnp.arange(0, ROPE_DIM, 2, dtype=jnp.float32) / ROPE_DIM)
    ang = positions.astype(jnp.float32)[..., None] * inv
    return jnp.cos(ang)[:, :, None, :], jnp.sin(ang)[:, :, None, :]


def partial_rope(t, cos, sin):
    half = ROPE_DIM // 2
    t1 = t[..., :half].astype(jnp.float32)
    t2 = t[..., half:ROPE_DIM].astype(jnp.float32)
    rot = jnp.concatenate([t1 * cos - t2 * sin, t2 * cos + t1 * sin], axis=-1).astype(t.dtype)
    return jnp.concatenate([rot, t[..., ROPE_DIM:]], axis=-1)


def windowed_gqa(q, k, v, sink):
    b, s = q.shape[0], q.shape[1]
    nb = s // BLOCK
    grp = N_Q_HEADS // N_KV_HEADS
    qb = q.reshape(b, nb, BLOCK, N_KV_HEADS, grp, HEAD_DIM)

    def bands(t):
        tp = jnp.pad(t, ((0, 0), (BLOCK, BLOCK), (0, 0), (0, 0)))
        tp = tp.reshape(b, nb + 2, BLOCK, N_KV_HEADS, HEAD_DIM)
        return jnp.concatenate([tp[:, :-2], tp[:, 1:-1], tp[:, 2:]], axis=2)

    kb, vb = bands(k), bands(v)
    scores = jnp.einsum('bnqhgd,bnkhd->bnhgqk', qb, kb).astype(jnp.float32) * (HEAD_DIM ** -0.5)
    qpos = jnp.arange(s).reshape(nb, BLOCK, 1)
    kpos = (jnp.arange(nb)[:, None, None] - 1) * BLOCK + jnp.arange(3 * BLOCK)[None, None, :]
    valid = (jnp.abs(qpos - kpos) <= WINDOW) & (kpos >= 0) & (kpos < s)
    scores = jnp.where(valid[None, :, None, None], scores, MASK_VALUE)
    sink_col = jnp.broadcast_to(
        sink.astype(jnp.float32).reshape(1, 1, N_KV_HEADS, grp, 1, 1), scores.shape[:-1] + (1,))
    probs = jax.nn.softmax(jnp.concatenate([scores, sink_col], axis=-1), axis=-1)[..., :-1]
    out = jnp.einsum('bnhgqk,bnkhd->bnqhgd', probs.astype(v.dtype), vb)
    return out.reshape(b, s, N_Q_HEADS * HEAD_DIM)


def short_conv(u, w):
    up = jnp.pad(u, ((0, 0), (1, 1), (0, 0)))
    return w[0] * up[:, :-2] + w[1] * up[:, 1:-1] + w[2] * up[:, 2:]


def multiscale_pool(u):
    s = u.shape[1]
    t = jnp.arange(s)
    uf = u.astype(jnp.float32)
    cs = jnp.pad(jnp.cumsum(uf, axis=1), ((0, 0), (1, 0), (0, 0)))
    outs = []
    for gi, w in enumerate(POOL_WINDOWS):
        lo = jnp.clip(t - w // 2, 0, s)
        hi = jnp.clip(t + w // 2, 0, s)
        csg = cs[..., gi * POOL_GROUP:(gi + 1) * POOL_GROUP]
        cnt = (hi - lo).astype(jnp.float32)[None, :, None]
        mean = (jnp.take(csg, hi, axis=1) - jnp.take(csg, lo, axis=1)) / cnt
        outs.append(mean - uf[..., gi * POOL_GROUP:(gi + 1) * POOL_GROUP])
    return jnp.stack(outs, axis=2).astype(u.dtype)


def peer_ffn(h, w_q, sub_keys, expert_u, expert_v):
    b, s, d = h.shape
    n_tok = b * s
    ht = h.reshape(n_tok, d)
    q = (ht @ w_q).reshape(n_tok, PEER_HEADS, PEER_QDIM)
    half = PEER_QDIM // 2
    s1 = jnp.einsum('thd,kd->thk', q[..., :half], sub_keys[0]).astype(jnp.float32)
    s2 = jnp.einsum('thd,kd->thk', q[..., half:], sub_keys[1]).astype(jnp.float32)
    v1, i1 = lax.top_k(s1, PEER_TOPK)
    v2, i2 = lax.top_k(s2, PEER_TOPK)
    cand = (v1[..., :, None] + v2[..., None, :]).reshape(n_tok, PEER_HEADS, PEER_TOPK * PEER_TOPK)
    cidx = (i1[..., :, None] * PEER_NKEYS + i2[..., None, :]).reshape(n_tok, PEER_HEADS, PEER_TOPK * PEER_TOPK)
    top, pos = lax.top_k(cand, PEER_TOPK)
    eidx = jnp.take_along_axis(cidx, pos, axis=-1)
    gate = jax.nn.softmax(top, axis=-1).astype(h.dtype)
    n_chunks = n_tok // PEER_CHUNK

    def chunk(args):
        hc, ec, gc = args
        u = expert_u[ec]
        act = jax.nn.gelu(jnp.einsum('td,thkd->thk', hc, u), approximate=False)
        return jnp.einsum('thk,thkd->td', gc * act, expert_v[ec])

    y = lax.map(chunk, (ht.reshape(n_chunks, PEER_CHUNK, d),
                        eidx.reshape(n_chunks, PEER_CHUNK, PEER_HEADS, PEER_TOPK),
                        gate.reshape(n_chunks, PEER_CHUNK, PEER_HEADS, PEER_TOPK)))
    return y.reshape(b, s, d)


def setup_inputs(seed: int = 0) -> dict:
    key = jax.random.key(seed)
    ks = jax.random.split(key, 20)
    L, D = DEPTH, D_MODEL

    def nrm(k, shape, scale):
        return jax.random.normal(k, shape, jnp.float32) * scale

    return {
        "x": nrm(ks[0], (BATCH, SEQ, D), 1.0),
        "c": nrm(ks[1], (BATCH, D), 1.0),
        "positions": jnp.arange(SEQ, dtype=jnp.int32)[None, :]
                     + jax.random.randint(ks[2], (BATCH, 1), 0, 1024, dtype=jnp.int32),
        "norm1_g": 1.0 + nrm(ks[3], (L, D), 0.02),
        "norm2_g": 1.0 + nrm(ks[4], (L, D), 0.02),
        "w_ada": nrm(ks[5], (L, D, 6 * D), 0.5 * D ** -0.5),
        "b_ada": nrm(ks[6], (L, 6 * D), 0.02),
        "w_in": nrm(ks[7], (L, D, IN_WIDTH), D ** -0.5),
        "q_norm_g": 1.0 + nrm(ks[8], (L, HEAD_DIM), 0.02),
        "k_norm_g": 1.0 + nrm(ks[9], (L, HEAD_DIM), 0.02),
        "attn_sink": nrm(ks[10], (L, N_Q_HEADS), 0.5),
        "conv_w": nrm(ks[11], (L, CONV_K, CONV_WIDTH), CONV_K ** -0.5),
        "pool_w": nrm(ks[12], (L, len(POOL_WINDOWS), POOL_GROUP, POOL_GROUP), POOL_GROUP ** -0.5),
        "pool_scale": 1.0 + nrm(ks[13], (L, POOL_WIDTH), 0.1),
        "w_out": nrm(ks[14], (L, MIX_WIDTH, D), MIX_WIDTH ** -0.5),
        "peer_wq": nrm(ks[15], (L, D, PEER_HEADS * PEER_QDIM), D ** -0.5),
        "peer_keys": nrm(ks[16], (L, 2, PEER_NKEYS, PEER_QDIM // 2), (PEER_QDIM // 2) ** -0.5),
        "peer_u": nrm(ks[17], (L, PEER_EXPERTS, D), D ** -0.5),
        "peer_v": nrm(ks[18], (L, PEER_EXPERTS, D), PEER_HEADS ** -0.5),
    }


def reference(x, c, positions, norm1_g, norm2_g, w_ada, b_ada, w_in, q_norm_g, k_norm_g,
              attn_sink, conv_w, pool_w, pool_scale, w_out, peer_wq, peer_keys, peer_u, peer_v):
    b, s, d = x.shape
    cos, sin = rope_tables(positions)
    c_act = jax.nn.silu(c)
    for l in range(DEPTH):
        mod = (c_act @ w_ada[l] + b_ada[l])[:, None, :]
        shift1, scale1, gate1, shift2, scale2, gate2 = jnp.split(mod, 6, axis=-1)

        h = rms_norm(x, norm1_g[l]) * (1.0 + scale1) + shift1
        z = h @ w_in[l]
        q, k, v, conv_x, conv_b, conv_c, pool_x = jnp.split(z, IN_SPLITS, axis=-1)

        q = q.reshape(b, s, N_Q_HEADS, HEAD_DIM)
        k = k.reshape(b, s, N_KV_HEADS, HEAD_DIM)
        v = v.reshape(b, s, N_KV_HEADS, HEAD_DIM)
        q = partial_rope(rms_norm(q, q_norm_g[l]), cos, sin)
        k = partial_rope(rms_norm(k, k_norm_g[l]), cos, sin)
        attn_o = windowed_gqa(q, k, v, attn_sink[l])

        conv_o = conv_b * short_conv(conv_c * conv_x, conv_w[l])

        pooled = multiscale_pool(pool_x)
        pool_o = jnp.einsum('bsgc,gce->bsge', pooled, pool_w[l]).reshape(b, s, POOL_WIDTH) * pool_scale[l]

        mix = jnp.concatenate([attn_o, conv_o, pool_o], axis=-1) @ w_out[l]
        x = x + gate1 * mix

        h2 = rms_norm(x, norm2_g[l]) * (1.0 + scale2) + shift2
        x = x + gate2 * peer_ffn(h2, peer_wq[l], peer_keys[l], peer_u[l], peer_v[l])
    return x
```

```python
import math
from contextlib import ExitStack

import numpy as np
import concourse.bass as bass
import concourse.mybir as mybir
from concourse.bass_utils import run_bass_kernel_spmd

F32 = mybir.dt.float32
U32 = mybir.dt.uint32
I32 = mybir.dt.int32
ALU = mybir.AluOpType
AF = mybir.ActivationFunctionType
AX = mybir.AxisListType

ENGINES = ["pe", "act", "dve", "pool", "sp"]
EPS = 1e-6
PI = math.pi


class Prog:
    def __init__(self, nc):
        self.nc = nc
        self.ops = []
        self.last_w = {}
        self.readers = {}
        self.last_eng = {}
        self.last_chan = {}
        self.pending = {}
        self.burst = set()

    def add(self, eng, fn, r=(), w=(), dma=False, chan=None):
        oid = len(self.ops)
        deps = set()
        for k in r:
            if k in self.last_w:
                deps.add(self.last_w[k])
        for k in w:
            if k in self.last_w:
                deps.add(self.last_w[k])
            for q in self.readers.get(k, ()):
                deps.add(q)
        if eng in self.pending:
            deps |= self.pending.pop(eng)
        for k in r:
            self.readers.setdefault(k, []).append(oid)
        for k in w:
            self.last_w[k] = oid
            self.readers[k] = []
        deps.discard(oid)
        self.ops.append(dict(eng=eng, fn=fn, deps=sorted(deps), dma=dma, chan=chan))
        if dma:
            self.last_chan[chan] = oid
        else:
            self.last_eng[eng] = oid
        return oid

    def barrier(self):
        b = set(self.last_eng.values()) | set(self.last_chan.values())
        for e in ENGINES:
            self.pending[e] = set(b) | self.pending.get(e, set())

    def pe(self, fn, r=(), w=()):
        return self.add("pe", fn, r, w)

    def act(self, fn, r=(), w=()):
        return self.add("act", fn, r, w)

    def dve(self, fn, r=(), w=()):
        return self.add("dve", fn, r, w)

    def pool(self, fn, r=(), w=()):
        return self.add("pool", fn, r, w)

    def dma(self, fn, r=(), w=(), chan=None, eng="sp"):
        return self.add(eng, fn, r, w, dma=True, chan=chan)

    def emit(self, final_wait_chans=()):
        nc = self.nc
        ops = self.ops
        eng_count = {e: 0 for e in ENGINES}
        chan_count = {}
        for op in ops:
            if op["dma"]:
                c = op["chan"]
                chan_count[c] = chan_count.get(c, 0) + 16
                op["sig"] = ("c", c, chan_count[c])
                op["inc"] = True
            else:
                eng_count[op["eng"]] += 1
                op["sig"] = ("e", op["eng"], eng_count[op["eng"]])
        for op in ops:
            if op["dma"] and op["chan"] in self.burst:
                op["sig"] = ("c", op["chan"], chan_count[op["chan"]])
        with ExitStack() as st:
            sems = {}
            for e in ENGINES:
                sems[("e", e)] = st.enter_context(nc.semaphore("s_" + e))
            for c in chan_count:
                sems[("c", c)] = st.enter_context(nc.semaphore("c_" + str(c)))
            block = st.enter_context(nc.Block())

            def make(engname):
                def body(eng):
                    waited = {}
                    for op in ops:
                        if op["eng"] != engname:
                            continue
                        for d in op["deps"]:
                            kind, key, val = ops[d]["sig"]
                            if kind == "e" and key == "pe" and engname == "pe":
                                continue
                            sk = (kind, key)
                            if waited.get(sk, 0) >= val:
                                continue
                            eng.wait_ge(sems[sk], val)
                            waited[sk] = val
                        ins = op["fn"](eng)
                        kind, key, val = op["sig"]
                        ins.then_inc(sems[(kind, key)], 16 if kind == "c" else 1)
                    if engname == "sp":
                        for c in final_wait_chans:
                            eng.wait_ge(sems[("c", c)], chan_count[c])
                return body

            block.tensor(make("pe"))
            block.scalar(make("act"))
            block.vector(make("dve"))
            block.gpsimd(make("pool"))
            block.sync(make("sp"))
        return eng_count, chan_count


def build(NT, L, taps=(), do_peer=True, do_gather=True, stage='all', cut=99):
    S = NT * 128
    nc = bass.Bass("TRN2", target_bir_lowering=False)
    P = Prog(nc)
    P.burst = {"cst"} | {"lw%d" % l for l in range(L)}

    def din(name, shape, dt=F32):
        return nc.dram_tensor(name, shape, dt, kind="ExternalInput").ap()

    x_d = din("x", [S, 1024])
    cT_d = din("cT", [128, 8])
    pos_d = din("posT", [128, NT], I32)
    n1g_d = din("n1gT", [L, 128, 8])
    n2g_d = din("n2gT", [L, 128, 8])
    bada_d = din("badaT", [L, 128, 48])
    wada_d = din("wada", [L, 1024, 6144])
    win_d = din("win", [L, 1024, 1792])
    wout_d = din("wout", [L, 1024, 1024])
    wq_d = din("wq", [L, 1024, 2048])
    gqk_d = din("gqk", [L, 128, 640])
    sink_d = din("sink", [L, 128, 8])
    convw_d = din("convw", [L, 128, 6])
    poolw_d = din("poolw", [L, 2, 128, 128])
    pscale_d = din("pscale", [L, 128, 2])
    keysT_d = din("keysT", [L, 2, 128, 128])
    puv_d = [din("puv%d" % l, [16384, 2048]) for l in range(L)]
    ident_d = din("ident", [128, 128])
    ones_d = din("ones", [128, 128])
    mlo_d = din("mlo", [128, 128])
    mhi_d = din("mhi", [128, 128])
    iota_d = din("iota", [128, 256])
    invcnt_d = din("invcnt", [128, 6, 128])
    invfreq_d = din("invfreq", [128, 8])
    y_d = nc.dram_tensor("y", [S, 1024], F32, kind="ExternalOutput").ap()
    tap_out = {}

    with ExitStack() as st:
        def sb(name, shape, dt=F32):
            return st.enter_context(nc.sbuf_tensor(name, shape, dt))

        X = sb("X", [128, NT, 1024])
        ident = sb("ident_s", [128, 128])
        ones = sb("ones_s", [128, 128])
        mlo = sb("mlo_s", [128, 128])
        mhi = sb("mhi_s", [128, 128])
        iota = sb("iota_s", [128, 256])
        invcnt = sb("invcnt_s", [128, 6, 128])
        invfreq = sb("invfreq_s", [128, 8])
        cT = sb("cT_s", [128, 8])
        cact = sb("cact", [128, 8])
        posi = sb("posi", [128, NT], I32)
        posf = sb("posf", [128, NT])
        ang = sb("ang", [128, NT, 8])
        ang2 = sb("ang2", [128, NT, 8])
        angi = sb("angi", [128, NT, 8], I32)
        cosT = sb("cosT", [128, NT, 8])
        sinT = sb("sinT", [128, NT, 8])
        n1g = sb("n1g", [128, 8])
        n2g = sb("n2g", [128, 8])
        bada = sb("bada", [128, 48])
        modT = sb("modT", [128, 48])
        der = sb("der", [128, 32])
        GATE1 = sb("GATE1", [128, 1024])
        GATE2 = sb("GATE2", [128, 1024])
        gqk = sb("gqk_s", [128, 640])
        sinkt = sb("sinkt", [128, 8])
        esink = sb("esink", [128, 8])
        convw = sb("convw_s", [128, 6])
        poolw = sb("poolw_s", [128, 2, 128])
        pscale = sb("pscale_s", [128, 2])
        keysT = sb("keysT_s", [128, 2, 128])
        st8 = sb("st8", [128, 64])
        AW = 29400
        arena = sb("arena", [128, AW])
        ps = [st.enter_context(nc.psum_tensor("ps%d" % i, [128, 512], F32)) for i in range(8)]

        class Carver:
            def __init__(self):
                self.off = 0

            def get(self, words):
                a = arena[:, self.off:self.off + words]
                self.off += words
                assert self.off <= AW, self.off
                return a

        def tap(name, ap, shape, r, dt=F32):
            if name not in taps:
                return
            t = nc.dram_tensor("tap_" + name, shape, dt, kind="ExternalOutput").ap()
            tap_out[name] = t
            P.dma(lambda e: e.dma_start(out=t, in_=ap), r=r, chan="tap")

        def ld(dst, src, name, chan="cst"):
            P.dma(lambda e: e.dma_start(out=dst, in_=src), w=[name], chan=chan)

        ld(ident[:], ident_d, "ident")
        ld(ones[:], ones_d, "ones")
        ld(mlo[:], mlo_d, "mlo")
        ld(mhi[:], mhi_d, "mhi")
        ld(iota[:], iota_d, "iota")
        ld(invcnt[:], invcnt_d, "invcnt")
        ld(invfreq[:], invfreq_d, "invfreq")
        ld(cT[:], cT_d, "cT")
        ld(posi[:], pos_d, "posi")
        for n in range(NT):
            ld(X[:, n, :], x_d[n * 128:(n + 1) * 128, :], "X%d" % n, chan="xin%d" % n)

        P.act(lambda e: e.activation(out=cact[:], in_=cT[:], func=AF.Silu), r=["cT"], w=["cact"])
        P.dve(lambda e: e.tensor_copy(out=posf[:], in_=posi[:]), r=["posi"], w=["posf"])
        P.dve(lambda e: e.tensor_tensor(out=ang[:], in0=posf[:].unsqueeze(2).broadcast_to([128, NT, 8]),
                                        in1=invfreq[:].unsqueeze(1).broadcast_to([128, NT, 8]), op=ALU.mult),
              r=["posf", "invfreq"], w=["ang"])
        C1 = 6.28125
        C2 = 2 * PI - C1
        P.dve(lambda e: e.tensor_scalar(out=ang2[:], in0=ang[:], scalar1=1.0 / (2 * PI), scalar2=None, op0=ALU.mult), r=["ang"], w=["ang2"])
        P.dve(lambda e: e.tensor_copy(out=angi[:], in_=ang2[:]), r=["ang2"], w=["angi"])
        P.dve(lambda e: e.tensor_copy(out=ang2[:], in_=angi[:]), r=["angi"], w=["ang2"])
        P.dve(lambda e: e.scalar_tensor_tensor(out=ang[:], in0=ang2[:], scalar=-C1, in1=ang[:], op0=ALU.mult, op1=ALU.add), r=["ang2", "ang"], w=["ang"])
        P.dve(lambda e: e.scalar_tensor_tensor(out=ang[:], in0=ang2[:], scalar=-C2, in1=ang[:], op0=ALU.mult, op1=ALU.add), r=["ang2", "ang"], w=["ang"])

        def wrap():
            P.dve(lambda e: e.tensor_scalar(out=ang2[:], in0=ang[:], scalar1=PI, scalar2=-2 * PI, op0=ALU.is_gt, op1=ALU.mult), r=["ang", "sinT", "cosT"], w=["ang2"])
            P.dve(lambda e: e.tensor_tensor(out=ang[:], in0=ang[:], in1=ang2[:], op=ALU.add), r=["ang", "ang2"], w=["ang"])
            P.dve(lambda e: e.tensor_scalar(out=ang[:], in0=ang[:], scalar1=-PI, scalar2=PI, op0=ALU.max, op1=ALU.min), r=["ang"], w=["ang"])

        wrap()
        P.act(lambda e: e.activation(out=sinT[:], in_=ang[:], func=AF.Sin), r=["ang"], w=["sinT"])
        P.dve(lambda e: e.tensor_scalar(out=ang[:], in0=ang[:], scalar1=0.5 * PI, scalar2=None, op0=ALU.add), r=["ang", "sinT"], w=["ang"])
        wrap()
        P.act(lambda e: e.activation(out=cosT[:], in_=ang[:], func=AF.Sin), r=["ang"], w=["cosT"])
        tap("cos", cosT[:], [128, NT, 8], ["cosT"])
        tap("sin", sinT[:], [128, NT, 8], ["sinT"])

        def rstd_from_ss(ss_ap, out_ap, n, rname, wname):
            P.dve(lambda e: e.tensor_scalar(out=out_ap, in0=ss_ap, scalar1=1.0 / n, scalar2=EPS, op0=ALU.mult, op1=ALU.add),
                  r=[rname], w=[wname])
            P.act(lambda e: e.activation(out=out_ap, in_=out_ap, func=AF.Sqrt), r=[wname], w=[wname])
            P.dve(lambda e: e.reciprocal(out=out_ap, in_=out_ap), r=[wname], w=[wname])

        def MM(out, lhsT, rhs, start, stop):
            return lambda e: e.matmul(out, lhsT=lhsT, rhs=rhs, start=start, stop=stop, skip_group_check=True)

        def TR(out, in_, idn):
            return lambda e: e.transpose(out=out, in_=in_, identity=idn)

        def layer(l):
            G1s, SH1 = der[:, 0:8], modT[:, 0:8]
            G2s, SH2 = der[:, 8:16], modT[:, 24:32]
            def prologue():
                P.barrier()
                cv = Carver()
                wad = [cv.get(3072), cv.get(3072)]
                Dt = cv.get(128)
                ld(n1g[:], n1g_d[l], "n1g", chan="lw%d" % l)
                ld(n2g[:], n2g_d[l], "n2g", chan="lw%d" % l)
                ld(bada[:], bada_d[l], "bada", chan="lw%d" % l)
                ld(gqk[:], gqk_d[l], "gqk", chan="lw%d" % l)
                ld(sinkt[:], sink_d[l], "sinkt", chan="lw%d" % l)
                ld(convw[:], convw_d[l], "convw", chan="lw%d" % l)
                for j in range(2):
                    ld(poolw[:, j, :], poolw_d[l, j], "poolw%d" % j, chan="lw%d" % l)
                    ld(keysT[:, j, :], keysT_d[l, j], "keysT%d" % j, chan="lw%d" % l)
                ld(pscale[:], pscale_d[l], "pscale", chan="lw%d" % l)
                first = True
                pi = 0
                for kc in range(8):
                    for half in range(2):
                        s = pi % 2
                        pi += 1
                        P.dma((lambda s, kc, half: lambda e: e.dma_start(
                            out=wad[s], in_=wada_d[l, kc * 128:(kc + 1) * 128, half * 3072:(half + 1) * 3072]))(s, kc, half),
                            w=["wad%d" % s], chan="wad%d" % s)
                        for jj in range(24):
                            j = half * 24 + jj
                            P.pe(MM(ps[4][:, j:j + 1], wad[s][:, jj * 128:(jj + 1) * 128], cact[:, kc:kc + 1], first, kc == 7),
                                 r=["wad%d" % s, "cact"], w=["ps4"])
                            first = False
                P.dve(lambda e: e.tensor_tensor(out=modT[:], in0=ps[4][:, 0:48], in1=bada[:], op=ALU.add),
                      r=["ps4", "bada"], w=["modT"])
                tap("modT%d" % l, modT[:], [128, 48], ["modT"])
                P.dve(lambda e: e.scalar_tensor_tensor(out=der[:, 0:8], in0=modT[:, 8:16], scalar=1.0, in1=n1g[:], op0=ALU.add, op1=ALU.mult),
                      r=["modT", "n1g"], w=["der"])
                P.dve(lambda e: e.scalar_tensor_tensor(out=der[:, 8:16], in0=modT[:, 32:40], scalar=1.0, in1=n2g[:], op0=ALU.add, op1=ALU.mult),
                      r=["modT", "n2g", "der"], w=["der"])
                for gi, (GT, c0) in enumerate(((GATE1, 16), (GATE2, 40))):
                    for ch in range(8):
                        P.dve((lambda ch, c0: lambda e: e.tensor_scalar(out=Dt, in0=ident[:], scalar1=modT[:, c0 + ch:c0 + ch + 1], scalar2=None, op0=ALU.mult))(ch, c0),
                              r=["ident", "modT"], w=["Dt"])
                        P.pe(MM(ps[ch // 4][:, (ch % 4) * 128:(ch % 4 + 1) * 128], ones[:], Dt, True, True),
                             r=["ones", "Dt"], w=["ps%d" % (ch // 4)])
                    for b in range(2):
                        P.act((lambda b, GT: lambda e: e.copy(out=GT[:, b * 512:(b + 1) * 512], in_=ps[b][:]))(b, GT),
                              r=["ps%d" % b], w=["GATE%d" % gi])
                tap("gate1_%d" % l, GATE1[:], [128, 1024], ["GATE0"])
                P.act(lambda e: e.activation(out=esink[:], in_=sinkt[:], func=AF.Exp), r=["sinkt"], w=["esink"])

            def mixing():
                P.barrier()
                cv = Carver()
                wi = [cv.get(1792) for _ in range(2)]
                wo = [cv.get(1024) for _ in range(2)]
                xn = cv.get(1024)
                hT = cv.get(1024).rearrange("p (c t) -> p c t", t=128)
                zsb = cv.get(1792)
                sq = cv.get(640)
                qn = cv.get(640)
                rt = [cv.get(80).rearrange("p (h d) -> p h d", d=8) for _ in range(6)]
                qT = [cv.get(1024).rearrange("p (h t) -> p h t", t=128) for _ in range(2)]
                kT = [cv.get(256).rearrange("p (g t) -> p g t", t=128) for _ in range(4)]
                vR = [cv.get(128).rearrange("p (g d) -> p g d", d=64) for _ in range(4)]
                Wb = [cv.get(1152).rearrange("p (c t) -> p c t", t=144) for _ in range(2)]
                tail = [cv.get(64).rearrange("p (c t) -> p c t", t=8) for _ in range(2)]
                cx = cv.get(144)
                cacc = cv.get(128)
                Alv = [cv.get(144) for _ in range(4)]
                ptmp = cv.get(128)
                pooled = cv.get(128)
                cpT = [cv.get(512).rearrange("p (c t) -> p c t", t=128) for _ in range(2)]
                PT = [cv.get(512) for _ in range(2)]
                den = cv.get(512)
                attnT = cv.get(1024).rearrange("p (h t) -> p h t", t=128)
                otmp = cv.get(1024)
                junk = otmp
                ss = st8[:, 0:1]
                rstd = st8[:, 1:2]
                ssq = st8[:, 8:18]
                rsq = st8[:, 24:34]
                wi_n = [0]
                wo_n = [0]

                def stepA(n):
                    Xn = X[:, n, :]
                    xr = "X%d" % n
                    P.dve(lambda e: e.memset(ss, 0.0), w=["ss"])
                    P.act(lambda e: e.activation(out=junk, in_=Xn, func=AF.Square, accum_out=ss), r=[xr, "ss"], w=["junk", "ss"])
                    rstd_from_ss(ss, rstd, 1024, "ss", "rstd")
                    P.dve(lambda e: e.tensor_scalar(out=xn, in0=Xn, scalar1=rstd, scalar2=None, op0=ALU.mult), r=[xr, "rstd"], w=["xn"])
                    if n == 0:
                        tap("xn%d" % l, xn, [128, 1024], ["xn"])
                        tap("st8_%d" % l, st8[:], [128, 64], ["rstd", "ss"])
                    for half in range(2):
                        for c4 in range(4):
                            kc = half * 4 + c4
                            P.pe(TR(ps[4][:, c4 * 128:(c4 + 1) * 128], xn[:, kc * 128:(kc + 1) * 128], ident[:]),
                                 r=["xn", "ident"], w=["ps4"])
                        for c4 in range(4):
                            kc = half * 4 + c4
                            P.dve((lambda kc, c4: lambda e: e.tensor_scalar(
                                out=hT[:, kc, :], in0=ps[4][:, c4 * 128:(c4 + 1) * 128], scalar1=G1s[:, kc:kc + 1], scalar2=SH1[:, kc:kc + 1],
                                op0=ALU.mult, op1=ALU.add))(kc, c4), r=["ps4", "der", "modT"], w=["hT"])
                    if n == 0:
                        tap("hT%d" % l, hT, [128, 8, 128], ["hT"])
                    if cut <= 1:
                        return
                    widths = [512, 512, 512, 256]
                    for kc in range(8):
                        s = wi_n[0] % 2
                        wi_n[0] += 1
                        P.dma((lambda s, kc: lambda e: e.dma_start(out=wi[s], in_=win_d[l, kc * 128:(kc + 1) * 128, :]))(s, kc),
                              w=["wi%d" % s], chan="wi%d" % s)
                        for b in range(4):
                            P.pe(MM(ps[b][:, 0:widths[b]], hT[:, kc, :], wi[s][:, b * 512:b * 512 + widths[b]], kc == 0, kc == 7),
                                 r=["hT", "wi%d" % s], w=["ps%d" % b])
                    for b in range(4):
                        if b % 2 == 0:
                            P.act((lambda b: lambda e: e.copy(out=zsb[:, b * 512:b * 512 + widths[b]], in_=ps[b][:, 0:widths[b]]))(b),
                                  r=["ps%d" % b], w=["zsb%d" % b])
                        else:
                            P.dve((lambda b: lambda e: e.tensor_copy(out=zsb[:, b * 512:b * 512 + widths[b]], in_=ps[b][:, 0:widths[b]]))(b),
                                  r=["ps%d" % b], w=["zsb%d" % b])
                    zall = ["zsb0", "zsb1", "zsb2", "zsb3"]
                    if cut <= 2:
                        return
                    if n == 0:
                        tap("z%d" % l, zsb, [128, 1792], zall)
                    zqk = zsb[:, 0:640]
                    P.dve(lambda e: e.tensor_tensor(out=sq, in0=zqk, in1=zqk, op=ALU.mult), r=zall, w=["sq"])
                    P.dve(lambda e: e.tensor_reduce(out=ssq, in_=sq.rearrange("p (h d) -> p h d", d=64), axis=AX.X, op=ALU.add),
                          r=["sq"], w=["ssq"])
                    rstd_from_ss(ssq, rsq, 64, "ssq", "rsq")
                    qn3 = qn.rearrange("p (h d) -> p h d", d=64)
                    P.dve(lambda e: e.tensor_tensor(out=qn3, in0=zqk.rearrange("p (h d) -> p h d", d=64),
                                                    in1=rsq.unsqueeze(2).broadcast_to([128, 10, 64]), op=ALU.mult),
                          r=zall + ["rsq"], w=["qn"])
                    P.dve(lambda e: e.tensor_tensor(out=qn, in0=qn, in1=gqk[:], op=ALU.mult), r=["qn", "gqk"], w=["qn"])
                    cosb = cosT[:, n, :].unsqueeze(1).broadcast_to([128, 10, 8])
                    if cut <= 3:
                        return
                    sinb = sinT[:, n, :].unsqueeze(1).broadcast_to([128, 10, 8])
                    t1 = qn3[:, :, 0:8]
                    t2 = qn3[:, :, 8:16]
                    P.dve(lambda e: e.tensor_tensor(out=rt[0], in0=t1, in1=cosb, op=ALU.mult), r=["qn", "cosT"], w=["rt0"])
                    P.dve(lambda e: e.tensor_tensor(out=rt[1], in0=t2, in1=sinb, op=ALU.mult), r=["qn", "sinT"], w=["rt1"])
                    P.dve(lambda e: e.tensor_tensor(out=rt[2], in0=t2, in1=cosb, op=ALU.mult), r=["qn", "cosT"], w=["rt2"])
                    P.dve(lambda e: e.tensor_tensor(out=rt[3], in0=t1, in1=sinb, op=ALU.mult), r=["qn", "sinT"], w=["rt3"])
                    P.dve(lambda e: e.tensor_tensor(out=t1, in0=rt[0], in1=rt[1], op=ALU.subtract), r=["rt0", "rt1", "rt2", "rt3", "qn"], w=["qn"])
                    P.dve(lambda e: e.tensor_tensor(out=t2, in0=rt[2], in1=rt[3], op=ALU.add), r=["rt2", "rt3", "qn"], w=["qn"])
                    if n == 0:
                        tap("qn%d" % l, qn, [128, 640], ["qn"])
                    if cut <= 4:
                        return
                    qs = n % 2
                    ks = n % 4
                    for grp in range(2):
                        for h4 in range(4):
                            h = grp * 4 + h4
                            P.pe(TR(ps[4][0:64, h4 * 128:(h4 + 1) * 128], qn3[:, h, :], ident[:]), r=["qn", "ident"], w=["ps4"])
                        P.act((lambda grp: lambda e: e.activation(
                            out=qT[qs][0:64, grp * 4:(grp + 1) * 4, :], in_=ps[4][0:64, :].rearrange("p (h t) -> p h t", t=128),
                            func=AF.Copy, scale=0.125))(grp), r=["ps4"], w=["qT%d" % qs])
                    for g in range(2):
                        P.pe(TR(ps[4][0:64, g * 128:(g + 1) * 128], qn3[:, 8 + g, :], ident[:]), r=["qn", "ident"], w=["ps4"])
                    P.dve(lambda e: e.tensor_copy(out=kT[ks][0:64, :, :], in_=ps[4][0:64, 0:256].rearrange("p (g t) -> p g t", t=128)),
                          r=["ps4"], w=["kT%d" % ks])
                    P.act(lambda e: e.copy(out=vR[ks], in_=zsb[:, 640:768].rearrange("p (g d) -> p g d", d=64)), r=zall, w=["vR%d" % ks])
                    if cut <= 5:
                        return
                    ws = n % 2
                    for half in range(2):
                        for c4 in range(4):
                            c = half * 4 + c4
                            P.pe(TR(ps[4][:, c4 * 128:(c4 + 1) * 128], zsb[:, 768 + c * 128:768 + (c + 1) * 128], ident[:]),
                                 r=zall + ["ident"], w=["ps4"])
                        pv4 = ps[4][:, :].rearrange("p (c t) -> p c t", t=128)
                        cs = slice(half * 4, half * 4 + 4)
                        P.act((lambda cs, pv4: lambda e: e.copy(out=Wb[ws][:, cs, 8:136], in_=pv4))(cs, pv4), r=["ps4"], w=["Wb%dm" % ws])
                    if cut <= 6:
                        return
                    if n == 0:
                        P.dve(lambda e: e.memset(Wb[ws][:, :, 0:8], 0.0), w=["Wb%dl" % ws])
                    else:
                        P.dve(lambda e: e.tensor_copy(out=Wb[1 - ws][:, :, 136:144], in_=Wb[ws][:, :, 8:16]), r=["Wb%dm" % ws], w=["Wb%dr" % (1 - ws)])
                        P.dve(lambda e: e.tensor_copy(out=Wb[ws][:, :, 0:8], in_=Wb[1 - ws][:, :, 128:136]), r=["Wb%dm" % (1 - ws)], w=["Wb%dl" % ws])
                    if n == NT - 1:
                        P.dve(lambda e: e.memset(Wb[ws][:, :, 136:144], 0.0), w=["Wb%dr" % ws])

                def stepC(m):
                    ws = m % 2
                    W = Wb[ws]
                    wr = ["Wb%dm" % ws, "Wb%dl" % ws, "Wb%dr" % ws]
                    cp = cpT[ws]
                    edge = 0 if m == 0 else (2 if m == NT - 1 else 1)
                    for j in range(2):
                        P.dve((lambda j: lambda e: e.tensor_tensor(out=cx, in0=W[:, 4 + j, :], in1=W[:, j, :], op=ALU.mult))(j), r=wr, w=["cx"])
                        P.dve((lambda j: lambda e: e.tensor_scalar(out=cacc, in0=cx[:, 8:136], scalar1=convw[:, j * 3 + 1:j * 3 + 2], scalar2=None, op0=ALU.mult))(j),
                              r=["cx", "convw"], w=["cacc"])
                        P.dve((lambda j: lambda e: e.scalar_tensor_tensor(out=cacc, in0=cx[:, 7:135], scalar=convw[:, j * 3:j * 3 + 1], in1=cacc, op0=ALU.mult, op1=ALU.add))(j),
                              r=["cx", "convw", "cacc"], w=["cacc"])
                        P.dve((lambda j: lambda e: e.scalar_tensor_tensor(out=cacc, in0=cx[:, 9:137], scalar=convw[:, j * 3 + 2:j * 3 + 3], in1=cacc, op0=ALU.mult, op1=ALU.add))(j),
                              r=["cx", "convw", "cacc"], w=["cacc"])
                        P.dve((lambda j: lambda e: e.tensor_tensor(out=cp[:, j, :], in0=cacc, in1=W[:, 2 + j, 8:136], op=ALU.mult))(j),
                              r=wr + ["cacc"], w=["cpT%d" % ws])
                    for j in range(2):
                        u = W[:, 6 + j, :]
                        A1, A4, A8, A16 = Alv
                        P.dve((lambda u: lambda e: e.tensor_tensor(out=A1[:, 1:144], in0=u[:, 0:143], in1=u[:, 1:144], op=ALU.add))(u), r=wr, w=["A1"])
                        P.dve(lambda e: e.tensor_tensor(out=A4[:, 2:143], in0=A1[:, 1:142], in1=A1[:, 3:144], op=ALU.add), r=["A1"], w=["A4"])
                        if j == 0:
                            lo, hi = A1, A4
                        else:
                            P.dve(lambda e: e.tensor_tensor(out=A8[:, 4:141], in0=A4[:, 2:139], in1=A4[:, 6:143], op=ALU.add), r=["A4"], w=["A8"])
                            P.dve(lambda e: e.tensor_tensor(out=A16[:, 8:137], in0=A8[:, 4:133], in1=A8[:, 12:141], op=ALU.add), r=["A8"], w=["A16"])
                            lo, hi = A8, A16
                        for (p0, p1, A) in ((0, 64, lo), (64, 128, hi)):
                            P.dve((lambda p0, p1, A, j: lambda e: e.tensor_tensor(out=ptmp[p0:p1, :], in0=A[p0:p1, 8:136], in1=invcnt[p0:p1, j * 3 + edge, :], op=ALU.mult))(p0, p1, A, j),
                                  r=["A1", "A4", "A8", "A16", "invcnt"], w=["ptmp"])
                        P.dve((lambda u: lambda e: e.tensor_tensor(out=pooled, in0=ptmp, in1=u[:, 8:136], op=ALU.subtract))(u), r=["ptmp"] + wr, w=["pooled"])
                        P.pe(MM(ps[5][:, 0:128], poolw[:, j, :], pooled, True, True), r=["pooled", "poolw%d" % j], w=["ps5"])
                        P.act((lambda j: lambda e: e.activation(out=cp[:, 2 + j, :], in_=ps[5][:, 0:128], func=AF.Copy, scale=pscale[:, j:j + 1]))(j),
                              r=["ps5", "pscale"], w=["cpT%d" % ws])
                    if m == 0:
                        tap("cp%d" % l, cp, [128, 4, 128], ["cpT%d" % ws])

                def stepD(m):
                    qs = m % 2
                    blocks = [j for j in (m - 1, m, m + 1) if 0 <= j < NT]
                    pslot = [0]
                    for g in range(2):
                        for bi, j in enumerate(blocks):
                            ks = j % 4
                            P.pe(MM(ps[5][:, :], kT[ks][0:64, g, :], qT[qs][0:64, g * 4:(g + 1) * 4, :], True, True),
                                 r=["kT%d" % ks, "qT%d" % qs], w=["ps5"])
                            s = pslot[0] % 2
                            pslot[0] += 1
                            P.act((lambda s: lambda e: e.activation(out=PT[s], in_=ps[5][:, :], func=AF.Exp))(s), r=["ps5"], w=["PT%d" % s])
                            if j != m:
                                mk = mlo if j < m else mhi
                                P.dve((lambda s, mk: lambda e: e.tensor_tensor(
                                    out=PT[s].rearrange("p (h t) -> p h t", t=128), in0=PT[s].rearrange("p (h t) -> p h t", t=128),
                                    in1=mk[:].unsqueeze(1).broadcast_to([128, 4, 128]), op=ALU.mult))(s, mk),
                                    r=["PT%d" % s, "mlo", "mhi"], w=["PT%d" % s])
                            P.pe(MM(ps[6][0:64, :], vR[ks][:, g, :], PT[s], bi == 0, bi == len(blocks) - 1), r=["vR%d" % ks, "PT%d" % s], w=["ps6"])
                            P.pe(MM(ps[7][0:64, :], ones[:, 0:64], PT[s], bi == 0, bi == len(blocks) - 1), r=["ones", "PT%d" % s], w=["ps7"])
                        P.dve((lambda g: lambda e: e.tensor_tensor(
                            out=den[0:64, :].rearrange("p (h t) -> p h t", t=128), in0=ps[7][0:64, :].rearrange("p (h t) -> p h t", t=128),
                            in1=esink[0:64, g * 4:(g + 1) * 4].unsqueeze(2).broadcast_to([64, 4, 128]), op=ALU.add))(g),
                            r=["ps7", "esink"], w=["den"])
                        P.dve(lambda e: e.reciprocal(out=den[0:64, :], in_=den[0:64, :]), r=["den"], w=["den"])
                        P.dve((lambda g: lambda e: e.tensor_tensor(
                            out=attnT[0:64, g * 4:(g + 1) * 4, :], in0=ps[6][0:64, :].rearrange("p (h t) -> p h t", t=128),
                            in1=den[0:64, :].rearrange("p (h t) -> p h t", t=128), op=ALU.mult))(g),
                            r=["ps6", "den"], w=["attnT"])
                    if m == 0:
                        tap("attnT%d" % l, attnT[0:64, :, :], [64, 8, 128], ["attnT"])
                    cp = cpT[m % 2]
                    npieces = 12
                    for pc in range(npieces):
                        s = wo_n[0] % 2
                        wo_n[0] += 1
                        if pc < 8:
                            P.dma((lambda s, pc: lambda e: e.dma_start(out=wo[s][0:64, :], in_=wout_d[l, pc * 64:(pc + 1) * 64, :]))(s, pc),
                                  w=["wo%d" % s], chan="wo%d" % s)
                            lhs = attnT[0:64, pc, :]
                            rw = wo[s][0:64, :]
                            rr = ["attnT"]
                        else:
                            c = pc - 8
                            P.dma((lambda s, c: lambda e: e.dma_start(out=wo[s], in_=wout_d[l, 512 + c * 128:512 + (c + 1) * 128, :]))(s, c),
                                  w=["wo%d" % s], chan="wo%d" % s)
                            lhs = cp[:, c, :]
                            rw = wo[s]
                            rr = ["cpT%d" % (m % 2)]
                        for b in range(2):
                            P.pe(MM(ps[b][:, :], lhs, rw[:, b * 512:(b + 1) * 512], pc == 0, pc == npieces - 1), r=rr + ["wo%d" % s], w=["ps%d" % b])
                    for b in range(2):
                        P.dve((lambda b: lambda e: e.tensor_tensor(out=otmp[:, b * 512:(b + 1) * 512], in0=ps[b][:, :], in1=GATE1[:, b * 512:(b + 1) * 512], op=ALU.mult))(b),
                              r=["ps%d" % b, "GATE0"], w=["junk"])
                    P.dve(lambda e: e.tensor_tensor(out=X[:, m, :], in0=X[:, m, :], in1=otmp, op=ALU.add), r=["junk", "X%d" % m], w=["X%d" % m])

                for n in range(NT):
                    if stage in ('A', 'AC', 'all'):
                        stepA(n)
                    if n >= 1:
                        if stage in ('AC', 'all'):
                            stepC(n - 1)
                        if stage == 'all':
                            stepD(n - 1)
                if stage in ('AC', 'all'):
                    stepC(NT - 1)
                if stage == 'all':
                    stepD(NT - 1)
                tap("xmid%d" % l, X[:, 0, :], [128, 1024], ["X0"])

            def peer():
                P.barrier()
                cv = Carver()
                wqr = [cv.get(1024) for _ in range(2)]
                xn = cv.get(1024)
                h2T = cv.get(1024).rearrange("p (c t) -> p c t", t=128)
                h2 = [cv.get(1024) for _ in range(2)]
                pqT = cv.get(2048).rearrange("p (c t) -> p c t", t=128)
                sc = cv.get(256)
                scr = cv.get(256)
                vv = cv.get(32)
                ii = cv.get(32).bitcast(U32)
                iif = cv.get(32)
                cand = cv.get(256)
                cand2 = cv.get(256)
                cidx = cv.get(256)
                top = cv.get(16)
                pos = cv.get(16).bitcast(U32)
                posff = cv.get(16)
                junk2 = cv.get(256)
                ex = cv.get(16)
                gate = [cv.get(128) for _ in range(2)]
                eidxf = cv.get(128)
                eidxi = [cv.get(128).bitcast(I32) for _ in range(2)]
                actv = cv.get(128)
                ag = cv.get(128)
                wgt = cv.get(128)
                NU = 7
                ub = [cv.get(2048) for _ in range(NU)]
                dg = [cv.get(128) for _ in range(3)]
                dg_n = [0]
                yacc = cv.get(1024)
                djunk = cv.get(1024)
                ss = st8[:, 0:1]
                rstd = st8[:, 1:2]
                negm = st8[:, 2:3]
                gs = st8[:, 3:4]
                wq_n = [0]
                ub_n = [0]

                def route(n):
                    p = n % 2
                    Xn = X[:, n, :]
                    xr = "X%d" % n
                    P.dve(lambda e: e.memset(ss, 0.0), w=["ss"])
                    P.act(lambda e: e.activation(out=xn, in_=Xn, func=AF.Square, accum_out=ss), r=[xr, "ss"], w=["xn", "ss"])
                    rstd_from_ss(ss, rstd, 1024, "ss", "rstd")
                    P.dve(lambda e: e.tensor_scalar(out=xn, in0=Xn, scalar1=rstd, scalar2=None, op0=ALU.mult), r=[xr, "rstd"], w=["xn"])
                    yield
                    for half in range(2):
                        for c4 in range(4):
                            kc = half * 4 + c4
                            P.pe(TR(ps[4][:, c4 * 128:(c4 + 1) * 128], xn[:, kc * 128:(kc + 1) * 128], ident[:]), r=["xn", "ident"], w=["ps4"])
                        for c4 in range(4):
                            kc = half * 4 + c4
                            P.dve((lambda kc, c4: lambda e: e.tensor_scalar(
                                out=h2T[:, kc, :], in0=ps[4][:, c4 * 128:(c4 + 1) * 128], scalar1=G2s[:, kc:kc + 1], scalar2=SH2[:, kc:kc + 1],
                                op0=ALU.mult, op1=ALU.add))(kc, c4), r=["ps4", "der", "modT"], w=["h2T"])
                        yield
                    for half in range(2):
                        for c4 in range(4):
                            kc = half * 4 + c4
                            P.pe(TR(ps[4][:, c4 * 128:(c4 + 1) * 128], h2T[:, kc, :], ident[:]), r=["h2T", "ident"], w=["ps4"])
                        P.act((lambda half: lambda e: e.copy(out=h2[p][:, half * 512:(half + 1) * 512], in_=ps[4][:, :]))(half), r=["ps4"], w=["h2_%d" % p])
                        yield
                    if n == 0:
                        tap("h2_%d" % l, h2[p], [128, 1024], ["h2_%d" % p])
                    for kc in range(8):
                        for hf in range(2):
                            s = wq_n[0] % 2
                            wq_n[0] += 1
                            P.dma((lambda s, kc, hf: lambda e: e.dma_start(out=wqr[s], in_=wq_d[l, kc * 128:(kc + 1) * 128, hf * 1024:(hf + 1) * 1024]))(s, kc, hf),
                                  w=["wq%d" % s], chan="wq%d" % s)
                            for j in range(8):
                                c = hf * 8 + j
                                b = c // 4
                                P.pe(MM(ps[b][:, (c % 4) * 128:(c % 4 + 1) * 128], wqr[s][:, j * 128:(j + 1) * 128], h2T[:, kc, :],
                                        kc == 0 and c % 4 == 0, kc == 7), r=["wq%d" % s, "h2T"], w=["ps%d" % b])
                            yield
                    for b in range(4):
                        src = ps[b][:, :].rearrange("p (c t) -> p c t", t=128)
                        if b % 2 == 0:
                            P.act((lambda b, src: lambda e: e.copy(out=pqT[:, b * 4:(b + 1) * 4, :], in_=src))(b, src), r=["ps%d" % b], w=["pqT%d" % b])
                        else:
                            P.dve((lambda b, src: lambda e: e.tensor_copy(out=pqT[:, b * 4:(b + 1) * 4, :], in_=src))(b, src), r=["ps%d" % b], w=["pqT%d" % b])
                    pq_all = ["pqT0", "pqT1", "pqT2", "pqT3"]
                    P.dve(lambda e: e.memset(eidxf, 0.0), w=["eidxf"])
                    yield
                    for h in range(8):
                        for sd in range(2):
                            P.pe(MM(ps[5][:, sd * 128:(sd + 1) * 128], pqT[:, 2 * h + sd, :], keysT[:, sd, :], True, True),
                                 r=pq_all + ["keysT%d" % sd], w=["ps5"])
                        P.act(lambda e: e.copy(out=sc, in_=ps[5][:, 0:256]), r=["ps5"], w=["sc"])
                        if n == 0 and h == 0:
                            tap("sc%d" % l, sc, [128, 256], ["sc"])
                        for sd in range(2):
                            src = sc[:, sd * 128:(sd + 1) * 128]
                            rep = scr[:, sd * 128:(sd + 1) * 128]
                            v = vv[:, sd * 16:(sd + 1) * 16]
                            ix = ii[:, sd * 16:(sd + 1) * 16]
                            P.dve((lambda v, src: lambda e: e.max(out=v[:, 0:8], in_=src))(v, src), r=["sc"], w=["vv"])
                            P.dve((lambda v, src, ix: lambda e: e.max_index(out=ix[:, 0:8], in_max=v[:, 0:8], in_values=src))(v, src, ix), r=["sc", "vv"], w=["ii"])
                            P.dve((lambda v, src, rep: lambda e: e.match_replace(out=rep, in_to_replace=v[:, 0:8], in_values=src, imm_value=-1e30))(v, src, rep),
                                  r=["sc", "vv"], w=["scr"])
                            P.dve((lambda v, rep: lambda e: e.max(out=v[:, 8:16], in_=rep))(v, rep), r=["scr"], w=["vv"])
                            P.dve((lambda v, rep, ix: lambda e: e.max_index(out=ix[:, 8:16], in_max=v[:, 8:16], in_values=rep))(v, rep, ix), r=["scr", "vv"], w=["ii"])
                        yield
                        P.dve(lambda e: e.tensor_copy(out=iif, in_=ii), r=["ii"], w=["iif"])
                        P.dve(lambda e: e.tensor_scalar(out=iif[:, 0:16], in0=iif[:, 0:16], scalar1=128.0, scalar2=None, op0=ALU.mult), r=["iif"], w=["iif"])
                        c3 = cand.rearrange("p (a b) -> p a b", b=16)
                        x3 = cidx.rearrange("p (a b) -> p a b", b=16)
                        P.dve(lambda e: e.tensor_tensor(out=c3, in0=vv[:, 0:16].unsqueeze(2).broadcast_to([128, 16, 16]),
                                                        in1=vv[:, 16:32].unsqueeze(1).broadcast_to([128, 16, 16]), op=ALU.add), r=["vv"], w=["cand"])
                        P.dve(lambda e: e.tensor_tensor(out=x3, in0=iif[:, 0:16].unsqueeze(2).broadcast_to([128, 16, 16]),
                                                        in1=iif[:, 16:32].unsqueeze(1).broadcast_to([128, 16, 16]), op=ALU.add), r=["iif"], w=["cidx"])
                        P.dve(lambda e: e.max(out=top[:, 0:8], in_=cand), r=["cand"], w=["top"])
                        P.dve(lambda e: e.max_index(out=pos[:, 0:8], in_max=top[:, 0:8], in_values=cand), r=["cand", "top"], w=["pos"])
                        P.dve(lambda e: e.match_replace(out=cand2, in_to_replace=top[:, 0:8], in_values=cand, imm_value=-1e30), r=["cand", "top"], w=["cand2"])
                        P.dve(lambda e: e.max(out=top[:, 8:16], in_=cand2), r=["cand2"], w=["top"])
                        P.dve(lambda e: e.max_index(out=pos[:, 8:16], in_max=top[:, 8:16], in_values=cand2), r=["cand2", "top"], w=["pos"])
                        yield
                        P.dve(lambda e: e.tensor_scalar(out=negm, in0=top[:, 0:1], scalar1=-1.0, scalar2=None, op0=ALU.mult), r=["top"], w=["negm"])
                        P.dve(lambda e: e.memset(gs, 0.0), w=["gs"])
                        P.act(lambda e: e.activation(out=ex, in_=top, func=AF.Exp, bias=negm, accum_out=gs), r=["top", "negm", "gs"], w=["ex", "gs"])
                        P.dve(lambda e: e.reciprocal(out=gs, in_=gs), r=["gs"], w=["gs"])
                        P.dve((lambda h: lambda e: e.tensor_scalar(out=gate[p][:, h * 16:(h + 1) * 16], in0=ex, scalar1=gs, scalar2=None, op0=ALU.mult))(h),
                              r=["ex", "gs"], w=["gate_%d" % p])
                        P.dve(lambda e: e.tensor_copy(out=posff, in_=pos), r=["pos"], w=["posff"])
                        for k in range(16):
                            P.dve((lambda h, k: lambda e: e.scalar_tensor_tensor(
                                out=junk2, in0=iota[:], scalar=posff[:, k:k + 1], in1=cidx, op0=ALU.is_equal, op1=ALU.mult,
                                accum_out=eidxf[:, h * 16 + k:h * 16 + k + 1]))(h, k), r=["iota", "posff", "cidx", "eidxf"], w=["junk2", "eidxf"])
                            if k % 8 == 7:
                                yield
                    P.dve(lambda e: e.tensor_copy(out=eidxi[p], in_=eidxf), r=["eidxf"], w=["eidxi_%d" % p])
                    if n == 0:
                        tap("eidx%d" % l, eidxi[p], [128, 128], ["eidxi_%d" % p], dt=I32)
                        tap("gate%d" % l, gate[p], [128, 128], ["gate_%d" % p])

                def gather(n, nxt):
                    p = n % 2
                    Xn = X[:, n, :]
                    xr = "X%d" % n
                    if not do_gather:
                        for _ in nxt:
                            pass
                        return
                    P.dve(lambda e: e.memset(actv, 0.0), w=["actv"])
                    NG = 64
                    slots = {}
                    for g in range(NG + 1):
                        if g < NG:
                            for j in range(2):
                                hk = 2 * g + j
                                s = ub_n[0] % NU
                                ub_n[0] += 1
                                slots[hk] = s
                                P.dma((lambda s, hk: lambda e: e.indirect_dma_start(
                                    out=ub[s], out_offset=None, in_=puv_d[l], in_offset=bass.IndirectOffsetOnAxis(ap=eidxi[p][:, hk:hk + 1], axis=0)))(s, hk),
                                    r=["eidxi_%d" % p], w=["ub%d" % s], chan="ub%d" % s, eng="pool")
                                P.dve((lambda s, hk: lambda e: e.scalar_tensor_tensor(
                                    out=djunk, in0=ub[s][:, 0:1024], scalar=1.0, in1=h2[p], op0=ALU.mult, op1=ALU.mult, accum_out=actv[:, hk:hk + 1]))(s, hk),
                                    r=["ub%d" % s, "h2_%d" % p, "actv"], w=["djunk", "actv"])
                            P.act((lambda g: lambda e: e.activation(out=ag[:, 2 * g:2 * g + 2], in_=actv[:, 2 * g:2 * g + 2], func=AF.Gelu))(g),
                                  r=["actv"], w=["ag"])
                        if g >= 1:
                            gp = g - 1
                            P.dve((lambda gp: lambda e: e.tensor_tensor(out=wgt[:, 2 * gp:2 * gp + 2], in0=ag[:, 2 * gp:2 * gp + 2],
                                                                        in1=gate[p][:, 2 * gp:2 * gp + 2], op=ALU.mult))(gp),
                                  r=["ag", "gate_%d" % p], w=["wgt"])
                            for j in range(2):
                                hk = 2 * gp + j
                                s = slots[hk]
                                if j == 0:
                                    d = dg_n[0] % 3
                                    dg_n[0] += 1
                                    P.act((lambda d, hk: lambda e: e.activation(out=dg[d], in_=ident[:], func=AF.Copy, scale=wgt[:, hk:hk + 1]))(d, hk),
                                          r=["ident", "wgt"], w=["dg%d" % d])
                                    for b in range(2):
                                        P.pe(MM(ps[6 + b][:, :], dg[d], ub[s][:, 1024 + b * 512:1024 + (b + 1) * 512], gp == 0, gp == NG - 1),
                                             r=["dg%d" % d, "ub%d" % s], w=["ps%d" % (6 + b)])
                                elif hk == 1:
                                    P.dve((lambda s: lambda e: e.tensor_scalar(out=yacc, in0=ub[s][:, 1024:2048], scalar1=wgt[:, 1:2], scalar2=None, op0=ALU.mult))(s),
                                          r=["ub%d" % s, "wgt"], w=["yacc"])
                                else:
                                    P.dve((lambda s, hk: lambda e: e.scalar_tensor_tensor(
                                        out=yacc, in0=ub[s][:, 1024:2048], scalar=wgt[:, hk:hk + 1], in1=yacc, op0=ALU.mult, op1=ALU.add))(s, hk),
                                        r=["ub%d" % s, "wgt", "yacc"], w=["yacc"])
                        next(nxt, None)
                    for _ in nxt:
                        pass
                    if n == 0:
                        tap("wgt%d" % l, wgt, [128, 128], ["wgt"])
                    for b in range(2):
                        P.dve((lambda b: lambda e: e.tensor_tensor(out=yacc[:, b * 512:(b + 1) * 512], in0=ps[6 + b][:, :], in1=yacc[:, b * 512:(b + 1) * 512], op=ALU.add))(b),
                              r=["ps%d" % (6 + b), "yacc"], w=["yacc"])
                    P.dve(lambda e: e.tensor_tensor(out=yacc, in0=yacc, in1=GATE2[:], op=ALU.mult), r=["yacc", "GATE1"], w=["yacc"])
                    P.dve(lambda e: e.tensor_tensor(out=Xn, in0=Xn, in1=yacc, op=ALU.add), r=["yacc", xr], w=[xr])

                if do_peer:
                    for _ in route(0):
                        pass
                    for n in range(NT):
                        nxt = route(n + 1) if n + 1 < NT else iter(())
                        gather(n, nxt)

            prologue()
            mixing()
            peer()

        for l in range(L):
            layer(l)

        for n in range(NT):
            P.dma((lambda n: lambda e: e.dma_start(out=y_d[n * 128:(n + 1) * 128, :], in_=X[:, n, :]))(n), r=["X%d" % n], chan="out")
        fw = ["out"] + (["tap"] if tap_out else [])
        counts = P.emit(final_wait_chans=fw)
    return nc, counts


def host_consts(NT):
    S = NT * 128
    ar = np.arange(128)
    mlo = (ar[None, :] <= ar[:, None]).astype(np.float32)
    mhi = (ar[:, None] <= ar[None, :]).astype(np.float32)
    invcnt = np.zeros((128, 6, 128), np.float32)
    for j in range(2):
        for e in range(3):
            t = ar + (0 if e == 0 else (S - 128 if e == 2 else 256))
            for p in range(128):
                w = (2, 4, 8, 16)[2 * j + (p // 64)]
                if e == 1:
                    cnt = np.full(128, w)
                else:
                    cnt = np.minimum(t + w // 2, S) - np.maximum(t - w // 2, 0)
                invcnt[p, j * 3 + e, :] = 1.0 / cnt.astype(np.float32)
    invf = (500000.0 ** (-np.arange(0, 16, 2, dtype=np.float32) / 16)).astype(np.float32)
    return dict(
        ident=np.eye(128, dtype=np.float32), ones=np.ones((128, 128), np.float32), mlo=mlo, mhi=mhi,
        iota=np.broadcast_to(np.arange(256, dtype=np.float32), (128, 256)).copy(),
        invcnt=invcnt, invfreq=np.broadcast_to(invf, (128, 8)).copy())


def host_weights(L, norm1_g, norm2_g, w_ada, b_ada, w_in, q_norm_g, k_norm_g, attn_sink, conv_w, pool_w,
                 pool_scale, w_out, peer_wq, peer_keys, peer_u, peer_v):
    f = lambda a: np.ascontiguousarray(np.asarray(a, dtype=np.float32))
    d = {}
    d["n1gT"] = f(np.asarray(norm1_g).reshape(L, 8, 128).transpose(0, 2, 1))
    d["n2gT"] = f(np.asarray(norm2_g).reshape(L, 8, 128).transpose(0, 2, 1))
    d["badaT"] = f(np.asarray(b_ada).reshape(L, 48, 128).transpose(0, 2, 1))
    d["wada"] = f(w_ada)
    d["win"] = f(w_in)
    d["wout"] = f(w_out)
    d["wq"] = f(peer_wq)
    gq = np.concatenate([np.tile(np.asarray(q_norm_g), (1, 8)), np.tile(np.asarray(k_norm_g), (1, 2))], axis=1)
    d["gqk"] = f(np.broadcast_to(gq[:, None, :], (L, 128, 640)))
    d["sink"] = f(np.broadcast_to(np.asarray(attn_sink)[:, None, :], (L, 128, 8)))
    cw = np.asarray(conv_w).reshape(L, 3, 2, 128).transpose(0, 3, 2, 1)
    d["convw"] = f(cw.reshape(L, 128, 6))
    pw = np.zeros((L, 2, 128, 128), np.float32)
    pwa = np.asarray(pool_w)
    for j in range(2):
        pw[:, j, 0:64, 0:64] = pwa[:, 2 * j]
        pw[:, j, 64:128, 64:128] = pwa[:, 2 * j + 1]
    d["poolw"] = pw
    d["pscale"] = f(np.asarray(pool_scale).reshape(L, 2, 128).transpose(0, 2, 1))
    d["keysT"] = f(np.asarray(peer_keys).transpose(0, 1, 3, 2))
    pu = np.asarray(peer_u, dtype=np.float32)
    pv = np.asarray(peer_v, dtype=np.float32)
    for l in range(L):
        d["puv%d" % l] = np.ascontiguousarray(np.concatenate([pu[l], pv[l]], axis=1))
    return d


_CACHE = {}


def kernel(x, c, positions, norm1_g, norm2_g, w_ada, b_ada, w_in, q_norm_g, k_norm_g, attn_sink, conv_w,
           pool_w, pool_scale, w_out, peer_wq, peer_keys, peer_u, peer_v):
    x = np.asarray(x, dtype=np.float32)
    c = np.asarray(c, dtype=np.float32)
    positions = np.asarray(positions).astype(np.int32)
    B, S, D = x.shape
    L = np.asarray(w_in).shape[0]
    NT = S // 128
    key = (NT, L)
    if key not in _CACHE:
        _CACHE[key] = build(NT, L)[0]
    nc = _CACHE[key]
    shared = host_weights(L, norm1_g, norm2_g, w_ada, b_ada, w_in, q_norm_g, k_norm_g, attn_sink, conv_w,
                          pool_w, pool_scale, w_out, peer_wq, peer_keys, peer_u, peer_v)
    shared.update(host_consts(NT))
    in_maps = []
    for b in range(B):
        m = dict(shared)
        m["x"] = np.ascontiguousarray(x[b])
        m["cT"] = np.ascontiguousarray(c[b].reshape(8, 128).T)
        m["posT"] = np.ascontiguousarray(positions[b].reshape(NT, 128).T)
        in_maps.append(m)
    res = run_bass_kernel_spmd(nc, in_maps, core_ids=list(range(B)))
    return np.stack([np.asarray(r["y"]) for r in res.results], axis=0).astype(np.float32)
```

```python
import math
from contextlib import ExitStack

import numpy as np
import concourse.bass as bass
import concourse.mybir as mybir
from concourse.bass_utils import run_bass_kernel_spmd

F32 = mybir.dt.float32
U32 = mybir.dt.uint32
I32 = mybir.dt.int32
ALU = mybir.AluOpType
AF = mybir.ActivationFunctionType
AX = mybir.AxisListType

ENGINES = ["pe", "act", "dve", "pool", "sp"]
EPS = 1e-6
PI = math.pi


class Prog:
    def __init__(self, nc):
        self.nc = nc
        self.ops = []
        self.last_w = {}
        self.readers = {}
        self.last_eng = {}
        self.last_chan = {}
        self.pending = {}
        self.burst = set()

    def add(self, eng, fn, r=(), w=(), dma=False, chan=None):
        oid = len(self.ops)
        deps = set()
        for k in r:
            if k in self.last_w:
                deps.add(self.last_w[k])
        for k in w:
            if k in self.last_w:
                deps.add(self.last_w[k])
            for q in self.readers.get(k, ()):
                deps.add(q)
        if eng in self.pending:
            deps |= self.pending.pop(eng)
        for k in r:
            self.readers.setdefault(k, []).append(oid)
        for k in w:
            self.last_w[k] = oid
            self.readers[k] = []
        deps.discard(oid)
        self.ops.append(dict(eng=eng, fn=fn, deps=sorted(deps), dma=dma, chan=chan))
        if dma:
            self.last_chan[chan] = oid
        else:
            self.last_eng[eng] = oid
        return oid

    def barrier(self):
        b = set(self.last_eng.values()) | set(self.last_chan.values())
        for e in ENGINES:
            self.pending[e] = set(b) | self.pending.get(e, set())

    def pe(self, fn, r=(), w=()):
        return self.add("pe", fn, r, w)

    def act(self, fn, r=(), w=()):
        return self.add("act", fn, r, w)

    def dve(self, fn, r=(), w=()):
        return self.add("dve", fn, r, w)

    def pool(self, fn, r=(), w=()):
        return self.add("pool", fn, r, w)

    def dma(self, fn, r=(), w=(), chan=None, eng="sp"):
        return self.add(eng, fn, r, w, dma=True, chan=chan)

    def emit(self, final_wait_chans=()):
        nc = self.nc
        ops = self.ops
        eng_count = {e: 0 for e in ENGINES}
        chan_count = {}
        for op in ops:
            if op["dma"]:
                c = op["chan"]
                chan_count[c] = chan_count.get(c, 0) + 16
                op["sig"] = ("c", c, chan_count[c])
                op["inc"] = True
            else:
                eng_count[op["eng"]] += 1
                op["sig"] = ("e", op["eng"], eng_count[op["eng"]])
        for op in ops:
            if op["dma"] and op["chan"] in self.burst:
                op["sig"] = ("c", op["chan"], chan_count[op["chan"]])
        with ExitStack() as st:
            sems = {}
            for e in ENGINES:
                sems[("e", e)] = st.enter_context(nc.semaphore("s_" + e))
            for c in chan_count:
                sems[("c", c)] = st.enter_context(nc.semaphore("c_" + str(c)))
            block = st.enter_context(nc.Block())

            def make(engname):
                def body(eng):
                    waited = {}
                    for op in ops:
                        if op["eng"] != engname:
                            continue
                        for d in op["deps"]:
                            kind, key, val = ops[d]["sig"]
                            if kind == "e" and key == "pe" and engname == "pe":
                                continue
                            sk = (kind, key)
                            if waited.get(sk, 0) >= val:
                                continue
                            eng.wait_ge(sems[sk], val)
                            waited[sk] = val
                        ins = op["fn"](eng)
                        kind, key, val = op["sig"]
                        ins.then_inc(sems[(kind, key)], 16 if kind == "c" else 1)
                    if engname == "sp":
                        for c in final_wait_chans:
                            eng.wait_ge(sems[("c", c)], chan_count[c])
                return body

            block.tensor(make("pe"))
            block.scalar(make("act"))
            block.vector(make("dve"))
            block.gpsimd(make("pool"))
            block.sync(make("sp"))
        return eng_count, chan_count


def build(NT, L, taps=(), do_peer=True, do_gather=True, stage='all', cut=99, PE_EVERY=2):
    S = NT * 128
    nc = bass.Bass("TRN2", target_bir_lowering=False)
    P = Prog(nc)
    P.burst = {"cst"} | {"lw%d" % l for l in range(L)}

    def din(name, shape, dt=F32):
        return nc.dram_tensor(name, shape, dt, kind="ExternalInput").ap()

    x_d = din("x", [S, 1024])
    cT_d = din("cT", [128, 8])
    pos_d = din("posT", [128, NT], I32)
    n1g_d = din("n1gT", [L, 128, 8])
    n2g_d = din("n2gT", [L, 128, 8])
    bada_d = din("badaT", [L, 128, 48])
    wada_d = din("wada", [L, 1024, 6144])
    win_d = din("win", [L, 1024, 1792])
    wout_d = din("wout", [L, 1024, 1024])
    wq_d = din("wq", [L, 1024, 2048])
    gqk_d = din("gqk", [L, 128, 640])
    sink_d = din("sink", [L, 128, 8])
    convw_d = din("convw", [L, 128, 6])
    poolw_d = din("poolw", [L, 2, 128, 128])
    pscale_d = din("pscale", [L, 128, 2])
    keysT_d = din("keysT", [L, 2, 128, 128])
    pu_d = [din("pu%d" % l, [16384, 1024]) for l in range(L)]
    pv_d = [din("pv%d" % l, [16384, 1024]) for l in range(L)]
    ident_d = din("ident", [128, 128])
    ones_d = din("ones", [128, 128])
    mlo_d = din("mlo", [128, 128])
    mhi_d = din("mhi", [128, 128])
    iota_d = din("iota", [128, 256])
    invcnt_d = din("invcnt", [128, 6, 128])
    invfreq_d = din("invfreq", [128, 8])
    y_d = nc.dram_tensor("y", [S, 1024], F32, kind="ExternalOutput").ap()
    tap_out = {}

    with ExitStack() as st:
        def sb(name, shape, dt=F32):
            return st.enter_context(nc.sbuf_tensor(name, shape, dt))

        X = sb("X", [128, NT, 1024])
        ident = sb("ident_s", [128, 128])
        ones = sb("ones_s", [128, 128])
        mlo = sb("mlo_s", [128, 128])
        mhi = sb("mhi_s", [128, 128])
        iota = sb("iota_s", [128, 256])
        invcnt = sb("invcnt_s", [128, 6, 128])
        invfreq = sb("invfreq_s", [128, 8])
        cT = sb("cT_s", [128, 8])
        cact = sb("cact", [128, 8])
        posi = sb("posi", [128, NT], I32)
        posf = sb("posf", [128, NT])
        ang = sb("ang", [128, NT, 8])
        ang2 = sb("ang2", [128, NT, 8])
        angi = sb("angi", [128, NT, 8], I32)
        cosT = sb("cosT", [128, NT, 8])
        sinT = sb("sinT", [128, NT, 8])
        n1g = sb("n1g", [128, 8])
        n2g = sb("n2g", [128, 8])
        bada = sb("bada", [128, 48])
        modT = sb("modT", [128, 48])
        der = sb("der", [128, 32])
        GATE1 = sb("GATE1", [128, 1024])
        GATE2 = sb("GATE2", [128, 1024])
        gqk = sb("gqk_s", [128, 640])
        sinkt = sb("sinkt", [128, 8])
        esink = sb("esink", [128, 8])
        convw = sb("convw_s", [128, 6])
        poolw = sb("poolw_s", [128, 2, 128])
        pscale = sb("pscale_s", [128, 2])
        keysT = sb("keysT_s", [128, 2, 128])
        st8 = sb("st8", [128, 64])
        AW = 29400
        arena = sb("arena", [128, AW])
        ps = [st.enter_context(nc.psum_tensor("ps%d" % i, [128, 512], F32)) for i in range(8)]

        class Carver:
            def __init__(self):
                self.off = 0

            def get(self, words):
                a = arena[:, self.off:self.off + words]
                self.off += words
                assert self.off <= AW, self.off
                return a

        def tap(name, ap, shape, r, dt=F32):
            if name not in taps:
                return
            t = nc.dram_tensor("tap_" + name, shape, dt, kind="ExternalOutput").ap()
            tap_out[name] = t
            P.dma(lambda e: e.dma_start(out=t, in_=ap), r=r, chan="tap")

        def ld(dst, src, name, chan="cst"):
            P.dma(lambda e: e.dma_start(out=dst, in_=src), w=[name], chan=chan)

        ld(ident[:], ident_d, "ident")
        ld(ones[:], ones_d, "ones")
        ld(mlo[:], mlo_d, "mlo")
        ld(mhi[:], mhi_d, "mhi")
        ld(iota[:], iota_d, "iota")
        ld(invcnt[:], invcnt_d, "invcnt")
        ld(invfreq[:], invfreq_d, "invfreq")
        ld(cT[:], cT_d, "cT")
        ld(posi[:], pos_d, "posi")
        for n in range(NT):
            ld(X[:, n, :], x_d[n * 128:(n + 1) * 128, :], "X%d" % n, chan="xin%d" % n)

        P.act(lambda e: e.activation(out=cact[:], in_=cT[:], func=AF.Silu), r=["cT"], w=["cact"])
        P.dve(lambda e: e.tensor_copy(out=posf[:], in_=posi[:]), r=["posi"], w=["posf"])
        P.dve(lambda e: e.tensor_tensor(out=ang[:], in0=posf[:].unsqueeze(2).broadcast_to([128, NT, 8]),
                                        in1=invfreq[:].unsqueeze(1).broadcast_to([128, NT, 8]), op=ALU.mult),
              r=["posf", "invfreq"], w=["ang"])
        C1 = 6.28125
        C2 = 2 * PI - C1
        P.dve(lambda e: e.tensor_scalar(out=ang2[:], in0=ang[:], scalar1=1.0 / (2 * PI), scalar2=None, op0=ALU.mult), r=["ang"], w=["ang2"])
        P.dve(lambda e: e.tensor_copy(out=angi[:], in_=ang2[:]), r=["ang2"], w=["angi"])
        P.dve(lambda e: e.tensor_copy(out=ang2[:], in_=angi[:]), r=["angi"], w=["ang2"])
        P.dve(lambda e: e.scalar_tensor_tensor(out=ang[:], in0=ang2[:], scalar=-C1, in1=ang[:], op0=ALU.mult, op1=ALU.add), r=["ang2", "ang"], w=["ang"])
        P.dve(lambda e: e.scalar_tensor_tensor(out=ang[:], in0=ang2[:], scalar=-C2, in1=ang[:], op0=ALU.mult, op1=ALU.add), r=["ang2", "ang"], w=["ang"])

        def wrap():
            P.dve(lambda e: e.tensor_scalar(out=ang2[:], in0=ang[:], scalar1=PI, scalar2=-2 * PI, op0=ALU.is_gt, op1=ALU.mult), r=["ang", "sinT", "cosT"], w=["ang2"])
            P.dve(lambda e: e.tensor_tensor(out=ang[:], in0=ang[:], in1=ang2[:], op=ALU.add), r=["ang", "ang2"], w=["ang"])
            P.dve(lambda e: e.tensor_scalar(out=ang[:], in0=ang[:], scalar1=-PI, scalar2=PI, op0=ALU.max, op1=ALU.min), r=["ang"], w=["ang"])

        wrap()
        P.act(lambda e: e.activation(out=sinT[:], in_=ang[:], func=AF.Sin), r=["ang"], w=["sinT"])
        P.dve(lambda e: e.tensor_scalar(out=ang[:], in0=ang[:], scalar1=0.5 * PI, scalar2=None, op0=ALU.add), r=["ang", "sinT"], w=["ang"])
        wrap()
        P.act(lambda e: e.activation(out=cosT[:], in_=ang[:], func=AF.Sin), r=["ang"], w=["cosT"])
        tap("cos", cosT[:], [128, NT, 8], ["cosT"])
        tap("sin", sinT[:], [128, NT, 8], ["sinT"])

        def rstd_from_ss(ss_ap, out_ap, n, rname, wname):
            P.dve(lambda e: e.tensor_scalar(out=out_ap, in0=ss_ap, scalar1=1.0 / n, scalar2=EPS, op0=ALU.mult, op1=ALU.add),
                  r=[rname], w=[wname])
            P.act(lambda e: e.activation(out=out_ap, in_=out_ap, func=AF.Sqrt), r=[wname], w=[wname])
            P.dve(lambda e: e.reciprocal(out=out_ap, in_=out_ap), r=[wname], w=[wname])

        def MM(out, lhsT, rhs, start, stop):
            return lambda e: e.matmul(out, lhsT=lhsT, rhs=rhs, start=start, stop=stop, skip_group_check=True)

        def TR(out, in_, idn):
            return lambda e: e.transpose(out=out, in_=in_, identity=idn)

        def layer(l):
            G1s, SH1 = der[:, 0:8], modT[:, 0:8]
            G2s, SH2 = der[:, 8:16], modT[:, 24:32]
            def prologue():
                P.barrier()
                cv = Carver()
                wad = [cv.get(3072), cv.get(3072)]
                Dt = cv.get(128)
                ld(n1g[:], n1g_d[l], "n1g", chan="lw%d" % l)
                ld(n2g[:], n2g_d[l], "n2g", chan="lw%d" % l)
                ld(bada[:], bada_d[l], "bada", chan="lw%d" % l)
                ld(gqk[:], gqk_d[l], "gqk", chan="lw%d" % l)
                ld(sinkt[:], sink_d[l], "sinkt", chan="lw%d" % l)
                ld(convw[:], convw_d[l], "convw", chan="lw%d" % l)
                for j in range(2):
                    ld(poolw[:, j, :], poolw_d[l, j], "poolw%d" % j, chan="lw%d" % l)
                    ld(keysT[:, j, :], keysT_d[l, j], "keysT%d" % j, chan="lw%d" % l)
                ld(pscale[:], pscale_d[l], "pscale", chan="lw%d" % l)
                first = True
                pi = 0
                for kc in range(8):
                    for half in range(2):
                        s = pi % 2
                        pi += 1
                        P.dma((lambda s, kc, half: lambda e: e.dma_start(
                            out=wad[s], in_=wada_d[l, kc * 128:(kc + 1) * 128, half * 3072:(half + 1) * 3072]))(s, kc, half),
                            w=["wad%d" % s], chan="wad%d" % s)
                        for jj in range(24):
                            j = half * 24 + jj
                            P.pe(MM(ps[4][:, j:j + 1], wad[s][:, jj * 128:(jj + 1) * 128], cact[:, kc:kc + 1], first, kc == 7),
                                 r=["wad%d" % s, "cact"], w=["ps4"])
                            first = False
                P.dve(lambda e: e.tensor_tensor(out=modT[:], in0=ps[4][:, 0:48], in1=bada[:], op=ALU.add),
                      r=["ps4", "bada"], w=["modT"])
                tap("modT%d" % l, modT[:], [128, 48], ["modT"])
                P.dve(lambda e: e.scalar_tensor_tensor(out=der[:, 0:8], in0=modT[:, 8:16], scalar=1.0, in1=n1g[:], op0=ALU.add, op1=ALU.mult),
                      r=["modT", "n1g"], w=["der"])
                P.dve(lambda e: e.scalar_tensor_tensor(out=der[:, 8:16], in0=modT[:, 32:40], scalar=1.0, in1=n2g[:], op0=ALU.add, op1=ALU.mult),
                      r=["modT", "n2g", "der"], w=["der"])
                for gi, (GT, c0) in enumerate(((GATE1, 16), (GATE2, 40))):
                    for ch in range(8):
                        P.dve((lambda ch, c0: lambda e: e.tensor_scalar(out=Dt, in0=ident[:], scalar1=modT[:, c0 + ch:c0 + ch + 1], scalar2=None, op0=ALU.mult))(ch, c0),
                              r=["ident", "modT"], w=["Dt"])
                        P.pe(MM(ps[ch // 4][:, (ch % 4) * 128:(ch % 4 + 1) * 128], ones[:], Dt, True, True),
                             r=["ones", "Dt"], w=["ps%d" % (ch // 4)])
                    for b in range(2):
                        P.act((lambda b, GT: lambda e: e.copy(out=GT[:, b * 512:(b + 1) * 512], in_=ps[b][:]))(b, GT),
                              r=["ps%d" % b], w=["GATE%d" % gi])
                tap("gate1_%d" % l, GATE1[:], [128, 1024], ["GATE0"])
                P.act(lambda e: e.activation(out=esink[:], in_=sinkt[:], func=AF.Exp), r=["sinkt"], w=["esink"])

            def mixing():
                P.barrier()
                cv = Carver()
                wi = [cv.get(1792) for _ in range(2)]
                wo = [cv.get(1024) for _ in range(2)]
                xn = cv.get(1024)
                hT = cv.get(1024).rearrange("p (c t) -> p c t", t=128)
                zsb = cv.get(1792)
                sq = cv.get(640)
                qn = cv.get(640)
                rt = [cv.get(80).rearrange("p (h d) -> p h d", d=8) for _ in range(6)]
                qT = [cv.get(1024).rearrange("p (h t) -> p h t", t=128) for _ in range(2)]
                kT = [cv.get(256).rearrange("p (g t) -> p g t", t=128) for _ in range(4)]
                vR = [cv.get(128).rearrange("p (g d) -> p g d", d=64) for _ in range(4)]
                Wb = [cv.get(1152).rearrange("p (c t) -> p c t", t=144) for _ in range(2)]
                tail = [cv.get(64).rearrange("p (c t) -> p c t", t=8) for _ in range(2)]
                cx = cv.get(144)
                cacc = cv.get(128)
                Alv = [cv.get(144) for _ in range(4)]
                ptmp = cv.get(128)
                pooled = cv.get(128)
                cpT = [cv.get(512).rearrange("p (c t) -> p c t", t=128) for _ in range(2)]
                PT = [cv.get(512) for _ in range(2)]
                den = cv.get(512)
                attnT = cv.get(1024).rearrange("p (h t) -> p h t", t=128)
                otmp = cv.get(1024)
                junk = otmp
                ss = st8[:, 0:1]
                rstd = st8[:, 1:2]
                ssq = st8[:, 8:18]
                rsq = st8[:, 24:34]
                wi_n = [0]
                wo_n = [0]

                def stepA(n):
                    Xn = X[:, n, :]
                    xr = "X%d" % n
                    P.dve(lambda e: e.memset(ss, 0.0), w=["ss"])
                    P.act(lambda e: e.activation(out=junk, in_=Xn, func=AF.Square, accum_out=ss), r=[xr, "ss"], w=["junk", "ss"])
                    rstd_from_ss(ss, rstd, 1024, "ss", "rstd")
                    P.dve(lambda e: e.tensor_scalar(out=xn, in0=Xn, scalar1=rstd, scalar2=None, op0=ALU.mult), r=[xr, "rstd"], w=["xn"])
                    if n == 0:
                        tap("xn%d" % l, xn, [128, 1024], ["xn"])
                        tap("st8_%d" % l, st8[:], [128, 64], ["rstd", "ss"])
                    for half in range(2):
                        for c4 in range(4):
                            kc = half * 4 + c4
                            P.pe(TR(ps[4][:, c4 * 128:(c4 + 1) * 128], xn[:, kc * 128:(kc + 1) * 128], ident[:]),
                                 r=["xn", "ident"], w=["ps4"])
                        for c4 in range(4):
                            kc = half * 4 + c4
                            P.dve((lambda kc, c4: lambda e: e.tensor_scalar(
                                out=hT[:, kc, :], in0=ps[4][:, c4 * 128:(c4 + 1) * 128], scalar1=G1s[:, kc:kc + 1], scalar2=SH1[:, kc:kc + 1],
                                op0=ALU.mult, op1=ALU.add))(kc, c4), r=["ps4", "der", "modT"], w=["hT"])
                    if n == 0:
                        tap("hT%d" % l, hT, [128, 8, 128], ["hT"])
                    if cut <= 1:
                        return
                    widths = [512, 512, 512, 256]
                    for kc in range(8):
                        s = wi_n[0] % 2
                        wi_n[0] += 1
                        P.dma((lambda s, kc: lambda e: e.dma_start(out=wi[s], in_=win_d[l, kc * 128:(kc + 1) * 128, :]))(s, kc),
                              w=["wi%d" % s], chan="wi%d" % s)
                        for b in range(4):
                            P.pe(MM(ps[b][:, 0:widths[b]], hT[:, kc, :], wi[s][:, b * 512:b * 512 + widths[b]], kc == 0, kc == 7),
                                 r=["hT", "wi%d" % s], w=["ps%d" % b])
                    for b in range(4):
                        if b % 2 == 0:
                            P.act((lambda b: lambda e: e.copy(out=zsb[:, b * 512:b * 512 + widths[b]], in_=ps[b][:, 0:widths[b]]))(b),
                                  r=["ps%d" % b], w=["zsb%d" % b])
                        else:
                            P.dve((lambda b: lambda e: e.tensor_copy(out=zsb[:, b * 512:b * 512 + widths[b]], in_=ps[b][:, 0:widths[b]]))(b),
                                  r=["ps%d" % b], w=["zsb%d" % b])
                    zall = ["zsb0", "zsb1", "zsb2", "zsb3"]
                    if cut <= 2:
                        return
                    if n == 0:
                        tap("z%d" % l, zsb, [128, 1792], zall)
                    zqk = zsb[:, 0:640]
                    P.dve(lambda e: e.tensor_tensor(out=sq, in0=zqk, in1=zqk, op=ALU.mult), r=zall, w=["sq"])
                    P.dve(lambda e: e.tensor_reduce(out=ssq, in_=sq.rearrange("p (h d) -> p h d", d=64), axis=AX.X, op=ALU.add),
                          r=["sq"], w=["ssq"])
                    rstd_from_ss(ssq, rsq, 64, "ssq", "rsq")
                    qn3 = qn.rearrange("p (h d) -> p h d", d=64)
                    P.dve(lambda e: e.tensor_tensor(out=qn3, in0=zqk.rearrange("p (h d) -> p h d", d=64),
                                                    in1=rsq.unsqueeze(2).broadcast_to([128, 10, 64]), op=ALU.mult),
                          r=zall + ["rsq"], w=["qn"])
                    P.dve(lambda e: e.tensor_tensor(out=qn, in0=qn, in1=gqk[:], op=ALU.mult), r=["qn", "gqk"], w=["qn"])
                    cosb = cosT[:, n, :].unsqueeze(1).broadcast_to([128, 10, 8])
                    if cut <= 3:
                        return
                    sinb = sinT[:, n, :].unsqueeze(1).broadcast_to([128, 10, 8])
                    t1 = qn3[:, :, 0:8]
                    t2 = qn3[:, :, 8:16]
                    P.dve(lambda e: e.tensor_tensor(out=rt[0], in0=t1, in1=cosb, op=ALU.mult), r=["qn", "cosT"], w=["rt0"])
                    P.dve(lambda e: e.tensor_tensor(out=rt[1], in0=t2, in1=sinb, op=ALU.mult), r=["qn", "sinT"], w=["rt1"])
                    P.dve(lambda e: e.tensor_tensor(out=rt[2], in0=t2, in1=cosb, op=ALU.mult), r=["qn", "cosT"], w=["rt2"])
                    P.dve(lambda e: e.tensor_tensor(out=rt[3], in0=t1, in1=sinb, op=ALU.mult), r=["qn", "sinT"], w=["rt3"])
                    P.dve(lambda e: e.tensor_tensor(out=t1, in0=rt[0], in1=rt[1], op=ALU.subtract), r=["rt0", "rt1", "rt2", "rt3", "qn"], w=["qn"])
                    P.dve(lambda e: e.tensor_tensor(out=t2, in0=rt[2], in1=rt[3], op=ALU.add), r=["rt2", "rt3", "qn"], w=["qn"])
                    if n == 0:
                        tap("qn%d" % l, qn, [128, 640], ["qn"])
                    if cut <= 4:
                        return
                    qs = n % 2
                    ks = n % 4
                    for grp in range(2):
                        for h4 in range(4):
                            h = grp * 4 + h4
                            P.pe(TR(ps[4][0:64, h4 * 128:(h4 + 1) * 128], qn3[:, h, :], ident[:]), r=["qn", "ident"], w=["ps4"])
                        P.act((lambda grp: lambda e: e.activation(
                            out=qT[qs][0:64, grp * 4:(grp + 1) * 4, :], in_=ps[4][0:64, :].rearrange("p (h t) -> p h t", t=128),
                            func=AF.Copy, scale=0.125))(grp), r=["ps4"], w=["qT%d" % qs])
                    for g in range(2):
                        P.pe(TR(ps[4][0:64, g * 128:(g + 1) * 128], qn3[:, 8 + g, :], ident[:]), r=["qn", "ident"], w=["ps4"])
                    P.dve(lambda e: e.tensor_copy(out=kT[ks][0:64, :, :], in_=ps[4][0:64, 0:256].rearrange("p (g t) -> p g t", t=128)),
                          r=["ps4"], w=["kT%d" % ks])
                    P.act(lambda e: e.copy(out=vR[ks], in_=zsb[:, 640:768].rearrange("p (g d) -> p g d", d=64)), r=zall, w=["vR%d" % ks])
                    if cut <= 5:
                        return
                    ws = n % 2
                    for half in range(2):
                        for c4 in range(4):
                            c = half * 4 + c4
                            P.pe(TR(ps[4][:, c4 * 128:(c4 + 1) * 128], zsb[:, 768 + c * 128:768 + (c + 1) * 128], ident[:]),
                                 r=zall + ["ident"], w=["ps4"])
                        pv4 = ps[4][:, :].rearrange("p (c t) -> p c t", t=128)
                        cs = slice(half * 4, half * 4 + 4)
                        P.act((lambda cs, pv4: lambda e: e.copy(out=Wb[ws][:, cs, 8:136], in_=pv4))(cs, pv4), r=["ps4"], w=["Wb%dm" % ws])
                    if cut <= 6:
                        return
                    if n == 0:
                        P.dve(lambda e: e.memset(Wb[ws][:, :, 0:8], 0.0), w=["Wb%dl" % ws])
                    else:
                        P.dve(lambda e: e.tensor_copy(out=Wb[1 - ws][:, :, 136:144], in_=Wb[ws][:, :, 8:16]), r=["Wb%dm" % ws], w=["Wb%dr" % (1 - ws)])
                        P.dve(lambda e: e.tensor_copy(out=Wb[ws][:, :, 0:8], in_=Wb[1 - ws][:, :, 128:136]), r=["Wb%dm" % (1 - ws)], w=["Wb%dl" % ws])
                    if n == NT - 1:
                        P.dve(lambda e: e.memset(Wb[ws][:, :, 136:144], 0.0), w=["Wb%dr" % ws])

                def stepC(m):
                    ws = m % 2
                    W = Wb[ws]
                    wr = ["Wb%dm" % ws, "Wb%dl" % ws, "Wb%dr" % ws]
                    cp = cpT[ws]
                    edge = 0 if m == 0 else (2 if m == NT - 1 else 1)
                    for j in range(2):
                        P.dve((lambda j: lambda e: e.tensor_tensor(out=cx, in0=W[:, 4 + j, :], in1=W[:, j, :], op=ALU.mult))(j), r=wr, w=["cx"])
                        P.dve((lambda j: lambda e: e.tensor_scalar(out=cacc, in0=cx[:, 8:136], scalar1=convw[:, j * 3 + 1:j * 3 + 2], scalar2=None, op0=ALU.mult))(j),
                              r=["cx", "convw"], w=["cacc"])
                        P.dve((lambda j: lambda e: e.scalar_tensor_tensor(out=cacc, in0=cx[:, 7:135], scalar=convw[:, j * 3:j * 3 + 1], in1=cacc, op0=ALU.mult, op1=ALU.add))(j),
                              r=["cx", "convw", "cacc"], w=["cacc"])
                        P.dve((lambda j: lambda e: e.scalar_tensor_tensor(out=cacc, in0=cx[:, 9:137], scalar=convw[:, j * 3 + 2:j * 3 + 3], in1=cacc, op0=ALU.mult, op1=ALU.add))(j),
                              r=["cx", "convw", "cacc"], w=["cacc"])
                        P.dve((lambda j: lambda e: e.tensor_tensor(out=cp[:, j, :], in0=cacc, in1=W[:, 2 + j, 8:136], op=ALU.mult))(j),
                              r=wr + ["cacc"], w=["cpT%d" % ws])
                    for j in range(2):
                        u = W[:, 6 + j, :]
                        A1, A4, A8, A16 = Alv
                        P.dve((lambda u: lambda e: e.tensor_tensor(out=A1[:, 1:144], in0=u[:, 0:143], in1=u[:, 1:144], op=ALU.add))(u), r=wr, w=["A1"])
                        P.dve(lambda e: e.tensor_tensor(out=A4[:, 2:143], in0=A1[:, 1:142], in1=A1[:, 3:144], op=ALU.add), r=["A1"], w=["A4"])
                        if j == 0:
                            lo, hi = A1, A4
                        else:
                            P.dve(lambda e: e.tensor_tensor(out=A8[:, 4:141], in0=A4[:, 2:139], in1=A4[:, 6:143], op=ALU.add), r=["A4"], w=["A8"])
                            P.dve(lambda e: e.tensor_tensor(out=A16[:, 8:137], in0=A8[:, 4:133], in1=A8[:, 12:141], op=ALU.add), r=["A8"], w=["A16"])
                            lo, hi = A8, A16
                        for (p0, p1, A) in ((0, 64, lo), (64, 128, hi)):
                            P.dve((lambda p0, p1, A, j: lambda e: e.tensor_tensor(out=ptmp[p0:p1, :], in0=A[p0:p1, 8:136], in1=invcnt[p0:p1, j * 3 + edge, :], op=ALU.mult))(p0, p1, A, j),
                                  r=["A1", "A4", "A8", "A16", "invcnt"], w=["ptmp"])
                        P.dve((lambda u: lambda e: e.tensor_tensor(out=pooled, in0=ptmp, in1=u[:, 8:136], op=ALU.subtract))(u), r=["ptmp"] + wr, w=["pooled"])
                        P.pe(MM(ps[5][:, 0:128], poolw[:, j, :], pooled, True, True), r=["pooled", "poolw%d" % j], w=["ps5"])
                        P.act((lambda j: lambda e: e.activation(out=cp[:, 2 + j, :], in_=ps[5][:, 0:128], func=AF.Copy, scale=pscale[:, j:j + 1]))(j),
                              r=["ps5", "pscale"], w=["cpT%d" % ws])
                    if m == 0:
                        tap("cp%d" % l, cp, [128, 4, 128], ["cpT%d" % ws])

                def stepD(m):
                    qs = m % 2
                    blocks = [j for j in (m - 1, m, m + 1) if 0 <= j < NT]
                    pslot = [0]
                    for g in range(2):
                        for bi, j in enumerate(blocks):
                            ks = j % 4
                            P.pe(MM(ps[5][:, :], kT[ks][0:64, g, :], qT[qs][0:64, g * 4:(g + 1) * 4, :], True, True),
                                 r=["kT%d" % ks, "qT%d" % qs], w=["ps5"])
                            s = pslot[0] % 2
                            pslot[0] += 1
                            P.act((lambda s: lambda e: e.activation(out=PT[s], in_=ps[5][:, :], func=AF.Exp))(s), r=["ps5"], w=["PT%d" % s])
                            if j != m:
                                mk = mlo if j < m else mhi
                                P.dve((lambda s, mk: lambda e: e.tensor_tensor(
                                    out=PT[s].rearrange("p (h t) -> p h t", t=128), in0=PT[s].rearrange("p (h t) -> p h t", t=128),
                                    in1=mk[:].unsqueeze(1).broadcast_to([128, 4, 128]), op=ALU.mult))(s, mk),
                                    r=["PT%d" % s, "mlo", "mhi"], w=["PT%d" % s])
                            P.pe(MM(ps[6][0:64, :], vR[ks][:, g, :], PT[s], bi == 0, bi == len(blocks) - 1), r=["vR%d" % ks, "PT%d" % s], w=["ps6"])
                            P.pe(MM(ps[7][0:64, :], ones[:, 0:64], PT[s], bi == 0, bi == len(blocks) - 1), r=["ones", "PT%d" % s], w=["ps7"])
                        P.dve((lambda g: lambda e: e.tensor_tensor(
                            out=den[0:64, :].rearrange("p (h t) -> p h t", t=128), in0=ps[7][0:64, :].rearrange("p (h t) -> p h t", t=128),
                            in1=esink[0:64, g * 4:(g + 1) * 4].unsqueeze(2).broadcast_to([64, 4, 128]), op=ALU.add))(g),
                            r=["ps7", "esink"], w=["den"])
                        P.dve(lambda e: e.reciprocal(out=den[0:64, :], in_=den[0:64, :]), r=["den"], w=["den"])
                        P.dve((lambda g: lambda e: e.tensor_tensor(
                            out=attnT[0:64, g * 4:(g + 1) * 4, :], in0=ps[6][0:64, :].rearrange("p (h t) -> p h t", t=128),
                            in1=den[0:64, :].rearrange("p (h t) -> p h t", t=128), op=ALU.mult))(g),
                            r=["ps6", "den"], w=["attnT"])
                    if m == 0:
                        tap("attnT%d" % l, attnT[0:64, :, :], [64, 8, 128], ["attnT"])
                    cp = cpT[m % 2]
                    npieces = 12
                    for pc in range(npieces):
                        s = wo_n[0] % 2
                        wo_n[0] += 1
                        if pc < 8:
                            P.dma((lambda s, pc: lambda e: e.dma_start(out=wo[s][0:64, :], in_=wout_d[l, pc * 64:(pc + 1) * 64, :]))(s, pc),
                                  w=["wo%d" % s], chan="wo%d" % s)
                            lhs = attnT[0:64, pc, :]
                            rw = wo[s][0:64, :]
                            rr = ["attnT"]
                        else:
                            c = pc - 8
                            P.dma((lambda s, c: lambda e: e.dma_start(out=wo[s], in_=wout_d[l, 512 + c * 128:512 + (c + 1) * 128, :]))(s, c),
                                  w=["wo%d" % s], chan="wo%d" % s)
                            lhs = cp[:, c, :]
                            rw = wo[s]
                            rr = ["cpT%d" % (m % 2)]
                        for b in range(2):
                            P.pe(MM(ps[b][:, :], lhs, rw[:, b * 512:(b + 1) * 512], pc == 0, pc == npieces - 1), r=rr + ["wo%d" % s], w=["ps%d" % b])
                    for b in range(2):
                        P.dve((lambda b: lambda e: e.tensor_tensor(out=otmp[:, b * 512:(b + 1) * 512], in0=ps[b][:, :], in1=GATE1[:, b * 512:(b + 1) * 512], op=ALU.mult))(b),
                              r=["ps%d" % b, "GATE0"], w=["junk"])
                    P.dve(lambda e: e.tensor_tensor(out=X[:, m, :], in0=X[:, m, :], in1=otmp, op=ALU.add), r=["junk", "X%d" % m], w=["X%d" % m])

                for n in range(NT):
                    if stage in ('A', 'AC', 'all'):
                        stepA(n)
                    if n >= 1:
                        if stage in ('AC', 'all'):
                            stepC(n - 1)
                        if stage == 'all':
                            stepD(n - 1)
                if stage in ('AC', 'all'):
                    stepC(NT - 1)
                if stage == 'all':
                    stepD(NT - 1)
                tap("xmid%d" % l, X[:, 0, :], [128, 1024], ["X0"])

            def peer():
                P.barrier()
                cv = Carver()
                wqr = [cv.get(1024) for _ in range(2)]
                xn = cv.get(1024)
                h2T = cv.get(1024).rearrange("p (c t) -> p c t", t=128)
                h2 = [cv.get(1024) for _ in range(2)]
                pqT = cv.get(2048).rearrange("p (c t) -> p c t", t=128)
                sc = cv.get(256)
                scr = cv.get(256)
                vv = cv.get(32)
                ii = cv.get(32).bitcast(U32)
                iif = cv.get(32)
                cand = cv.get(256)
                cand2 = cv.get(256)
                cidx = cv.get(256)
                top = cv.get(16)
                pos = cv.get(16).bitcast(U32)
                posff = cv.get(16)
                junk2 = cv.get(256)
                ex = cv.get(16)
                gate = [cv.get(128) for _ in range(2)]
                eidxf = cv.get(128)
                eidxi = [cv.get(128).bitcast(I32) for _ in range(2)]
                actv = cv.get(128)
                ag = cv.get(128)
                wgt = cv.get(128)
                NU = 12
                ub = [cv.get(1024) for _ in range(NU)]
                dg = [cv.get(128) for _ in range(3)]
                dg_n = [0]
                yacc = cv.get(1024)
                djunk = cv.get(1024)
                ss = st8[:, 0:1]
                rstd = st8[:, 1:2]
                negm = st8[:, 2:3]
                gs = st8[:, 3:4]
                wq_n = [0]
                ub_n = [0]

                def route(n):
                    p = n % 2
                    Xn = X[:, n, :]
                    xr = "X%d" % n
                    P.dve(lambda e: e.memset(ss, 0.0), w=["ss"])
                    P.act(lambda e: e.activation(out=xn, in_=Xn, func=AF.Square, accum_out=ss), r=[xr, "ss"], w=["xn", "ss"])
                    rstd_from_ss(ss, rstd, 1024, "ss", "rstd")
                    P.dve(lambda e: e.tensor_scalar(out=xn, in0=Xn, scalar1=rstd, scalar2=None, op0=ALU.mult), r=[xr, "rstd"], w=["xn"])
                    yield
                    for half in range(2):
                        for c4 in range(4):
                            kc = half * 4 + c4
                            P.pe(TR(ps[4][:, c4 * 128:(c4 + 1) * 128], xn[:, kc * 128:(kc + 1) * 128], ident[:]), r=["xn", "ident"], w=["ps4"])
                        for c4 in range(4):
                            kc = half * 4 + c4
                            P.dve((lambda kc, c4: lambda e: e.tensor_scalar(
                                out=h2T[:, kc, :], in0=ps[4][:, c4 * 128:(c4 + 1) * 128], scalar1=G2s[:, kc:kc + 1], scalar2=SH2[:, kc:kc + 1],
                                op0=ALU.mult, op1=ALU.add))(kc, c4), r=["ps4", "der", "modT"], w=["h2T"])
                        yield
                    for half in range(2):
                        for c4 in range(4):
                            kc = half * 4 + c4
                            P.pe(TR(ps[4][:, c4 * 128:(c4 + 1) * 128], h2T[:, kc, :], ident[:]), r=["h2T", "ident"], w=["ps4"])
                        P.act((lambda half: lambda e: e.copy(out=h2[p][:, half * 512:(half + 1) * 512], in_=ps[4][:, :]))(half), r=["ps4"], w=["h2_%d" % p])
                        yield
                    if n == 0:
                        tap("h2_%d" % l, h2[p], [128, 1024], ["h2_%d" % p])
                    for kc in range(8):
                        for hf in range(2):
                            s = wq_n[0] % 2
                            wq_n[0] += 1
                            P.dma((lambda s, kc, hf: lambda e: e.dma_start(out=wqr[s], in_=wq_d[l, kc * 128:(kc + 1) * 128, hf * 1024:(hf + 1) * 1024]))(s, kc, hf),
                                  w=["wq%d" % s], chan="wq%d" % s)
                            for j in range(8):
                                c = hf * 8 + j
                                b = c // 4
                                P.pe(MM(ps[b][:, (c % 4) * 128:(c % 4 + 1) * 128], wqr[s][:, j * 128:(j + 1) * 128], h2T[:, kc, :],
                                        kc == 0 and c % 4 == 0, kc == 7), r=["wq%d" % s, "h2T"], w=["ps%d" % b])
                            yield
                    for b in range(4):
                        src = ps[b][:, :].rearrange("p (c t) -> p c t", t=128)
                        if b % 2 == 0:
                            P.act((lambda b, src: lambda e: e.copy(out=pqT[:, b * 4:(b + 1) * 4, :], in_=src))(b, src), r=["ps%d" % b], w=["pqT%d" % b])
                        else:
                            P.dve((lambda b, src: lambda e: e.tensor_copy(out=pqT[:, b * 4:(b + 1) * 4, :], in_=src))(b, src), r=["ps%d" % b], w=["pqT%d" % b])
                    pq_all = ["pqT0", "pqT1", "pqT2", "pqT3"]
                    P.dve(lambda e: e.memset(eidxf, 0.0), w=["eidxf"])
                    yield
                    for h in range(8):
                        for sd in range(2):
                            P.pe(MM(ps[5][:, sd * 128:(sd + 1) * 128], pqT[:, 2 * h + sd, :], keysT[:, sd, :], True, True),
                                 r=pq_all + ["keysT%d" % sd], w=["ps5"])
                        P.act(lambda e: e.copy(out=sc, in_=ps[5][:, 0:256]), r=["ps5"], w=["sc"])
                        if n == 0 and h == 0:
                            tap("sc%d" % l, sc, [128, 256], ["sc"])
                        for sd in range(2):
                            src = sc[:, sd * 128:(sd + 1) * 128]
                            rep = scr[:, sd * 128:(sd + 1) * 128]
                            v = vv[:, sd * 16:(sd + 1) * 16]
                            ix = ii[:, sd * 16:(sd + 1) * 16]
                            P.dve((lambda v, src: lambda e: e.max(out=v[:, 0:8], in_=src))(v, src), r=["sc"], w=["vv"])
                            P.dve((lambda v, src, ix: lambda e: e.max_index(out=ix[:, 0:8], in_max=v[:, 0:8], in_values=src))(v, src, ix), r=["sc", "vv"], w=["ii"])
                            P.dve((lambda v, src, rep: lambda e: e.match_replace(out=rep, in_to_replace=v[:, 0:8], in_values=src, imm_value=-1e30))(v, src, rep),
                                  r=["sc", "vv"], w=["scr"])
                            P.dve((lambda v, rep: lambda e: e.max(out=v[:, 8:16], in_=rep))(v, rep), r=["scr"], w=["vv"])
                            P.dve((lambda v, rep, ix: lambda e: e.max_index(out=ix[:, 8:16], in_max=v[:, 8:16], in_values=rep))(v, rep, ix), r=["scr", "vv"], w=["ii"])
                        yield
                        P.dve(lambda e: e.tensor_copy(out=iif, in_=ii), r=["ii"], w=["iif"])
                        P.dve(lambda e: e.tensor_scalar(out=iif[:, 0:16], in0=iif[:, 0:16], scalar1=128.0, scalar2=None, op0=ALU.mult), r=["iif"], w=["iif"])
                        c3 = cand.rearrange("p (a b) -> p a b", b=16)
                        x3 = cidx.rearrange("p (a b) -> p a b", b=16)
                        P.dve(lambda e: e.tensor_tensor(out=c3, in0=vv[:, 0:16].unsqueeze(2).broadcast_to([128, 16, 16]),
                                                        in1=vv[:, 16:32].unsqueeze(1).broadcast_to([128, 16, 16]), op=ALU.add), r=["vv"], w=["cand"])
                        P.dve(lambda e: e.tensor_tensor(out=x3, in0=iif[:, 0:16].unsqueeze(2).broadcast_to([128, 16, 16]),
                                                        in1=iif[:, 16:32].unsqueeze(1).broadcast_to([128, 16, 16]), op=ALU.add), r=["iif"], w=["cidx"])
                        P.dve(lambda e: e.max(out=top[:, 0:8], in_=cand), r=["cand"], w=["top"])
                        P.dve(lambda e: e.max_index(out=pos[:, 0:8], in_max=top[:, 0:8], in_values=cand), r=["cand", "top"], w=["pos"])
                        P.dve(lambda e: e.match_replace(out=cand2, in_to_replace=top[:, 0:8], in_values=cand, imm_value=-1e30), r=["cand", "top"], w=["cand2"])
                        P.dve(lambda e: e.max(out=top[:, 8:16], in_=cand2), r=["cand2"], w=["top"])
                        P.dve(lambda e: e.max_index(out=pos[:, 8:16], in_max=top[:, 8:16], in_values=cand2), r=["cand2", "top"], w=["pos"])
                        yield
                        P.dve(lambda e: e.tensor_scalar(out=negm, in0=top[:, 0:1], scalar1=-1.0, scalar2=None, op0=ALU.mult), r=["top"], w=["negm"])
                        P.dve(lambda e: e.memset(gs, 0.0), w=["gs"])
                        P.act(lambda e: e.activation(out=ex, in_=top, func=AF.Exp, bias=negm, accum_out=gs), r=["top", "negm", "gs"], w=["ex", "gs"])
                        P.dve(lambda e: e.reciprocal(out=gs, in_=gs), r=["gs"], w=["gs"])
                        P.dve((lambda h: lambda e: e.tensor_scalar(out=gate[p][:, h * 16:(h + 1) * 16], in0=ex, scalar1=gs, scalar2=None, op0=ALU.mult))(h),
                              r=["ex", "gs"], w=["gate_%d" % p])
                        P.dve(lambda e: e.tensor_copy(out=posff, in_=pos), r=["pos"], w=["posff"])
                        for k in range(16):
                            P.dve((lambda h, k: lambda e: e.scalar_tensor_tensor(
                                out=junk2, in0=iota[:], scalar=posff[:, k:k + 1], in1=cidx, op0=ALU.is_equal, op1=ALU.mult,
                                accum_out=eidxf[:, h * 16 + k:h * 16 + k + 1]))(h, k), r=["iota", "posff", "cidx", "eidxf"], w=["junk2", "eidxf"])
                            if k % 8 == 7:
                                yield
                    P.dve(lambda e: e.tensor_copy(out=eidxi[p], in_=eidxf), r=["eidxf"], w=["eidxi_%d" % p])
                    if n == 0:
                        tap("eidx%d" % l, eidxi[p], [128, 128], ["eidxi_%d" % p], dt=I32)
                        tap("gate%d" % l, gate[p], [128, 128], ["gate_%d" % p])

                def gather(n, nxt):
                    p = n % 2
                    Xn = X[:, n, :]
                    xr = "X%d" % n
                    if not do_gather:
                        for _ in nxt:
                            pass
                        return
                    P.dve(lambda e: e.memset(actv, 0.0), w=["actv"])
                    for hk in range(128):
                        s = ub_n[0] % NU
                        ub_n[0] += 1
                        P.dma((lambda s, hk: lambda e: e.indirect_dma_start(
                            out=ub[s], out_offset=None, in_=pu_d[l], in_offset=bass.IndirectOffsetOnAxis(ap=eidxi[p][:, hk:hk + 1], axis=0)))(s, hk),
                            r=["eidxi_%d" % p], w=["ub%d" % s], chan="ub%d" % s, eng="pool")
                        P.dve((lambda s, hk: lambda e: e.scalar_tensor_tensor(
                            out=djunk, in0=ub[s], scalar=1.0, in1=h2[p], op0=ALU.mult, op1=ALU.mult, accum_out=actv[:, hk:hk + 1]))(s, hk),
                            r=["ub%d" % s, "h2_%d" % p, "actv"], w=["djunk", "actv"])
                        if hk % 4 == 3:
                            next(nxt, None)
                    P.act(lambda e: e.activation(out=ag, in_=actv, func=AF.Gelu), r=["actv"], w=["ag"])
                    P.dve(lambda e: e.tensor_tensor(out=wgt, in0=ag, in1=gate[p], op=ALU.mult), r=["ag", "gate_%d" % p], w=["wgt"])
                    pe_hks = [hk for hk in range(128) if PE_EVERY and hk % PE_EVERY == PE_EVERY - 1]
                    first_dve = True
                    for hk in range(128):
                        s = ub_n[0] % NU
                        ub_n[0] += 1
                        P.dma((lambda s, hk: lambda e: e.indirect_dma_start(
                            out=ub[s], out_offset=None, in_=pv_d[l], in_offset=bass.IndirectOffsetOnAxis(ap=eidxi[p][:, hk:hk + 1], axis=0)))(s, hk),
                            r=["eidxi_%d" % p], w=["ub%d" % s], chan="ub%d" % s, eng="pool")
                        if hk in pe_hks:
                            d = dg_n[0] % 3
                            dg_n[0] += 1
                            P.act((lambda d, hk: lambda e: e.activation(out=dg[d], in_=ident[:], func=AF.Copy, scale=wgt[:, hk:hk + 1]))(d, hk),
                                  r=["ident", "wgt"], w=["dg%d" % d])
                            for b in range(2):
                                P.pe(MM(ps[6 + b][:, :], dg[d], ub[s][:, b * 512:(b + 1) * 512], hk == pe_hks[0], hk == pe_hks[-1]),
                                     r=["dg%d" % d, "ub%d" % s], w=["ps%d" % (6 + b)])
                        elif first_dve:
                            first_dve = False
                            P.dve((lambda s, hk: lambda e: e.tensor_scalar(out=yacc, in0=ub[s], scalar1=wgt[:, hk:hk + 1], scalar2=None, op0=ALU.mult))(s, hk),
                                  r=["ub%d" % s, "wgt"], w=["yacc"])
                        else:
                            P.dve((lambda s, hk: lambda e: e.scalar_tensor_tensor(
                                out=yacc, in0=ub[s], scalar=wgt[:, hk:hk + 1], in1=yacc, op0=ALU.mult, op1=ALU.add))(s, hk),
                                r=["ub%d" % s, "wgt", "yacc"], w=["yacc"])
                        if hk % 4 == 3:
                            next(nxt, None)
                    for _ in nxt:
                        pass
                    if n == 0:
                        tap("wgt%d" % l, wgt, [128, 128], ["wgt"])
                    if PE_EVERY:
                        for b in range(2):
                            P.dve((lambda b: lambda e: e.tensor_tensor(out=yacc[:, b * 512:(b + 1) * 512], in0=ps[6 + b][:, :], in1=yacc[:, b * 512:(b + 1) * 512], op=ALU.add))(b),
                                  r=["ps%d" % (6 + b), "yacc"], w=["yacc"])
                    P.dve(lambda e: e.tensor_tensor(out=yacc, in0=yacc, in1=GATE2[:], op=ALU.mult), r=["yacc", "GATE1"], w=["yacc"])
                    P.dve(lambda e: e.tensor_tensor(out=Xn, in0=Xn, in1=yacc, op=ALU.add), r=["yacc", xr], w=[xr])

                if do_peer:
                    for _ in route(0):
                        pass
                    for n in range(NT):
                        nxt = route(n + 1) if n + 1 < NT else iter(())
                        gather(n, nxt)

            prologue()
            mixing()
            peer()

        for l in range(L):
            layer(l)

        for n in range(NT):
            P.dma((lambda n: lambda e: e.dma_start(out=y_d[n * 128:(n + 1) * 128, :], in_=X[:, n, :]))(n), r=["X%d" % n], chan="out")
        fw = ["out"] + (["tap"] if tap_out else [])
        counts = P.emit(final_wait_chans=fw)
    return nc, counts


def host_consts(NT):
    S = NT * 128
    ar = np.arange(128)
    mlo = (ar[None, :] <= ar[:, None]).astype(np.float32)
    mhi = (ar[:, None] <= ar[None, :]).astype(np.float32)
    invcnt = np.zeros((128, 6, 128), np.float32)
    for j in range(2):
        for e in range(3):
            t = ar + (0 if e == 0 else (S - 128 if e == 2 else 256))
            for p in range(128):
                w = (2, 4, 8, 16)[2 * j + (p // 64)]
                if e == 1:
                    cnt = np.full(128, w)
                else:
                    cnt = np.minimum(t + w // 2, S) - np.maximum(t - w // 2, 0)
                invcnt[p, j * 3 + e, :] = 1.0 / cnt.astype(np.float32)
    invf = (500000.0 ** (-np.arange(0, 16, 2, dtype=np.float32) / 16)).astype(np.float32)
    return dict(
        ident=np.eye(128, dtype=np.float32), ones=np.ones((128, 128), np.float32), mlo=mlo, mhi=mhi,
        iota=np.broadcast_to(np.arange(256, dtype=np.float32), (128, 256)).copy(),
        invcnt=invcnt, invfreq=np.broadcast_to(invf, (128, 8)).copy())


def host_weights(L, norm1_g, norm2_g, w_ada, b_ada, w_in, q_norm_g, k_norm_g, attn_sink, conv_w, pool_w,
                 pool_scale, w_out, peer_wq, peer_keys, peer_u, peer_v):
    f = lambda a: np.ascontiguousarray(np.asarray(a, dtype=np.float32))
    d = {}
    d["n1gT"] = f(np.asarray(norm1_g).reshape(L, 8, 128).transpose(0, 2, 1))
    d["n2gT"] = f(np.asarray(norm2_g).reshape(L, 8, 128).transpose(0, 2, 1))
    d["badaT"] = f(np.asarray(b_ada).reshape(L, 48, 128).transpose(0, 2, 1))
    d["wada"] = f(w_ada)
    d["win"] = f(w_in)
    d["wout"] = f(w_out)
    d["wq"] = f(peer_wq)
    gq = np.concatenate([np.tile(np.asarray(q_norm_g), (1, 8)), np.tile(np.asarray(k_norm_g), (1, 2))], axis=1)
    d["gqk"] = f(np.broadcast_to(gq[:, None, :], (L, 128, 640)))
    d["sink"] = f(np.broadcast_to(np.asarray(attn_sink)[:, None, :], (L, 128, 8)))
    cw = np.asarray(conv_w).reshape(L, 3, 2, 128).transpose(0, 3, 2, 1)
    d["convw"] = f(cw.reshape(L, 128, 6))
    pw = np.zeros((L, 2, 128, 128), np.float32)
    pwa = np.asarray(pool_w)
    for j in range(2):
        pw[:, j, 0:64, 0:64] = pwa[:, 2 * j]
        pw[:, j, 64:128, 64:128] = pwa[:, 2 * j + 1]
    d["poolw"] = pw
    d["pscale"] = f(np.asarray(pool_scale).reshape(L, 2, 128).transpose(0, 2, 1))
    d["keysT"] = f(np.asarray(peer_keys).transpose(0, 1, 3, 2))
    pu = np.asarray(peer_u, dtype=np.float32)
    pv = np.asarray(peer_v, dtype=np.float32)
    for l in range(L):
        d["pu%d" % l] = np.ascontiguousarray(pu[l])
        d["pv%d" % l] = np.ascontiguousarray(pv[l])
    return d


_CACHE = {}


def kernel(x, c, positions, norm1_g, norm2_g, w_ada, b_ada, w_in, q_norm_g, k_norm_g, attn_sink, conv_w,
           pool_w, pool_scale, w_out, peer_wq, peer_keys, peer_u, peer_v):
    x = np.asarray(x, dtype=np.float32)
    c = np.asarray(c, dtype=np.float32)
    positions = np.asarray(positions).astype(np.int32)
    B, S, D = x.shape
    L = np.asarray(w_in).shape[0]
    NT = S // 128
    key = (NT, L)
    if key not in _CACHE:
        _CACHE[key] = build(NT, L)[0]
    nc = _CACHE[key]
    shared = host_weights(L, norm1_g, norm2_g, w_ada, b_ada, w_in, q_norm_g, k_norm_g, attn_sink, conv_w,
                          pool_w, pool_scale, w_out, peer_wq, peer_keys, peer_u, peer_v)
    shared.update(host_consts(NT))
    in_maps = []
    for b in range(B):
        m = dict(shared)
        m["x"] = np.ascontiguousarray(x[b])
        m["cT"] = np.ascontiguousarray(c[b].reshape(8, 128).T)
        m["posT"] = np.ascontiguousarray(positions[b].reshape(NT, 128).T)
        in_maps.append(m)
    res = run_bass_kernel_spmd(nc, in_maps, core_ids=list(range(B)))
    return np.stack([np.asarray(r["y"]) for r in res.results], axis=0).astype(np.float32)
```

```python
import math
from contextlib import ExitStack

import numpy as np
import concourse.bass as bass
import concourse.mybir as mybir
from concourse.bass_utils import run_bass_kernel_spmd

F32 = mybir.dt.float32
U32 = mybir.dt.uint32
I32 = mybir.dt.int32
ALU = mybir.AluOpType
AF = mybir.ActivationFunctionType
AX = mybir.AxisListType

ENGINES = ["pe", "act", "dve", "pool", "sp"]
EPS = 1e-6
PI = math.pi


class Prog:
    def __init__(self, nc):
        self.nc = nc
        self.ops = []
        self.last_w = {}
        self.readers = {}
        self.last_eng = {}
        self.last_chan = {}
        self.pending = {}
        self.burst = set()

    def add(self, eng, fn, r=(), w=(), dma=False, chan=None):
        oid = len(self.ops)
        deps = set()
        for k in r:
            if k in self.last_w:
                deps.add(self.last_w[k])
        for k in w:
            if k in self.last_w:
                deps.add(self.last_w[k])
            for q in self.readers.get(k, ()):
                deps.add(q)
        if eng in self.pending:
            deps |= self.pending.pop(eng)
        for k in r:
            self.readers.setdefault(k, []).append(oid)
        for k in w:
            self.last_w[k] = oid
            self.readers[k] = []
        deps.discard(oid)
        self.ops.append(dict(eng=eng, fn=fn, deps=sorted(deps), dma=dma, chan=chan))
        if dma:
            self.last_chan[chan] = oid
        else:
            self.last_eng[eng] = oid
        return oid

    def barrier(self):
        b = set(self.last_eng.values()) | set(self.last_chan.values())
        for e in ENGINES:
            self.pending[e] = set(b) | self.pending.get(e, set())

    def pe(self, fn, r=(), w=()):
        return self.add("pe", fn, r, w)

    def act(self, fn, r=(), w=()):
        return self.add("act", fn, r, w)

    def dve(self, fn, r=(), w=()):
        return self.add("dve", fn, r, w)

    def pool(self, fn, r=(), w=()):
        return self.add("pool", fn, r, w)

    def dma(self, fn, r=(), w=(), chan=None, eng="sp"):
        return self.add(eng, fn, r, w, dma=True, chan=chan)

    def emit(self, final_wait_chans=()):
        nc = self.nc
        ops = self.ops
        eng_count = {e: 0 for e in ENGINES}
        chan_count = {}
        for op in ops:
            if op["dma"]:
                c = op["chan"]
                chan_count[c] = chan_count.get(c, 0) + 16
                op["sig"] = ("c", c, chan_count[c])
                op["inc"] = True
            else:
                eng_count[op["eng"]] += 1
                op["sig"] = ("e", op["eng"], eng_count[op["eng"]])
        for op in ops:
            if op["dma"] and op["chan"] in self.burst:
                op["sig"] = ("c", op["chan"], chan_count[op["chan"]])
        with ExitStack() as st:
            sems = {}
            for e in ENGINES:
                sems[("e", e)] = st.enter_context(nc.semaphore("s_" + e))
            for c in chan_count:
                sems[("c", c)] = st.enter_context(nc.semaphore("c_" + str(c)))
            block = st.enter_context(nc.Block())

            def make(engname):
                def body(eng):
                    waited = {}
                    for op in ops:
                        if op["eng"] != engname:
                            continue
                        for d in op["deps"]:
                            kind, key, val = ops[d]["sig"]
                            if kind == "e" and key == "pe" and engname == "pe":
                                continue
                            sk = (kind, key)
                            if waited.get(sk, 0) >= val:
                                continue
                            eng.wait_ge(sems[sk], val)
                            waited[sk] = val
                        ins = op["fn"](eng)
                        kind, key, val = op["sig"]
                        ins.then_inc(sems[(kind, key)], 16 if kind == "c" else 1)
                    if engname == "sp":
                        for c in final_wait_chans:
                            eng.wait_ge(sems[("c", c)], chan_count[c])
                return body

            block.tensor(make("pe"))
            block.scalar(make("act"))
            block.vector(make("dve"))
            block.gpsimd(make("pool"))
            block.sync(make("sp"))
        return eng_count, chan_count


def build(NT, L, taps=(), do_peer=True, do_gather=True, stage='all', cut=99, PE_EVERY=34, NU_RING=12):
    S = NT * 128
    nc = bass.Bass("TRN2", target_bir_lowering=False)
    P = Prog(nc)
    P.burst = {"cst"} | {"lw%d" % l for l in range(L)}

    def din(name, shape, dt=F32):
        return nc.dram_tensor(name, shape, dt, kind="ExternalInput").ap()

    x_d = din("x", [S, 1024])
    cT_d = din("cT", [128, 8])
    pos_d = din("posT", [128, NT], I32)
    n1g_d = din("n1gT", [L, 128, 8])
    n2g_d = din("n2gT", [L, 128, 8])
    bada_d = din("badaT", [L, 128, 48])
    wada_d = din("wada", [L, 1024, 6144])
    win_d = din("win", [L, 1024, 1792])
    wout_d = din("wout", [L, 1024, 1024])
    wq_d = din("wq", [L, 1024, 2048])
    gqk_d = din("gqk", [L, 128, 640])
    sink_d = din("sink", [L, 128, 8])
    convw_d = din("convw", [L, 128, 6])
    poolw_d = din("poolw", [L, 2, 128, 128])
    pscale_d = din("pscale", [L, 128, 2])
    keysT_d = din("keysT", [L, 2, 128, 128])
    pu_d = [din("pu%d" % l, [16384, 1024]) for l in range(L)]
    pv_d = [din("pv%d" % l, [16384, 1024]) for l in range(L)]
    ident_d = din("ident", [128, 128])
    ones_d = din("ones", [128, 128])
    mlo_d = din("mlo", [128, 128])
    mhi_d = din("mhi", [128, 128])
    iota_d = din("iota", [128, 256])
    invcnt_d = din("invcnt", [128, 6, 128])
    invfreq_d = din("invfreq", [128, 8])
    y_d = nc.dram_tensor("y", [S, 1024], F32, kind="ExternalOutput").ap()
    tap_out = {}

    with ExitStack() as st:
        def sb(name, shape, dt=F32):
            return st.enter_context(nc.sbuf_tensor(name, shape, dt))

        X = sb("X", [128, NT, 1024])
        ident = sb("ident_s", [128, 128])
        ones = sb("ones_s", [128, 128])
        mlo = sb("mlo_s", [128, 128])
        mhi = sb("mhi_s", [128, 128])
        iota = sb("iota_s", [128, 256])
        invcnt = sb("invcnt_s", [128, 6, 128])
        invfreq = sb("invfreq_s", [128, 8])
        cT = sb("cT_s", [128, 8])
        cact = sb("cact", [128, 8])
        posi = sb("posi", [128, NT], I32)
        posf = sb("posf", [128, NT])
        ang = sb("ang", [128, NT, 8])
        ang2 = sb("ang2", [128, NT, 8])
        angi = sb("angi", [128, NT, 8], I32)
        cosT = sb("cosT", [128, NT, 8])
        sinT = sb("sinT", [128, NT, 8])
        n1g = sb("n1g", [128, 8])
        n2g = sb("n2g", [128, 8])
        bada = sb("bada", [128, 48])
        modT = sb("modT", [128, 48])
        der = sb("der", [128, 32])
        GATE1 = sb("GATE1", [128, 1024])
        GATE2 = sb("GATE2", [128, 1024])
        gqk = sb("gqk_s", [128, 640])
        sinkt = sb("sinkt", [128, 8])
        esink = sb("esink", [128, 8])
        convw = sb("convw_s", [128, 6])
        poolw = sb("poolw_s", [128, 2, 128])
        pscale = sb("pscale_s", [128, 2])
        keysT = sb("keysT_s", [128, 2, 128])
        st8 = sb("st8", [128, 64])
        AW = 29800
        arena = sb("arena", [128, AW])
        ps = [st.enter_context(nc.psum_tensor("ps%d" % i, [128, 512], F32)) for i in range(8)]

        class Carver:
            def __init__(self):
                self.off = 0

            def get(self, words):
                a = arena[:, self.off:self.off + words]
                self.off += words
                assert self.off <= AW, self.off
                return a

        def tap(name, ap, shape, r, dt=F32):
            if name not in taps:
                return
            t = nc.dram_tensor("tap_" + name, shape, dt, kind="ExternalOutput").ap()
            tap_out[name] = t
            P.dma(lambda e: e.dma_start(out=t, in_=ap), r=r, chan="tap")

        def ld(dst, src, name, chan="cst"):
            P.dma(lambda e: e.dma_start(out=dst, in_=src), w=[name], chan=chan)

        ld(ident[:], ident_d, "ident")
        ld(ones[:], ones_d, "ones")
        ld(mlo[:], mlo_d, "mlo")
        ld(mhi[:], mhi_d, "mhi")
        ld(iota[:], iota_d, "iota")
        ld(invcnt[:], invcnt_d, "invcnt")
        ld(invfreq[:], invfreq_d, "invfreq")
        ld(cT[:], cT_d, "cT")
        ld(posi[:], pos_d, "posi")
        for n in range(NT):
            ld(X[:, n, :], x_d[n * 128:(n + 1) * 128, :], "X%d" % n, chan="xin%d" % n)

        P.act(lambda e: e.activation(out=cact[:], in_=cT[:], func=AF.Silu), r=["cT"], w=["cact"])
        P.dve(lambda e: e.tensor_copy(out=posf[:], in_=posi[:]), r=["posi"], w=["posf"])
        P.dve(lambda e: e.tensor_tensor(out=ang[:], in0=posf[:].unsqueeze(2).broadcast_to([128, NT, 8]),
                                        in1=invfreq[:].unsqueeze(1).broadcast_to([128, NT, 8]), op=ALU.mult),
              r=["posf", "invfreq"], w=["ang"])
        C1 = 6.28125
        C2 = 2 * PI - C1
        P.dve(lambda e: e.tensor_scalar(out=ang2[:], in0=ang[:], scalar1=1.0 / (2 * PI), scalar2=None, op0=ALU.mult), r=["ang"], w=["ang2"])
        P.dve(lambda e: e.tensor_copy(out=angi[:], in_=ang2[:]), r=["ang2"], w=["angi"])
        P.dve(lambda e: e.tensor_copy(out=ang2[:], in_=angi[:]), r=["angi"], w=["ang2"])
        P.dve(lambda e: e.scalar_tensor_tensor(out=ang[:], in0=ang2[:], scalar=-C1, in1=ang[:], op0=ALU.mult, op1=ALU.add), r=["ang2", "ang"], w=["ang"])
        P.dve(lambda e: e.scalar_tensor_tensor(out=ang[:], in0=ang2[:], scalar=-C2, in1=ang[:], op0=ALU.mult, op1=ALU.add), r=["ang2", "ang"], w=["ang"])

        def wrap():
            P.dve(lambda e: e.tensor_scalar(out=ang2[:], in0=ang[:], scalar1=PI, scalar2=-2 * PI, op0=ALU.is_gt, op1=ALU.mult), r=["ang", "sinT", "cosT"], w=["ang2"])
            P.dve(lambda e: e.tensor_tensor(out=ang[:], in0=ang[:], in1=ang2[:], op=ALU.add), r=["ang", "ang2"], w=["ang"])
            P.dve(lambda e: e.tensor_scalar(out=ang[:], in0=ang[:], scalar1=-PI, scalar2=PI, op0=ALU.max, op1=ALU.min), r=["ang"], w=["ang"])

        wrap()
        P.act(lambda e: e.activation(out=sinT[:], in_=ang[:], func=AF.Sin), r=["ang"], w=["sinT"])
        P.dve(lambda e: e.tensor_scalar(out=ang[:], in0=ang[:], scalar1=0.5 * PI, scalar2=None, op0=ALU.add), r=["ang", "sinT"], w=["ang"])
        wrap()
        P.act(lambda e: e.activation(out=cosT[:], in_=ang[:], func=AF.Sin), r=["ang"], w=["cosT"])
        tap("cos", cosT[:], [128, NT, 8], ["cosT"])
        tap("sin", sinT[:], [128, NT, 8], ["sinT"])

        def rstd_from_ss(ss_ap, out_ap, n, rname, wname):
            P.dve(lambda e: e.tensor_scalar(out=out_ap, in0=ss_ap, scalar1=1.0 / n, scalar2=EPS, op0=ALU.mult, op1=ALU.add),
                  r=[rname], w=[wname])
            P.act(lambda e: e.activation(out=out_ap, in_=out_ap, func=AF.Sqrt), r=[wname], w=[wname])
            P.dve(lambda e: e.reciprocal(out=out_ap, in_=out_ap), r=[wname], w=[wname])

        def MM(out, lhsT, rhs, start, stop):
            return lambda e: e.matmul(out, lhsT=lhsT, rhs=rhs, start=start, stop=stop, skip_group_check=True)

        def TR(out, in_, idn):
            return lambda e: e.transpose(out=out, in_=in_, identity=idn)

        def layer(l):
            G1s, SH1 = der[:, 0:8], modT[:, 0:8]
            G2s, SH2 = der[:, 8:16], modT[:, 24:32]
            def prologue():
                P.barrier()
                cv = Carver()
                wad = [cv.get(3072), cv.get(3072)]
                Dt = cv.get(128)
                ld(n1g[:], n1g_d[l], "n1g", chan="lw%d" % l)
                ld(n2g[:], n2g_d[l], "n2g", chan="lw%d" % l)
                ld(bada[:], bada_d[l], "bada", chan="lw%d" % l)
                ld(gqk[:], gqk_d[l], "gqk", chan="lw%d" % l)
                ld(sinkt[:], sink_d[l], "sinkt", chan="lw%d" % l)
                ld(convw[:], convw_d[l], "convw", chan="lw%d" % l)
                for j in range(2):
                    ld(poolw[:, j, :], poolw_d[l, j], "poolw%d" % j, chan="lw%d" % l)
                    ld(keysT[:, j, :], keysT_d[l, j], "keysT%d" % j, chan="lw%d" % l)
                ld(pscale[:], pscale_d[l], "pscale", chan="lw%d" % l)
                first = True
                pi = 0
                for kc in range(8):
                    for half in range(2):
                        s = pi % 2
                        pi += 1
                        P.dma((lambda s, kc, half: lambda e: e.dma_start(
                            out=wad[s], in_=wada_d[l, kc * 128:(kc + 1) * 128, half * 3072:(half + 1) * 3072]))(s, kc, half),
                            w=["wad%d" % s], chan="wad%d" % s)
                        for jj in range(24):
                            j = half * 24 + jj
                            P.pe(MM(ps[4][:, j:j + 1], wad[s][:, jj * 128:(jj + 1) * 128], cact[:, kc:kc + 1], first, kc == 7),
                                 r=["wad%d" % s, "cact"], w=["ps4"])
                            first = False
                P.dve(lambda e: e.tensor_tensor(out=modT[:], in0=ps[4][:, 0:48], in1=bada[:], op=ALU.add),
                      r=["ps4", "bada"], w=["modT"])
                tap("modT%d" % l, modT[:], [128, 48], ["modT"])
                P.dve(lambda e: e.scalar_tensor_tensor(out=der[:, 0:8], in0=modT[:, 8:16], scalar=1.0, in1=n1g[:], op0=ALU.add, op1=ALU.mult),
                      r=["modT", "n1g"], w=["der"])
                P.dve(lambda e: e.scalar_tensor_tensor(out=der[:, 8:16], in0=modT[:, 32:40], scalar=1.0, in1=n2g[:], op0=ALU.add, op1=ALU.mult),
                      r=["modT", "n2g", "der"], w=["der"])
                for gi, (GT, c0) in enumerate(((GATE1, 16), (GATE2, 40))):
                    for ch in range(8):
                        P.dve((lambda ch, c0: lambda e: e.tensor_scalar(out=Dt, in0=ident[:], scalar1=modT[:, c0 + ch:c0 + ch + 1], scalar2=None, op0=ALU.mult))(ch, c0),
                              r=["ident", "modT"], w=["Dt"])
                        P.pe(MM(ps[ch // 4][:, (ch % 4) * 128:(ch % 4 + 1) * 128], ones[:], Dt, True, True),
                             r=["ones", "Dt"], w=["ps%d" % (ch // 4)])
                    for b in range(2):
                        P.act((lambda b, GT: lambda e: e.copy(out=GT[:, b * 512:(b + 1) * 512], in_=ps[b][:]))(b, GT),
                              r=["ps%d" % b], w=["GATE%d" % gi])
                tap("gate1_%d" % l, GATE1[:], [128, 1024], ["GATE0"])
                P.act(lambda e: e.activation(out=esink[:], in_=sinkt[:], func=AF.Exp), r=["sinkt"], w=["esink"])

            def mixing():
                P.barrier()
                cv = Carver()
                wi = [cv.get(1792) for _ in range(2)]
                wo = [cv.get(1024) for _ in range(2)]
                xn = cv.get(1024)
                hT = cv.get(1024).rearrange("p (c t) -> p c t", t=128)
                zsb = cv.get(1792)
                sq = cv.get(640)
                qn = cv.get(640)
                rt = [cv.get(80).rearrange("p (h d) -> p h d", d=8) for _ in range(6)]
                qT = [cv.get(1024).rearrange("p (h t) -> p h t", t=128) for _ in range(2)]
                kT = [cv.get(256).rearrange("p (g t) -> p g t", t=128) for _ in range(4)]
                vR = [cv.get(128).rearrange("p (g d) -> p g d", d=64) for _ in range(4)]
                Wb = [cv.get(1152).rearrange("p (c t) -> p c t", t=144) for _ in range(2)]
                tail = [cv.get(64).rearrange("p (c t) -> p c t", t=8) for _ in range(2)]
                cx = cv.get(144)
                cacc = cv.get(128)
                Alv = [cv.get(144) for _ in range(4)]
                ptmp = cv.get(128)
                pooled = cv.get(128)
                cpT = [cv.get(512).rearrange("p (c t) -> p c t", t=128) for _ in range(2)]
                PT = [cv.get(512) for _ in range(2)]
                den = cv.get(512)
                attnT = cv.get(1024).rearrange("p (h t) -> p h t", t=128)
                otmp = cv.get(1024)
                junk = otmp
                ss = st8[:, 0:1]
                rstd = st8[:, 1:2]
                ssq = st8[:, 8:18]
                rsq = st8[:, 24:34]
                wi_n = [0]
                wo_n = [0]

                def stepA(n):
                    Xn = X[:, n, :]
                    xr = "X%d" % n
                    P.dve(lambda e: e.memset(ss, 0.0), w=["ss"])
                    P.act(lambda e: e.activation(out=junk, in_=Xn, func=AF.Square, accum_out=ss), r=[xr, "ss"], w=["junk", "ss"])
                    rstd_from_ss(ss, rstd, 1024, "ss", "rstd")
                    P.dve(lambda e: e.tensor_scalar(out=xn, in0=Xn, scalar1=rstd, scalar2=None, op0=ALU.mult), r=[xr, "rstd"], w=["xn"])
                    if n == 0:
                        tap("xn%d" % l, xn, [128, 1024], ["xn"])
                        tap("st8_%d" % l, st8[:], [128, 64], ["rstd", "ss"])
                    for half in range(2):
                        for c4 in range(4):
                            kc = half * 4 + c4
                            P.pe(TR(ps[4][:, c4 * 128:(c4 + 1) * 128], xn[:, kc * 128:(kc + 1) * 128], ident[:]),
                                 r=["xn", "ident"], w=["ps4"])
                        for c4 in range(4):
                            kc = half * 4 + c4
                            P.dve((lambda kc, c4: lambda e: e.tensor_scalar(
                                out=hT[:, kc, :], in0=ps[4][:, c4 * 128:(c4 + 1) * 128], scalar1=G1s[:, kc:kc + 1], scalar2=SH1[:, kc:kc + 1],
                                op0=ALU.mult, op1=ALU.add))(kc, c4), r=["ps4", "der", "modT"], w=["hT"])
                    if n == 0:
                        tap("hT%d" % l, hT, [128, 8, 128], ["hT"])
                    if cut <= 1:
                        return
                    widths = [512, 512, 512, 256]
                    for kc in range(8):
                        s = wi_n[0] % 2
                        wi_n[0] += 1
                        P.dma((lambda s, kc: lambda e: e.dma_start(out=wi[s], in_=win_d[l, kc * 128:(kc + 1) * 128, :]))(s, kc),
                              w=["wi%d" % s], chan="wi%d" % s)
                        for b in range(4):
                            P.pe(MM(ps[b][:, 0:widths[b]], hT[:, kc, :], wi[s][:, b * 512:b * 512 + widths[b]], kc == 0, kc == 7),
                                 r=["hT", "wi%d" % s], w=["ps%d" % b])
                    for b in range(4):
                        if b % 2 == 0:
                            P.act((lambda b: lambda e: e.copy(out=zsb[:, b * 512:b * 512 + widths[b]], in_=ps[b][:, 0:widths[b]]))(b),
                                  r=["ps%d" % b], w=["zsb%d" % b])
                        else:
                            P.dve((lambda b: lambda e: e.tensor_copy(out=zsb[:, b * 512:b * 512 + widths[b]], in_=ps[b][:, 0:widths[b]]))(b),
                                  r=["ps%d" % b], w=["zsb%d" % b])
                    zall = ["zsb0", "zsb1", "zsb2", "zsb3"]
                    if cut <= 2:
                        return
                    if n == 0:
                        tap("z%d" % l, zsb, [128, 1792], zall)
                    zqk = zsb[:, 0:640]
                    P.dve(lambda e: e.tensor_tensor(out=sq, in0=zqk, in1=zqk, op=ALU.mult), r=zall, w=["sq"])
                    P.dve(lambda e: e.tensor_reduce(out=ssq, in_=sq.rearrange("p (h d) -> p h d", d=64), axis=AX.X, op=ALU.add),
                          r=["sq"], w=["ssq"])
                    rstd_from_ss(ssq, rsq, 64, "ssq", "rsq")
                    qn3 = qn.rearrange("p (h d) -> p h d", d=64)
                    P.dve(lambda e: e.tensor_tensor(out=qn3, in0=zqk.rearrange("p (h d) -> p h d", d=64),
                                                    in1=rsq.unsqueeze(2).broadcast_to([128, 10, 64]), op=ALU.mult),
                          r=zall + ["rsq"], w=["qn"])
                    P.dve(lambda e: e.tensor_tensor(out=qn, in0=qn, in1=gqk[:], op=ALU.mult), r=["qn", "gqk"], w=["qn"])
                    cosb = cosT[:, n, :].unsqueeze(1).broadcast_to([128, 10, 8])
                    if cut <= 3:
                        return
                    sinb = sinT[:, n, :].unsqueeze(1).broadcast_to([128, 10, 8])
                    t1 = qn3[:, :, 0:8]
                    t2 = qn3[:, :, 8:16]
                    P.dve(lambda e: e.tensor_tensor(out=rt[0], in0=t1, in1=cosb, op=ALU.mult), r=["qn", "cosT"], w=["rt0"])
                    P.dve(lambda e: e.tensor_tensor(out=rt[1], in0=t2, in1=sinb, op=ALU.mult), r=["qn", "sinT"], w=["rt1"])
                    P.dve(lambda e: e.tensor_tensor(out=rt[2], in0=t2, in1=cosb, op=ALU.mult), r=["qn", "cosT"], w=["rt2"])
                    P.dve(lambda e: e.tensor_tensor(out=rt[3], in0=t1, in1=sinb, op=ALU.mult), r=["qn", "sinT"], w=["rt3"])
                    P.dve(lambda e: e.tensor_tensor(out=t1, in0=rt[0], in1=rt[1], op=ALU.subtract), r=["rt0", "rt1", "rt2", "rt3", "qn"], w=["qn"])
                    P.dve(lambda e: e.tensor_tensor(out=t2, in0=rt[2], in1=rt[3], op=ALU.add), r=["rt2", "rt3", "qn"], w=["qn"])
                    if n == 0:
                        tap("qn%d" % l, qn, [128, 640], ["qn"])
                    if cut <= 4:
                        return
                    qs = n % 2
                    ks = n % 4
                    for grp in range(2):
                        for h4 in range(4):
                            h = grp * 4 + h4
                            P.pe(TR(ps[4][0:64, h4 * 128:(h4 + 1) * 128], qn3[:, h, :], ident[:]), r=["qn", "ident"], w=["ps4"])
                        P.act((lambda grp: lambda e: e.activation(
                            out=qT[qs][0:64, grp * 4:(grp + 1) * 4, :], in_=ps[4][0:64, :].rearrange("p (h t) -> p h t", t=128),
                            func=AF.Copy, scale=0.125))(grp), r=["ps4"], w=["qT%d" % qs])
                    for g in range(2):
                        P.pe(TR(ps[4][0:64, g * 128:(g + 1) * 128], qn3[:, 8 + g, :], ident[:]), r=["qn", "ident"], w=["ps4"])
                    P.dve(lambda e: e.tensor_copy(out=kT[ks][0:64, :, :], in_=ps[4][0:64, 0:256].rearrange("p (g t) -> p g t", t=128)),
                          r=["ps4"], w=["kT%d" % ks])
                    P.act(lambda e: e.copy(out=vR[ks], in_=zsb[:, 640:768].rearrange("p (g d) -> p g d", d=64)), r=zall, w=["vR%d" % ks])
                    if cut <= 5:
                        return
                    ws = n % 2
                    for half in range(2):
                        for c4 in range(4):
                            c = half * 4 + c4
                            P.pe(TR(ps[4][:, c4 * 128:(c4 + 1) * 128], zsb[:, 768 + c * 128:768 + (c + 1) * 128], ident[:]),
                                 r=zall + ["ident"], w=["ps4"])
                        pv4 = ps[4][:, :].rearrange("p (c t) -> p c t", t=128)
                        cs = slice(half * 4, half * 4 + 4)
                        P.act((lambda cs, pv4: lambda e: e.copy(out=Wb[ws][:, cs, 8:136], in_=pv4))(cs, pv4), r=["ps4"], w=["Wb%dm" % ws])
                    if cut <= 6:
                        return
                    if n == 0:
                        P.dve(lambda e: e.memset(Wb[ws][:, :, 0:8], 0.0), w=["Wb%dl" % ws])
                    else:
                        P.dve(lambda e: e.tensor_copy(out=Wb[1 - ws][:, :, 136:144], in_=Wb[ws][:, :, 8:16]), r=["Wb%dm" % ws], w=["Wb%dr" % (1 - ws)])
                        P.dve(lambda e: e.tensor_copy(out=Wb[ws][:, :, 0:8], in_=Wb[1 - ws][:, :, 128:136]), r=["Wb%dm" % (1 - ws)], w=["Wb%dl" % ws])
                    if n == NT - 1:
                        P.dve(lambda e: e.memset(Wb[ws][:, :, 136:144], 0.0), w=["Wb%dr" % ws])

                def stepC(m):
                    ws = m % 2
                    W = Wb[ws]
                    wr = ["Wb%dm" % ws, "Wb%dl" % ws, "Wb%dr" % ws]
                    cp = cpT[ws]
                    edge = 0 if m == 0 else (2 if m == NT - 1 else 1)
                    for j in range(2):
                        P.dve((lambda j: lambda e: e.tensor_tensor(out=cx, in0=W[:, 4 + j, :], in1=W[:, j, :], op=ALU.mult))(j), r=wr, w=["cx"])
                        P.dve((lambda j: lambda e: e.tensor_scalar(out=cacc, in0=cx[:, 8:136], scalar1=convw[:, j * 3 + 1:j * 3 + 2], scalar2=None, op0=ALU.mult))(j),
                              r=["cx", "convw"], w=["cacc"])
                        P.dve((lambda j: lambda e: e.scalar_tensor_tensor(out=cacc, in0=cx[:, 7:135], scalar=convw[:, j * 3:j * 3 + 1], in1=cacc, op0=ALU.mult, op1=ALU.add))(j),
                              r=["cx", "convw", "cacc"], w=["cacc"])
                        P.dve((lambda j: lambda e: e.scalar_tensor_tensor(out=cacc, in0=cx[:, 9:137], scalar=convw[:, j * 3 + 2:j * 3 + 3], in1=cacc, op0=ALU.mult, op1=ALU.add))(j),
                              r=["cx", "convw", "cacc"], w=["cacc"])
                        P.dve((lambda j: lambda e: e.tensor_tensor(out=cp[:, j, :], in0=cacc, in1=W[:, 2 + j, 8:136], op=ALU.mult))(j),
                              r=wr + ["cacc"], w=["cpT%d" % ws])
                    for j in range(2):
                        u = W[:, 6 + j, :]
                        A1, A4, A8, A16 = Alv
                        P.dve((lambda u: lambda e: e.tensor_tensor(out=A1[:, 1:144], in0=u[:, 0:143], in1=u[:, 1:144], op=ALU.add))(u), r=wr, w=["A1"])
                        P.dve(lambda e: e.tensor_tensor(out=A4[:, 2:143], in0=A1[:, 1:142], in1=A1[:, 3:144], op=ALU.add), r=["A1"], w=["A4"])
                        if j == 0:
                            lo, hi = A1, A4
                        else:
                            P.dve(lambda e: e.tensor_tensor(out=A8[:, 4:141], in0=A4[:, 2:139], in1=A4[:, 6:143], op=ALU.add), r=["A4"], w=["A8"])
                            P.dve(lambda e: e.tensor_tensor(out=A16[:, 8:137], in0=A8[:, 4:133], in1=A8[:, 12:141], op=ALU.add), r=["A8"], w=["A16"])
                            lo, hi = A8, A16
                        for (p0, p1, A) in ((0, 64, lo), (64, 128, hi)):
                            P.dve((lambda p0, p1, A, j: lambda e: e.tensor_tensor(out=ptmp[p0:p1, :], in0=A[p0:p1, 8:136], in1=invcnt[p0:p1, j * 3 + edge, :], op=ALU.mult))(p0, p1, A, j),
                                  r=["A1", "A4", "A8", "A16", "invcnt"], w=["ptmp"])
                        P.dve((lambda u: lambda e: e.tensor_tensor(out=pooled, in0=ptmp, in1=u[:, 8:136], op=ALU.subtract))(u), r=["ptmp"] + wr, w=["pooled"])
                        P.pe(MM(ps[5][:, 0:128], poolw[:, j, :], pooled, True, True), r=["pooled", "poolw%d" % j], w=["ps5"])
                        P.act((lambda j: lambda e: e.activation(out=cp[:, 2 + j, :], in_=ps[5][:, 0:128], func=AF.Copy, scale=pscale[:, j:j + 1]))(j),
                              r=["ps5", "pscale"], w=["cpT%d" % ws])
                    if m == 0:
                        tap("cp%d" % l, cp, [128, 4, 128], ["cpT%d" % ws])

                def stepD(m):
                    qs = m % 2
                    blocks = [j for j in (m - 1, m, m + 1) if 0 <= j < NT]
                    pslot = [0]
                    for g in range(2):
                        for bi, j in enumerate(blocks):
                            ks = j % 4
                            P.pe(MM(ps[5][:, :], kT[ks][0:64, g, :], qT[qs][0:64, g * 4:(g + 1) * 4, :], True, True),
                                 r=["kT%d" % ks, "qT%d" % qs], w=["ps5"])
                            s = pslot[0] % 2
                            pslot[0] += 1
                            P.act((lambda s: lambda e: e.activation(out=PT[s], in_=ps[5][:, :], func=AF.Exp))(s), r=["ps5"], w=["PT%d" % s])
                            if j != m:
                                mk = mlo if j < m else mhi
                                P.dve((lambda s, mk: lambda e: e.tensor_tensor(
                                    out=PT[s].rearrange("p (h t) -> p h t", t=128), in0=PT[s].rearrange("p (h t) -> p h t", t=128),
                                    in1=mk[:].unsqueeze(1).broadcast_to([128, 4, 128]), op=ALU.mult))(s, mk),
                                    r=["PT%d" % s, "mlo", "mhi"], w=["PT%d" % s])
                            P.pe(MM(ps[6][0:64, :], vR[ks][:, g, :], PT[s], bi == 0, bi == len(blocks) - 1), r=["vR%d" % ks, "PT%d" % s], w=["ps6"])
                            P.pe(MM(ps[7][0:64, :], ones[:, 0:64], PT[s], bi == 0, bi == len(blocks) - 1), r=["ones", "PT%d" % s], w=["ps7"])
                        P.dve((lambda g: lambda e: e.tensor_tensor(
                            out=den[0:64, :].rearrange("p (h t) -> p h t", t=128), in0=ps[7][0:64, :].rearrange("p (h t) -> p h t", t=128),
                            in1=esink[0:64, g * 4:(g + 1) * 4].unsqueeze(2).broadcast_to([64, 4, 128]), op=ALU.add))(g),
                            r=["ps7", "esink"], w=["den"])
                        P.dve(lambda e: e.reciprocal(out=den[0:64, :], in_=den[0:64, :]), r=["den"], w=["den"])
                        P.dve((lambda g: lambda e: e.tensor_tensor(
                            out=attnT[0:64, g * 4:(g + 1) * 4, :], in0=ps[6][0:64, :].rearrange("p (h t) -> p h t", t=128),
                            in1=den[0:64, :].rearrange("p (h t) -> p h t", t=128), op=ALU.mult))(g),
                            r=["ps6", "den"], w=["attnT"])
                    if m == 0:
                        tap("attnT%d" % l, attnT[0:64, :, :], [64, 8, 128], ["attnT"])
                    cp = cpT[m % 2]
                    npieces = 12
                    for pc in range(npieces):
                        s = wo_n[0] % 2
                        wo_n[0] += 1
                        if pc < 8:
                            P.dma((lambda s, pc: lambda e: e.dma_start(out=wo[s][0:64, :], in_=wout_d[l, pc * 64:(pc + 1) * 64, :]))(s, pc),
                                  w=["wo%d" % s], chan="wo%d" % s)
                            lhs = attnT[0:64, pc, :]
                            rw = wo[s][0:64, :]
                            rr = ["attnT"]
                        else:
                            c = pc - 8
                            P.dma((lambda s, c: lambda e: e.dma_start(out=wo[s], in_=wout_d[l, 512 + c * 128:512 + (c + 1) * 128, :]))(s, c),
                                  w=["wo%d" % s], chan="wo%d" % s)
                            lhs = cp[:, c, :]
                            rw = wo[s]
                            rr = ["cpT%d" % (m % 2)]
                        for b in range(2):
                            P.pe(MM(ps[b][:, :], lhs, rw[:, b * 512:(b + 1) * 512], pc == 0, pc == npieces - 1), r=rr + ["wo%d" % s], w=["ps%d" % b])
                    for b in range(2):
                        P.dve((lambda b: lambda e: e.tensor_tensor(out=otmp[:, b * 512:(b + 1) * 512], in0=ps[b][:, :], in1=GATE1[:, b * 512:(b + 1) * 512], op=ALU.mult))(b),
                              r=["ps%d" % b, "GATE0"], w=["junk"])
                    P.dve(lambda e: e.tensor_tensor(out=X[:, m, :], in0=X[:, m, :], in1=otmp, op=ALU.add), r=["junk", "X%d" % m], w=["X%d" % m])

                for n in range(NT):
                    if stage in ('A', 'AC', 'all'):
                        stepA(n)
                    if n >= 1:
                        if stage in ('AC', 'all'):
                            stepC(n - 1)
                        if stage == 'all':
                            stepD(n - 1)
                if stage in ('AC', 'all'):
                    stepC(NT - 1)
                if stage == 'all':
                    stepD(NT - 1)
                tap("xmid%d" % l, X[:, 0, :], [128, 1024], ["X0"])

            def peer():
                P.barrier()
                cv = Carver()
                NWQ = 2
                wqr = [cv.get(1024) for _ in range(NWQ)]
                xn = cv.get(1024)
                h2T = cv.get(1024).rearrange("p (c t) -> p c t", t=128)
                h2 = [cv.get(1024) for _ in range(2)]
                pqT = cv.get(2048).rearrange("p (c t) -> p c t", t=128)
                sc = cv.get(256)
                scr = cv.get(256)
                vv = cv.get(32)
                ii = cv.get(32).bitcast(U32)
                iif = cv.get(32)
                cand = cv.get(256)
                cand2 = cv.get(256)
                cidx = cv.get(256)
                top = cv.get(16)
                pos = cv.get(16).bitcast(U32)
                posff = cv.get(16)
                junk2 = cv.get(256)
                ex = cv.get(16)
                gate = [cv.get(128) for _ in range(2)]
                eidxf = cv.get(128)
                eidxi = [cv.get(128).bitcast(I32) for _ in range(2)]
                actv = cv.get(128)
                ag = cv.get(128)
                wgt = cv.get(128)
                NU = NU_RING
                ub = [cv.get(1024) for _ in range(NU)]
                dg = [cv.get(128) for _ in range(3)]
                dg_n = [0]
                yacc = cv.get(1024)
                djunk = cv.get(1024)
                ss = st8[:, 0:1]
                rstd = st8[:, 1:2]
                negm = st8[:, 2:3]
                gs = st8[:, 3:4]
                wq_n = [0]
                ub_n = [0]

                def route(n):
                    p = n % 2
                    Xn = X[:, n, :]
                    xr = "X%d" % n
                    P.dve(lambda e: e.memset(ss, 0.0), w=["ss"])
                    P.act(lambda e: e.activation(out=xn, in_=Xn, func=AF.Square, accum_out=ss), r=[xr, "ss"], w=["xn", "ss"])
                    rstd_from_ss(ss, rstd, 1024, "ss", "rstd")
                    P.dve(lambda e: e.tensor_scalar(out=xn, in0=Xn, scalar1=rstd, scalar2=None, op0=ALU.mult), r=[xr, "rstd"], w=["xn"])
                    yield
                    for half in range(2):
                        for c4 in range(4):
                            kc = half * 4 + c4
                            P.pe(TR(ps[4][:, c4 * 128:(c4 + 1) * 128], xn[:, kc * 128:(kc + 1) * 128], ident[:]), r=["xn", "ident"], w=["ps4"])
                        for c4 in range(4):
                            kc = half * 4 + c4
                            P.dve((lambda kc, c4: lambda e: e.tensor_scalar(
                                out=h2T[:, kc, :], in0=ps[4][:, c4 * 128:(c4 + 1) * 128], scalar1=G2s[:, kc:kc + 1], scalar2=SH2[:, kc:kc + 1],
                                op0=ALU.mult, op1=ALU.add))(kc, c4), r=["ps4", "der", "modT"], w=["h2T"])
                        yield
                    for half in range(2):
                        for c4 in range(4):
                            kc = half * 4 + c4
                            P.pe(TR(ps[4][:, c4 * 128:(c4 + 1) * 128], h2T[:, kc, :], ident[:]), r=["h2T", "ident"], w=["ps4"])
                        P.act((lambda half: lambda e: e.copy(out=h2[p][:, half * 512:(half + 1) * 512], in_=ps[4][:, :]))(half), r=["ps4"], w=["h2_%d" % p])
                        yield
                    if n == 0:
                        tap("h2_%d" % l, h2[p], [128, 1024], ["h2_%d" % p])
                    for kc in range(8):
                        for hf in range(2):
                            s = wq_n[0] % NWQ
                            wq_n[0] += 1
                            P.dma((lambda s, kc, hf: lambda e: e.dma_start(out=wqr[s], in_=wq_d[l, kc * 128:(kc + 1) * 128, hf * 1024:(hf + 1) * 1024]))(s, kc, hf),
                                  w=["wq%d" % s], chan="wq%d" % s)
                            for j in range(8):
                                c = hf * 8 + j
                                b = c // 4
                                P.pe(MM(ps[b][:, (c % 4) * 128:(c % 4 + 1) * 128], wqr[s][:, j * 128:(j + 1) * 128], h2T[:, kc, :],
                                        kc == 0 and c % 4 == 0, kc == 7), r=["wq%d" % s, "h2T"], w=["ps%d" % b])
                            yield
                    for b in range(4):
                        src = ps[b][:, :].rearrange("p (c t) -> p c t", t=128)
                        if b % 2 == 0:
                            P.act((lambda b, src: lambda e: e.copy(out=pqT[:, b * 4:(b + 1) * 4, :], in_=src))(b, src), r=["ps%d" % b], w=["pqT%d" % b])
                        else:
                            P.dve((lambda b, src: lambda e: e.tensor_copy(out=pqT[:, b * 4:(b + 1) * 4, :], in_=src))(b, src), r=["ps%d" % b], w=["pqT%d" % b])
                    pq_all = ["pqT0", "pqT1", "pqT2", "pqT3"]
                    P.dve(lambda e: e.memset(eidxf, 0.0), w=["eidxf"])
                    yield
                    for h in range(8):
                        for sd in range(2):
                            P.pe(MM(ps[5][:, sd * 128:(sd + 1) * 128], pqT[:, 2 * h + sd, :], keysT[:, sd, :], True, True),
                                 r=pq_all + ["keysT%d" % sd], w=["ps5"])
                        P.act(lambda e: e.copy(out=sc, in_=ps[5][:, 0:256]), r=["ps5"], w=["sc"])
                        if n == 0 and h == 0:
                            tap("sc%d" % l, sc, [128, 256], ["sc"])
                        for sd in range(2):
                            src = sc[:, sd * 128:(sd + 1) * 128]
                            rep = scr[:, sd * 128:(sd + 1) * 128]
                            v = vv[:, sd * 16:(sd + 1) * 16]
                            ix = ii[:, sd * 16:(sd + 1) * 16]
                            P.dve((lambda v, src: lambda e: e.max(out=v[:, 0:8], in_=src))(v, src), r=["sc"], w=["vv"])
                            P.dve((lambda v, src, ix: lambda e: e.max_index(out=ix[:, 0:8], in_max=v[:, 0:8], in_values=src))(v, src, ix), r=["sc", "vv"], w=["ii"])
                            P.dve((lambda v, src, rep: lambda e: e.match_replace(out=rep, in_to_replace=v[:, 0:8], in_values=src, imm_value=-1e30))(v, src, rep),
                                  r=["sc", "vv"], w=["scr"])
                            P.dve((lambda v, rep: lambda e: e.max(out=v[:, 8:16], in_=rep))(v, rep), r=["scr"], w=["vv"])
                            P.dve((lambda v, rep, ix: lambda e: e.max_index(out=ix[:, 8:16], in_max=v[:, 8:16], in_values=rep))(v, rep, ix), r=["scr", "vv"], w=["ii"])
                        yield
                        P.dve(lambda e: e.tensor_copy(out=iif, in_=ii), r=["ii"], w=["iif"])
                        P.dve(lambda e: e.tensor_scalar(out=iif[:, 0:16], in0=iif[:, 0:16], scalar1=128.0, scalar2=None, op0=ALU.mult), r=["iif"], w=["iif"])
                        off = 0
                        for (a0, a1, nb) in ((0, 2, 16), (2, 4, 5), (4, 8, 3), (8, 16, 1)):
                            na = a1 - a0
                            if nb == 1:
                                P.dve((lambda off, a0, a1: lambda e: e.tensor_scalar(out=cand[:, off:off + (a1 - a0)], in0=vv[:, a0:a1], scalar1=vv[:, 16:17], scalar2=None, op0=ALU.add))(off, a0, a1),
                                      r=["vv"], w=["cand"])
                                P.dve((lambda off, a0, a1: lambda e: e.tensor_scalar(out=cidx[:, off:off + (a1 - a0)], in0=iif[:, a0:a1], scalar1=iif[:, 16:17], scalar2=None, op0=ALU.add))(off, a0, a1),
                                      r=["iif"], w=["cidx"])
                                off += na
                                continue
                            segc = cand[:, off:off + na * nb].rearrange("p (a b) -> p a b", b=nb)
                            segx = cidx[:, off:off + na * nb].rearrange("p (a b) -> p a b", b=nb)
                            P.dve((lambda segc, a0, a1, na, nb: lambda e: e.tensor_tensor(
                                out=segc, in0=vv[:, a0:a1].unsqueeze(2).broadcast_to([128, na, nb]),
                                in1=vv[:, 16:16 + nb].unsqueeze(1).broadcast_to([128, na, nb]), op=ALU.add))(segc, a0, a1, na, nb), r=["vv"], w=["cand"])
                            P.dve((lambda segx, a0, a1, na, nb: lambda e: e.tensor_tensor(
                                out=segx, in0=iif[:, a0:a1].unsqueeze(2).broadcast_to([128, na, nb]),
                                in1=iif[:, 16:16 + nb].unsqueeze(1).broadcast_to([128, na, nb]), op=ALU.add))(segx, a0, a1, na, nb), r=["iif"], w=["cidx"])
                            off += na * nb
                        NC = off
                        candv = cand[:, 0:NC]
                        cand2v = cand2[:, 0:NC]
                        P.dve(lambda e: e.max(out=top[:, 0:8], in_=candv), r=["cand"], w=["top"])
                        P.dve(lambda e: e.max_index(out=pos[:, 0:8], in_max=top[:, 0:8], in_values=candv), r=["cand", "top"], w=["pos"])
                        P.dve(lambda e: e.match_replace(out=cand2v, in_to_replace=top[:, 0:8], in_values=candv, imm_value=-1e30), r=["cand", "top"], w=["cand2"])
                        P.dve(lambda e: e.max(out=top[:, 8:16], in_=cand2v), r=["cand2"], w=["top"])
                        P.dve(lambda e: e.max_index(out=pos[:, 8:16], in_max=top[:, 8:16], in_values=cand2v), r=["cand2", "top"], w=["pos"])
                        yield
                        P.dve(lambda e: e.tensor_scalar(out=negm, in0=top[:, 0:1], scalar1=-1.0, scalar2=None, op0=ALU.mult), r=["top"], w=["negm"])
                        P.dve(lambda e: e.memset(gs, 0.0), w=["gs"])
                        P.act(lambda e: e.activation(out=ex, in_=top, func=AF.Exp, bias=negm, accum_out=gs), r=["top", "negm", "gs"], w=["ex", "gs"])
                        P.dve(lambda e: e.reciprocal(out=gs, in_=gs), r=["gs"], w=["gs"])
                        P.dve((lambda h: lambda e: e.tensor_scalar(out=gate[p][:, h * 16:(h + 1) * 16], in0=ex, scalar1=gs, scalar2=None, op0=ALU.mult))(h),
                              r=["ex", "gs"], w=["gate_%d" % p])
                        P.dve(lambda e: e.tensor_copy(out=posff, in_=pos), r=["pos"], w=["posff"])
                        for k in range(16):
                            P.dve((lambda h, k: lambda e: e.scalar_tensor_tensor(
                                out=junk2[:, 0:NC], in0=iota[:, 0:NC], scalar=posff[:, k:k + 1], in1=cidx[:, 0:NC], op0=ALU.is_equal, op1=ALU.mult,
                                accum_out=eidxf[:, h * 16 + k:h * 16 + k + 1]))(h, k), r=["iota", "posff", "cidx", "eidxf"], w=["junk2", "eidxf"])
                            if k % 8 == 7:
                                yield
                    P.dve(lambda e: e.tensor_copy(out=eidxi[p], in_=eidxf), r=["eidxf"], w=["eidxi_%d" % p])
                    if n == 0:
                        tap("eidx%d" % l, eidxi[p], [128, 128], ["eidxi_%d" % p], dt=I32)
                        tap("gate%d" % l, gate[p], [128, 128], ["gate_%d" % p])

                def gather(n, nxt):
                    p = n % 2
                    Xn = X[:, n, :]
                    xr = "X%d" % n
                    if not do_gather:
                        for _ in nxt:
                            pass
                        return
                    P.dve(lambda e: e.memset(actv, 0.0), w=["actv"])
                    for hk in range(128):
                        s = ub_n[0] % NU
                        ub_n[0] += 1
                        P.dma((lambda s, hk: lambda e: e.indirect_dma_start(
                            out=ub[s], out_offset=None, in_=pu_d[l], in_offset=bass.IndirectOffsetOnAxis(ap=eidxi[p][:, hk:hk + 1], axis=0)))(s, hk),
                            r=["eidxi_%d" % p], w=["ub%d" % s], chan="ub%d" % s, eng="pool")
                        P.dve((lambda s, hk: lambda e: e.scalar_tensor_tensor(
                            out=djunk, in0=ub[s], scalar=1.0, in1=h2[p], op0=ALU.mult, op1=ALU.mult, accum_out=actv[:, hk:hk + 1]))(s, hk),
                            r=["ub%d" % s, "h2_%d" % p, "actv"], w=["djunk", "actv"])
                        if hk % 4 == 3:
                            next(nxt, None)
                    P.act(lambda e: e.activation(out=ag, in_=actv, func=AF.Gelu), r=["actv"], w=["ag"])
                    P.dve(lambda e: e.tensor_tensor(out=wgt, in0=ag, in1=gate[p], op=ALU.mult), r=["ag", "gate_%d" % p], w=["wgt"])
                    if PE_EVERY == 34:
                        pe_hks = [hk for hk in range(128) if hk % 4 != 0]
                    elif PE_EVERY == 23:
                        pe_hks = [hk for hk in range(128) if hk % 3 != 0]
                    else:
                        pe_hks = [hk for hk in range(128) if PE_EVERY and hk % PE_EVERY == PE_EVERY - 1]
                    first_dve = True
                    for hk in range(128):
                        s = ub_n[0] % NU
                        ub_n[0] += 1
                        P.dma((lambda s, hk: lambda e: e.indirect_dma_start(
                            out=ub[s], out_offset=None, in_=pv_d[l], in_offset=bass.IndirectOffsetOnAxis(ap=eidxi[p][:, hk:hk + 1], axis=0)))(s, hk),
                            r=["eidxi_%d" % p], w=["ub%d" % s], chan="ub%d" % s, eng="pool")
                        if hk in pe_hks:
                            d = dg_n[0] % 3
                            dg_n[0] += 1
                            P.act((lambda d, hk: lambda e: e.activation(out=dg[d], in_=ident[:], func=AF.Copy, scale=wgt[:, hk:hk + 1]))(d, hk),
                                  r=["ident", "wgt"], w=["dg%d" % d])
                            for b in range(2):
                                P.pe(MM(ps[6 + b][:, :], dg[d], ub[s][:, b * 512:(b + 1) * 512], hk == pe_hks[0], hk == pe_hks[-1]),
                                     r=["dg%d" % d, "ub%d" % s], w=["ps%d" % (6 + b)])
                        elif first_dve:
                            first_dve = False
                            P.dve((lambda s, hk: lambda e: e.tensor_scalar(out=yacc, in0=ub[s], scalar1=wgt[:, hk:hk + 1], scalar2=None, op0=ALU.mult))(s, hk),
                                  r=["ub%d" % s, "wgt"], w=["yacc"])
                        else:
                            P.dve((lambda s, hk: lambda e: e.scalar_tensor_tensor(
                                out=yacc, in0=ub[s], scalar=wgt[:, hk:hk + 1], in1=yacc, op0=ALU.mult, op1=ALU.add))(s, hk),
                                r=["ub%d" % s, "wgt", "yacc"], w=["yacc"])
                        if hk % 4 == 3:
                            next(nxt, None)
                    for _ in nxt:
                        pass
                    if n == 0:
                        tap("wgt%d" % l, wgt, [128, 128], ["wgt"])
                    if PE_EVERY:
                        for b in range(2):
                            P.dve((lambda b: lambda e: e.tensor_tensor(out=yacc[:, b * 512:(b + 1) * 512], in0=ps[6 + b][:, :], in1=yacc[:, b * 512:(b + 1) * 512], op=ALU.add))(b),
                                  r=["ps%d" % (6 + b), "yacc"], w=["yacc"])
                    P.dve(lambda e: e.tensor_tensor(out=yacc, in0=yacc, in1=GATE2[:], op=ALU.mult), r=["yacc", "GATE1"], w=["yacc"])
                    P.dve(lambda e: e.tensor_tensor(out=Xn, in0=Xn, in1=yacc, op=ALU.add), r=["yacc", xr], w=[xr])

                if do_peer:
                    for _ in route(0):
                        pass
                    for n in range(NT):
                        nxt = route(n + 1) if n + 1 < NT else iter(())
                        gather(n, nxt)

            prologue()
            mixing()
            peer()

        for l in range(L):
            layer(l)

        for n in range(NT):
            P.dma((lambda n: lambda e: e.dma_start(out=y_d[n * 128:(n + 1) * 128, :], in_=X[:, n, :]))(n), r=["X%d" % n], chan="out")
        fw = ["out"] + (["tap"] if tap_out else [])
        counts = P.emit(final_wait_chans=fw)
    return nc, counts


def host_consts(NT):
    S = NT * 128
    ar = np.arange(128)
    mlo = (ar[None, :] <= ar[:, None]).astype(np.float32)
    mhi = (ar[:, None] <= ar[None, :]).astype(np.float32)
    invcnt = np.zeros((128, 6, 128), np.float32)
    for j in range(2):
        for e in range(3):
            t = ar + (0 if e == 0 else (S - 128 if e == 2 else 256))
            for p in range(128):
                w = (2, 4, 8, 16)[2 * j + (p // 64)]
                if e == 1:
                    cnt = np.full(128, w)
                else:
                    cnt = np.minimum(t + w // 2, S) - np.maximum(t - w // 2, 0)
                invcnt[p, j * 3 + e, :] = 1.0 / cnt.astype(np.float32)
    invf = (500000.0 ** (-np.arange(0, 16, 2, dtype=np.float32) / 16)).astype(np.float32)
    return dict(
        ident=np.eye(128, dtype=np.float32), ones=np.ones((128, 128), np.float32), mlo=mlo, mhi=mhi,
        iota=np.broadcast_to(np.arange(256, dtype=np.float32), (128, 256)).copy(),
        invcnt=invcnt, invfreq=np.broadcast_to(invf, (128, 8)).copy())


def host_weights(L, norm1_g, norm2_g, w_ada, b_ada, w_in, q_norm_g, k_norm_g, attn_sink, conv_w, pool_w,
                 pool_scale, w_out, peer_wq, peer_keys, peer_u, peer_v):
    f = lambda a: np.ascontiguousarray(np.asarray(a, dtype=np.float32))
    d = {}
    d["n1gT"] = f(np.asarray(norm1_g).reshape(L, 8, 128).transpose(0, 2, 1))
    d["n2gT"] = f(np.asarray(norm2_g).reshape(L, 8, 128).transpose(0, 2, 1))
    d["badaT"] = f(np.asarray(b_ada).reshape(L, 48, 128).transpose(0, 2, 1))
    d["wada"] = f(w_ada)
    d["win"] = f(w_in)
    d["wout"] = f(w_out)
    d["wq"] = f(peer_wq)
    gq = np.concatenate([np.tile(np.asarray(q_norm_g), (1, 8)), np.tile(np.asarray(k_norm_g), (1, 2))], axis=1)
    d["gqk"] = f(np.broadcast_to(gq[:, None, :], (L, 128, 640)))
    d["sink"] = f(np.broadcast_to(np.asarray(attn_sink)[:, None, :], (L, 128, 8)))
    cw = np.asarray(conv_w).reshape(L, 3, 2, 128).transpose(0, 3, 2, 1)
    d["convw"] = f(cw.reshape(L, 128, 6))
    pw = np.zeros((L, 2, 128, 128), np.float32)
    pwa = np.asarray(pool_w)
    for j in range(2):
        pw[:, j, 0:64, 0:64] = pwa[:, 2 * j]
        pw[:, j, 64:128, 64:128] = pwa[:, 2 * j + 1]
    d["poolw"] = pw
    d["pscale"] = f(np.asarray(pool_scale).reshape(L, 2, 128).transpose(0, 2, 1))
    d["keysT"] = f(np.asarray(peer_keys).transpose(0, 1, 3, 2))
    pu = np.asarray(peer_u, dtype=np.float32)
    pv = np.asarray(peer_v, dtype=np.float32)
    for l in range(L):
        d["pu%d" % l] = np.ascontiguousarray(pu[l])
        d["pv%d" % l] = np.ascontiguousarray(pv[l])
    return d


_CACHE = {}


def kernel(x, c, positions, norm1_g, norm2_g, w_ada, b_ada, w_in, q_norm_g, k_norm_g, attn_sink, conv_w,
           pool_w, pool_scale, w_out, peer_wq, peer_keys, peer_u, peer_v):
    x = np.asarray(x, dtype=np.float32)
    c = np.asarray(c, dtype=np.float32)
    positions = np.asarray(positions).astype(np.int32)
    B, S, D = x.shape
    L = np.asarray(w_in).shape[0]
    NT = S // 128
    key = (NT, L)
    if key not in _CACHE:
        _CACHE[key] = build(NT, L)[0]
    nc = _CACHE[key]
    shared = host_weights(L, norm1_g, norm2_g, w_ada, b_ada, w_in, q_norm_g, k_norm_g, attn_sink, conv_w,
                          pool_w, pool_scale, w_out, peer_wq, peer_keys, peer_u, peer_v)
    shared.update(host_consts(NT))
    in_maps = []
    for b in range(B):
        m = dict(shared)
        m["x"] = np.ascontiguousarray(x[b])
        m["cT"] = np.ascontiguousarray(c[b].reshape(8, 128).T)
        m["posT"] = np.ascontiguousarray(positions[b].reshape(NT, 128).T)
        in_maps.append(m)
    res = run_bass_kernel_spmd(nc, in_maps, core_ids=list(range(B)))
    return np.stack([np.asarray(r["y"]) for r in res.results], axis=0).astype(np.float32)
```

```python
import math
from contextlib import ExitStack

import numpy as np
import concourse.bass as bass
import concourse.mybir as mybir
from concourse.bass_utils import run_bass_kernel_spmd

F32 = mybir.dt.float32
U32 = mybir.dt.uint32
I32 = mybir.dt.int32
ALU = mybir.AluOpType
AF = mybir.ActivationFunctionType
AX = mybir.AxisListType

ENGINES = ["pe", "act", "dve", "pool", "sp"]
EPS = 1e-6
PI = math.pi


class Prog:
    def __init__(self, nc):
        self.nc = nc
        self.ops = []
        self.last_w = {}
        self.readers = {}
        self.last_eng = {}
        self.last_chan = {}
        self.pending = {}
        self.burst = set()

    def add(self, eng, fn, r=(), w=(), dma=False, chan=None):
        oid = len(self.ops)
        deps = set()
        for k in r:
            if k in self.last_w:
                deps.add(self.last_w[k])
        for k in w:
            if k in self.last_w:
                deps.add(self.last_w[k])
            for q in self.readers.get(k, ()):
                deps.add(q)
        if eng in self.pending:
            deps |= self.pending.pop(eng)
        for k in r:
            self.readers.setdefault(k, []).append(oid)
        for k in w:
            self.last_w[k] = oid
            self.readers[k] = []
        deps.discard(oid)
        self.ops.append(dict(eng=eng, fn=fn, deps=sorted(deps), dma=dma, chan=chan))
        if dma:
            self.last_chan[chan] = oid
        else:
            self.last_eng[eng] = oid
        return oid

    def barrier(self):
        b = set(self.last_eng.values()) | set(self.last_chan.values())
        for e in ENGINES:
            self.pending[e] = set(b) | self.pending.get(e, set())

    def pe(self, fn, r=(), w=()):
        return self.add("pe", fn, r, w)

    def act(self, fn, r=(), w=()):
        return self.add("act", fn, r, w)

    def dve(self, fn, r=(), w=()):
        return self.add("dve", fn, r, w)

    def pool(self, fn, r=(), w=()):
        return self.add("pool", fn, r, w)

    def dma(self, fn, r=(), w=(), chan=None, eng="sp"):
        return self.add(eng, fn, r, w, dma=True, chan=chan)

    def emit(self, final_wait_chans=()):
        nc = self.nc
        ops = self.ops
        eng_count = {e: 0 for e in ENGINES}
        chan_count = {}
        for op in ops:
            if op["dma"]:
                c = op["chan"]
                chan_count[c] = chan_count.get(c, 0) + 16
                op["sig"] = ("c", c, chan_count[c])
                op["inc"] = True
            else:
                eng_count[op["eng"]] += 1
                op["sig"] = ("e", op["eng"], eng_count[op["eng"]])
        for op in ops:
            if op["dma"] and op["chan"] in self.burst:
                op["sig"] = ("c", op["chan"], chan_count[op["chan"]])
        with ExitStack() as st:
            sems = {}
            for e in ENGINES:
                sems[("e", e)] = st.enter_context(nc.semaphore("s_" + e))
            for c in chan_count:
                sems[("c", c)] = st.enter_context(nc.semaphore("c_" + str(c)))
            block = st.enter_context(nc.Block())

            def make(engname):
                def body(eng):
                    waited = {}
                    for op in ops:
                        if op["eng"] != engname:
                            continue
                        for d in op["deps"]:
                            kind, key, val = ops[d]["sig"]
                            if kind == "e" and key == "pe" and engname == "pe":
                                continue
                            sk = (kind, key)
                            if waited.get(sk, 0) >= val:
                                continue
                            eng.wait_ge(sems[sk], val)
                            waited[sk] = val
                        ins = op["fn"](eng)
                        kind, key, val = op["sig"]
                        ins.then_inc(sems[(kind, key)], 16 if kind == "c" else 1)
                    if engname == "sp":
                        for c in final_wait_chans:
                            eng.wait_ge(sems[("c", c)], chan_count[c])
                return body

            block.tensor(make("pe"))
            block.scalar(make("act"))
            block.vector(make("dve"))
            block.gpsimd(make("pool"))
            block.sync(make("sp"))
        return eng_count, chan_count


def build(NT, L, taps=(), do_peer=True, do_gather=True, stage='all', cut=99, PE_EVERY=23, NU_RING=16):
    S = NT * 128
    nc = bass.Bass("TRN2", target_bir_lowering=False)
    P = Prog(nc)
    P.burst = {"cst"} | {"lw%d" % l for l in range(L)}

    def din(name, shape, dt=F32):
        return nc.dram_tensor(name, shape, dt, kind="ExternalInput").ap()

    x_d = din("x", [S, 1024])
    cT_d = din("cT", [128, 8])
    pos_d = din("posT", [128, NT], I32)
    n1g_d = din("n1gT", [L, 128, 8])
    n2g_d = din("n2gT", [L, 128, 8])
    bada_d = din("badaT", [L, 128, 48])
    wada_d = din("wada", [L, 1024, 6144])
    win_d = din("win", [L, 1024, 1792])
    wout_d = din("wout", [L, 1024, 1024])
    wq_d = din("wq", [L, 1024, 2048])
    gqk_d = din("gqk", [L, 128, 640])
    sink_d = din("sink", [L, 128, 8])
    sink2_d = din("sink2", [L, 128, 4])
    convw_d = din("convw", [L, 128, 6])
    poolw_d = din("poolw", [L, 2, 128, 128])
    pscale_d = din("pscale", [L, 128, 2])
    keysT_d = din("keysT", [L, 2, 128, 128])
    pu_d = [din("pu%d" % l, [16384, 1024]) for l in range(L)]
    pv_d = [din("pv%d" % l, [16384, 1024]) for l in range(L)]
    ident_d = din("ident", [128, 128])
    ones_d = din("ones", [128, 128])
    mlo_d = din("mlo", [128, 128])
    mhi_d = din("mhi", [128, 128])
    iota_d = din("iota", [128, 256])
    invcnt_d = din("invcnt", [128, 6, 128])
    invfreq_d = din("invfreq", [128, 8])
    onesg_d = din("onesg", [128, 2, 128])
    y_d = nc.dram_tensor("y", [S, 1024], F32, kind="ExternalOutput").ap()
    tap_out = {}

    with ExitStack() as st:
        def sb(name, shape, dt=F32):
            return st.enter_context(nc.sbuf_tensor(name, shape, dt))

        X = sb("X", [128, NT, 1024])
        ident = sb("ident_s", [128, 128])
        ones = sb("ones_s", [128, 128])
        mlo = sb("mlo_s", [128, 128])
        mhi = sb("mhi_s", [128, 128])
        iota = sb("iota_s", [128, 256])
        invcnt = sb("invcnt_s", [128, 6, 128])
        invfreq = sb("invfreq_s", [128, 8])
        cT = sb("cT_s", [128, 8])
        cact = sb("cact", [128, 8])
        posi = sb("posi", [128, NT], I32)
        posf = sb("posf", [128, NT])
        ang = sb("ang", [128, NT, 8])
        ang2 = sb("ang2", [128, NT, 8])
        angi = sb("angi", [128, NT, 8], I32)
        cosT = sb("cosT", [128, NT, 8])
        sinT = sb("sinT", [128, NT, 8])
        n1g = sb("n1g", [128, 8])
        n2g = sb("n2g", [128, 8])
        bada = sb("bada", [128, 48])
        modT = sb("modT", [128, 48])
        der = sb("der", [128, 32])
        GATE1 = sb("GATE1", [128, 1024])
        GATE2 = sb("GATE2", [128, 1024])
        gqk = sb("gqk_s", [128, 640])
        sinkt = sb("sinkt", [128, 8])
        esink = sb("esink", [128, 8])
        sink2t = sb("sink2t", [128, 4])
        esink2 = sb("esink2", [128, 4])
        onesg = sb("onesg_s", [128, 2, 128])
        convw = sb("convw_s", [128, 6])
        poolw = sb("poolw_s", [128, 2, 128])
        pscale = sb("pscale_s", [128, 2])
        keysT = sb("keysT_s", [128, 2, 128])
        st8 = sb("st8", [128, 64])
        AW = 29800
        arena = sb("arena", [128, AW])
        ps = [st.enter_context(nc.psum_tensor("ps%d" % i, [128, 512], F32)) for i in range(8)]

        class Carver:
            def __init__(self):
                self.off = 0

            def get(self, words):
                a = arena[:, self.off:self.off + words]
                self.off += words
                assert self.off <= AW, self.off
                return a

        def tap(name, ap, shape, r, dt=F32):
            if name not in taps:
                return
            t = nc.dram_tensor("tap_" + name, shape, dt, kind="ExternalOutput").ap()
            tap_out[name] = t
            P.dma(lambda e: e.dma_start(out=t, in_=ap), r=r, chan="tap")

        def ld(dst, src, name, chan="cst"):
            P.dma(lambda e: e.dma_start(out=dst, in_=src), w=[name], chan=chan)

        ld(ident[:], ident_d, "ident")
        ld(ones[:], ones_d, "ones")
        ld(mlo[:], mlo_d, "mlo")
        ld(mhi[:], mhi_d, "mhi")
        ld(iota[:], iota_d, "iota")
        ld(invcnt[:], invcnt_d, "invcnt")
        ld(invfreq[:], invfreq_d, "invfreq")
        ld(onesg[:], onesg_d, "onesg")
        ld(cT[:], cT_d, "cT")
        ld(posi[:], pos_d, "posi")
        for n in range(NT):
            ld(X[:, n, :], x_d[n * 128:(n + 1) * 128, :], "X%d" % n, chan="xin%d" % n)

        P.act(lambda e: e.activation(out=cact[:], in_=cT[:], func=AF.Silu), r=["cT"], w=["cact"])
        P.dve(lambda e: e.tensor_copy(out=posf[:], in_=posi[:]), r=["posi"], w=["posf"])
        P.dve(lambda e: e.tensor_tensor(out=ang[:], in0=posf[:].unsqueeze(2).broadcast_to([128, NT, 8]),
                                        in1=invfreq[:].unsqueeze(1).broadcast_to([128, NT, 8]), op=ALU.mult),
              r=["posf", "invfreq"], w=["ang"])
        C1 = 6.28125
        C2 = 2 * PI - C1
        P.dve(lambda e: e.tensor_scalar(out=ang2[:], in0=ang[:], scalar1=1.0 / (2 * PI), scalar2=None, op0=ALU.mult), r=["ang"], w=["ang2"])
        P.dve(lambda e: e.tensor_copy(out=angi[:], in_=ang2[:]), r=["ang2"], w=["angi"])
        P.dve(lambda e: e.tensor_copy(out=ang2[:], in_=angi[:]), r=["angi"], w=["ang2"])
        P.dve(lambda e: e.scalar_tensor_tensor(out=ang[:], in0=ang2[:], scalar=-C1, in1=ang[:], op0=ALU.mult, op1=ALU.add), r=["ang2", "ang"], w=["ang"])
        P.dve(lambda e: e.scalar_tensor_tensor(out=ang[:], in0=ang2[:], scalar=-C2, in1=ang[:], op0=ALU.mult, op1=ALU.add), r=["ang2", "ang"], w=["ang"])

        def wrap():
            P.dve(lambda e: e.tensor_scalar(out=ang2[:], in0=ang[:], scalar1=PI, scalar2=-2 * PI, op0=ALU.is_gt, op1=ALU.mult), r=["ang", "sinT", "cosT"], w=["ang2"])
            P.dve(lambda e: e.tensor_tensor(out=ang[:], in0=ang[:], in1=ang2[:], op=ALU.add), r=["ang", "ang2"], w=["ang"])
            P.dve(lambda e: e.tensor_scalar(out=ang[:], in0=ang[:], scalar1=-PI, scalar2=PI, op0=ALU.max, op1=ALU.min), r=["ang"], w=["ang"])

        wrap()
        P.act(lambda e: e.activation(out=sinT[:], in_=ang[:], func=AF.Sin), r=["ang"], w=["sinT"])
        P.dve(lambda e: e.tensor_scalar(out=ang[:], in0=ang[:], scalar1=0.5 * PI, scalar2=None, op0=ALU.add), r=["ang", "sinT"], w=["ang"])
        wrap()
        P.act(lambda e: e.activation(out=cosT[:], in_=ang[:], func=AF.Sin), r=["ang"], w=["cosT"])
        tap("cos", cosT[:], [128, NT, 8], ["cosT"])
        tap("sin", sinT[:], [128, NT, 8], ["sinT"])

        def rstd_from_ss(ss_ap, out_ap, n, rname, wname):
            P.dve(lambda e: e.tensor_scalar(out=out_ap, in0=ss_ap, scalar1=1.0 / n, scalar2=EPS, op0=ALU.mult, op1=ALU.add),
                  r=[rname], w=[wname])
            P.act(lambda e: e.activation(out=out_ap, in_=out_ap, func=AF.Sqrt), r=[wname], w=[wname])
            P.dve(lambda e: e.reciprocal(out=out_ap, in_=out_ap), r=[wname], w=[wname])

        def MM(out, lhsT, rhs, start, stop):
            return lambda e: e.matmul(out, lhsT=lhsT, rhs=rhs, start=start, stop=stop, skip_group_check=True)

        def TR(out, in_, idn):
            return lambda e: e.transpose(out=out, in_=in_, identity=idn)

        def layer(l):
            G1s, SH1 = der[:, 0:8], modT[:, 0:8]
            G2s, SH2 = der[:, 8:16], modT[:, 24:32]
            def prologue():
                P.barrier()
                cv = Carver()
                wad = [cv.get(3072), cv.get(3072)]
                Dt = cv.get(128)
                ld(n1g[:], n1g_d[l], "n1g", chan="lw%d" % l)
                ld(n2g[:], n2g_d[l], "n2g", chan="lw%d" % l)
                ld(bada[:], bada_d[l], "bada", chan="lw%d" % l)
                ld(gqk[:], gqk_d[l], "gqk", chan="lw%d" % l)
                ld(sinkt[:], sink_d[l], "sinkt", chan="lw%d" % l)
                ld(sink2t[:], sink2_d[l], "sink2t", chan="lw%d" % l)
                ld(convw[:], convw_d[l], "convw", chan="lw%d" % l)
                for j in range(2):
                    ld(poolw[:, j, :], poolw_d[l, j], "poolw%d" % j, chan="lw%d" % l)
                    ld(keysT[:, j, :], keysT_d[l, j], "keysT%d" % j, chan="lw%d" % l)
                ld(pscale[:], pscale_d[l], "pscale", chan="lw%d" % l)
                first = True
                pi = 0
                for kc in range(8):
                    for half in range(2):
                        s = pi % 2
                        pi += 1
                        P.dma((lambda s, kc, half: lambda e: e.dma_start(
                            out=wad[s], in_=wada_d[l, kc * 128:(kc + 1) * 128, half * 3072:(half + 1) * 3072]))(s, kc, half),
                            w=["wad%d" % s], chan="wad%d" % s)
                        for jj in range(24):
                            j = half * 24 + jj
                            P.pe(MM(ps[4][:, j:j + 1], wad[s][:, jj * 128:(jj + 1) * 128], cact[:, kc:kc + 1], first, kc == 7),
                                 r=["wad%d" % s, "cact"], w=["ps4"])
                            first = False
                P.dve(lambda e: e.tensor_tensor(out=modT[:], in0=ps[4][:, 0:48], in1=bada[:], op=ALU.add),
                      r=["ps4", "bada"], w=["modT"])
                tap("modT%d" % l, modT[:], [128, 48], ["modT"])
                P.dve(lambda e: e.scalar_tensor_tensor(out=der[:, 0:8], in0=modT[:, 8:16], scalar=1.0, in1=n1g[:], op0=ALU.add, op1=ALU.mult),
                      r=["modT", "n1g"], w=["der"])
                P.dve(lambda e: e.scalar_tensor_tensor(out=der[:, 8:16], in0=modT[:, 32:40], scalar=1.0, in1=n2g[:], op0=ALU.add, op1=ALU.mult),
                      r=["modT", "n2g", "der"], w=["der"])
                for gi, (GT, c0) in enumerate(((GATE1, 16), (GATE2, 40))):
                    for ch in range(8):
                        P.dve((lambda ch, c0: lambda e: e.tensor_scalar(out=Dt, in0=ident[:], scalar1=modT[:, c0 + ch:c0 + ch + 1], scalar2=None, op0=ALU.mult))(ch, c0),
                              r=["ident", "modT"], w=["Dt"])
                        P.pe(MM(ps[ch // 4][:, (ch % 4) * 128:(ch % 4 + 1) * 128], ones[:], Dt, True, True),
                             r=["ones", "Dt"], w=["ps%d" % (ch // 4)])
                    for b in range(2):
                        P.act((lambda b, GT: lambda e: e.copy(out=GT[:, b * 512:(b + 1) * 512], in_=ps[b][:]))(b, GT),
                              r=["ps%d" % b], w=["GATE%d" % gi])
                tap("gate1_%d" % l, GATE1[:], [128, 1024], ["GATE0"])
                P.act(lambda e: e.activation(out=esink[:], in_=sinkt[:], func=AF.Exp), r=["sinkt"], w=["esink"])
                P.act(lambda e: e.activation(out=esink2[:], in_=sink2t[:], func=AF.Exp), r=["sink2t"], w=["esink2"])

            def mixing():
                P.barrier()
                cv = Carver()
                wi = [cv.get(1792) for _ in range(2)]
                wo = [cv.get(1024) for _ in range(2)]
                xn = cv.get(1024)
                hT = cv.get(1024).rearrange("p (c t) -> p c t", t=128)
                zsb = cv.get(1792)
                sq = cv.get(640)
                qn = cv.get(640)
                rt = [cv.get(80).rearrange("p (h d) -> p h d", d=8) for _ in range(6)]
                qT = [cv.get(1024).rearrange("p (h t) -> p h t", t=128) for _ in range(2)]
                kT = [cv.get(256).rearrange("p (g t) -> p g t", t=128) for _ in range(4)]
                VP = [cv.get(256).rearrange("p (g c) -> p g c", c=128) for _ in range(4)]
                Wb = [cv.get(1152).rearrange("p (c t) -> p c t", t=144) for _ in range(2)]
                tail = [cv.get(64).rearrange("p (c t) -> p c t", t=8) for _ in range(2)]
                cx = cv.get(144)
                cacc = cv.get(128)
                Alv = [cv.get(144) for _ in range(4)]
                ptmp = cv.get(128)
                pooled = cv.get(128)
                cpT = [cv.get(512).rearrange("p (c t) -> p c t", t=128) for _ in range(2)]
                PT = [cv.get(512) for _ in range(2)]
                den = cv.get(512)
                attnT = cv.get(1024).rearrange("p (h t) -> p h t", t=128)
                otmp = cv.get(1024)
                junk = otmp
                ss = st8[:, 0:1]
                rstd = st8[:, 1:2]
                ssq = st8[:, 8:18]
                rsq = st8[:, 24:34]
                wi_n = [0]
                wo_n = [0]
                for i4 in range(4):
                    P.dve((lambda i4: lambda e: e.memset(kT[i4][64:128, :, :], 0.0))(i4), w=["kT%d" % i4])
                    P.dve((lambda i4: lambda e: e.memset(VP[i4][:, 0, 64:128], 0.0))(i4), w=["VP%dz" % i4])
                    P.dve((lambda i4: lambda e: e.memset(VP[i4][:, 1, 0:64], 0.0))(i4), w=["VP%dz" % i4])
                for i2 in range(2):
                    P.dve((lambda i2: lambda e: e.memset(qT[i2][64:128, :, :], 0.0))(i2), w=["qT%d" % i2])

                def stepA(n):
                    Xn = X[:, n, :]
                    xr = "X%d" % n
                    P.dve(lambda e: e.memset(ss, 0.0), w=["ss"])
                    P.act(lambda e: e.activation(out=junk, in_=Xn, func=AF.Square, accum_out=ss), r=[xr, "ss"], w=["junk", "ss"])
                    rstd_from_ss(ss, rstd, 1024, "ss", "rstd")
                    P.dve(lambda e: e.tensor_scalar(out=xn, in0=Xn, scalar1=rstd, scalar2=None, op0=ALU.mult), r=[xr, "rstd"], w=["xn"])
                    if n == 0:
                        tap("xn%d" % l, xn, [128, 1024], ["xn"])
                        tap("st8_%d" % l, st8[:], [128, 64], ["rstd", "ss"])
                    for half in range(2):
                        for c4 in range(4):
                            kc = half * 4 + c4
                            P.pe(TR(ps[4][:, c4 * 128:(c4 + 1) * 128], xn[:, kc * 128:(kc + 1) * 128], ident[:]),
                                 r=["xn", "ident"], w=["ps4"])
                        for c4 in range(4):
                            kc = half * 4 + c4
                            P.dve((lambda kc, c4: lambda e: e.tensor_scalar(
                                out=hT[:, kc, :], in0=ps[4][:, c4 * 128:(c4 + 1) * 128], scalar1=G1s[:, kc:kc + 1], scalar2=SH1[:, kc:kc + 1],
                                op0=ALU.mult, op1=ALU.add))(kc, c4), r=["ps4", "der", "modT"], w=["hT"])
                    if n == 0:
                        tap("hT%d" % l, hT, [128, 8, 128], ["hT"])
                    if cut <= 1:
                        return
                    widths = [512, 512, 512, 256]
                    for kc in range(8):
                        s = wi_n[0] % 2
                        wi_n[0] += 1
                        P.dma((lambda s, kc: lambda e: e.dma_start(out=wi[s], in_=win_d[l, kc * 128:(kc + 1) * 128, :]))(s, kc),
                              w=["wi%d" % s], chan="wi%d" % s)
                        for b in range(4):
                            P.pe(MM(ps[b][:, 0:widths[b]], hT[:, kc, :], wi[s][:, b * 512:b * 512 + widths[b]], kc == 0, kc == 7),
                                 r=["hT", "wi%d" % s], w=["ps%d" % b])
                    for b in range(4):
                        if b % 2 == 0:
                            P.act((lambda b: lambda e: e.copy(out=zsb[:, b * 512:b * 512 + widths[b]], in_=ps[b][:, 0:widths[b]]))(b),
                                  r=["ps%d" % b], w=["zsb%d" % b])
                        else:
                            P.dve((lambda b: lambda e: e.tensor_copy(out=zsb[:, b * 512:b * 512 + widths[b]], in_=ps[b][:, 0:widths[b]]))(b),
                                  r=["ps%d" % b], w=["zsb%d" % b])
                    zall = ["zsb0", "zsb1", "zsb2", "zsb3"]
                    if cut <= 2:
                        return
                    if n == 0:
                        tap("z%d" % l, zsb, [128, 1792], zall)
                    zqk = zsb[:, 0:640]
                    P.dve(lambda e: e.tensor_tensor(out=sq, in0=zqk, in1=zqk, op=ALU.mult), r=zall, w=["sq"])
                    P.dve(lambda e: e.tensor_reduce(out=ssq, in_=sq.rearrange("p (h d) -> p h d", d=64), axis=AX.X, op=ALU.add),
                          r=["sq"], w=["ssq"])
                    rstd_from_ss(ssq, rsq, 64, "ssq", "rsq")
                    qn3 = qn.rearrange("p (h d) -> p h d", d=64)
                    P.dve(lambda e: e.tensor_tensor(out=qn3, in0=zqk.rearrange("p (h d) -> p h d", d=64),
                                                    in1=rsq.unsqueeze(2).broadcast_to([128, 10, 64]), op=ALU.mult),
                          r=zall + ["rsq"], w=["qn"])
                    P.dve(lambda e: e.tensor_tensor(out=qn, in0=qn, in1=gqk[:], op=ALU.mult), r=["qn", "gqk"], w=["qn"])
                    cosb = cosT[:, n, :].unsqueeze(1).broadcast_to([128, 10, 8])
                    if cut <= 3:
                        return
                    sinb = sinT[:, n, :].unsqueeze(1).broadcast_to([128, 10, 8])
                    t1 = qn3[:, :, 0:8]
                    t2 = qn3[:, :, 8:16]
                    P.dve(lambda e: e.tensor_tensor(out=rt[0], in0=t1, in1=cosb, op=ALU.mult), r=["qn", "cosT"], w=["rt0"])
                    P.dve(lambda e: e.tensor_tensor(out=rt[1], in0=t2, in1=sinb, op=ALU.mult), r=["qn", "sinT"], w=["rt1"])
                    P.dve(lambda e: e.tensor_tensor(out=rt[2], in0=t2, in1=cosb, op=ALU.mult), r=["qn", "cosT"], w=["rt2"])
                    P.dve(lambda e: e.tensor_tensor(out=rt[3], in0=t1, in1=sinb, op=ALU.mult), r=["qn", "sinT"], w=["rt3"])
                    P.dve(lambda e: e.tensor_tensor(out=t1, in0=rt[0], in1=rt[1], op=ALU.subtract), r=["rt0", "rt1", "rt2", "rt3", "qn"], w=["qn"])
                    P.dve(lambda e: e.tensor_tensor(out=t2, in0=rt[2], in1=rt[3], op=ALU.add), r=["rt2", "rt3", "qn"], w=["qn"])
                    if n == 0:
                        tap("qn%d" % l, qn, [128, 640], ["qn"])
                    if cut <= 4:
                        return
                    qs = n % 2
                    ks = n % 4
                    for grp in range(2):
                        for h4 in range(4):
                            h = grp * 4 + h4
                            P.pe(TR(ps[4][0:64, h4 * 128:(h4 + 1) * 128], qn3[:, h, :], ident[:]), r=["qn", "ident"], w=["ps4"])
                        P.act((lambda grp: lambda e: e.activation(
                            out=qT[qs][0:64, grp * 4:(grp + 1) * 4, :], in_=ps[4][0:64, :].rearrange("p (h t) -> p h t", t=128),
                            func=AF.Copy, scale=0.125))(grp), r=["ps4"], w=["qT%d" % qs])
                    for g in range(2):
                        P.pe(TR(ps[4][0:64, g * 128:(g + 1) * 128], qn3[:, 8 + g, :], ident[:]), r=["qn", "ident"], w=["ps4"])
                    P.dve(lambda e: e.tensor_copy(out=kT[ks][0:64, :, :], in_=ps[4][0:64, 0:256].rearrange("p (g t) -> p g t", t=128)),
                          r=["ps4"], w=["kT%d" % ks])
                    P.act(lambda e: e.copy(out=VP[ks][:, 0, 0:64], in_=zsb[:, 640:704]), r=zall, w=["vR%d" % ks])
                    P.act(lambda e: e.copy(out=VP[ks][:, 1, 64:128], in_=zsb[:, 704:768]), r=zall + ["vR%d" % ks], w=["vR%d" % ks])
                    if cut <= 5:
                        return
                    ws = n % 2
                    for half in range(2):
                        for c4 in range(4):
                            c = half * 4 + c4
                            P.pe(TR(ps[4][:, c4 * 128:(c4 + 1) * 128], zsb[:, 768 + c * 128:768 + (c + 1) * 128], ident[:]),
                                 r=zall + ["ident"], w=["ps4"])
                        pv4 = ps[4][:, :].rearrange("p (c t) -> p c t", t=128)
                        cs = slice(half * 4, half * 4 + 4)
                        P.act((lambda cs, pv4: lambda e: e.copy(out=Wb[ws][:, cs, 8:136], in_=pv4))(cs, pv4), r=["ps4"], w=["Wb%dm" % ws])
                    if cut <= 6:
                        return
                    if n == 0:
                        P.dve(lambda e: e.memset(Wb[ws][:, :, 0:8], 0.0), w=["Wb%dl" % ws])
                    else:
                        P.dve(lambda e: e.tensor_copy(out=Wb[1 - ws][:, :, 136:144], in_=Wb[ws][:, :, 8:16]), r=["Wb%dm" % ws], w=["Wb%dr" % (1 - ws)])
                        P.dve(lambda e: e.tensor_copy(out=Wb[ws][:, :, 0:8], in_=Wb[1 - ws][:, :, 128:136]), r=["Wb%dm" % (1 - ws)], w=["Wb%dl" % ws])
                    if n == NT - 1:
                        P.dve(lambda e: e.memset(Wb[ws][:, :, 136:144], 0.0), w=["Wb%dr" % ws])

                def stepC(m):
                    ws = m % 2
                    W = Wb[ws]
                    wr = ["Wb%dm" % ws, "Wb%dl" % ws, "Wb%dr" % ws]
                    cp = cpT[ws]
                    edge = 0 if m == 0 else (2 if m == NT - 1 else 1)
                    for j in range(2):
                        P.dve((lambda j: lambda e: e.tensor_tensor(out=cx, in0=W[:, 4 + j, :], in1=W[:, j, :], op=ALU.mult))(j), r=wr, w=["cx"])
                        P.dve((lambda j: lambda e: e.tensor_scalar(out=cacc, in0=cx[:, 8:136], scalar1=convw[:, j * 3 + 1:j * 3 + 2], scalar2=None, op0=ALU.mult))(j),
                              r=["cx", "convw"], w=["cacc"])
                        P.dve((lambda j: lambda e: e.scalar_tensor_tensor(out=cacc, in0=cx[:, 7:135], scalar=convw[:, j * 3:j * 3 + 1], in1=cacc, op0=ALU.mult, op1=ALU.add))(j),
                              r=["cx", "convw", "cacc"], w=["cacc"])
                        P.dve((lambda j: lambda e: e.scalar_tensor_tensor(out=cacc, in0=cx[:, 9:137], scalar=convw[:, j * 3 + 2:j * 3 + 3], in1=cacc, op0=ALU.mult, op1=ALU.add))(j),
                              r=["cx", "convw", "cacc"], w=["cacc"])
                        P.dve((lambda j: lambda e: e.tensor_tensor(out=cp[:, j, :], in0=cacc, in1=W[:, 2 + j, 8:136], op=ALU.mult))(j),
                              r=wr + ["cacc"], w=["cpT%d" % ws])
                    for j in range(2):
                        u = W[:, 6 + j, :]
                        A1, A4, A8, A16 = Alv
                        P.dve((lambda u: lambda e: e.tensor_tensor(out=A1[:, 1:144], in0=u[:, 0:143], in1=u[:, 1:144], op=ALU.add))(u), r=wr, w=["A1"])
                        P.dve(lambda e: e.tensor_tensor(out=A4[:, 2:143], in0=A1[:, 1:142], in1=A1[:, 3:144], op=ALU.add), r=["A1"], w=["A4"])
                        if j == 0:
                            lo, hi = A1, A4
                        else:
                            P.dve(lambda e: e.tensor_tensor(out=A8[:, 4:141], in0=A4[:, 2:139], in1=A4[:, 6:143], op=ALU.add), r=["A4"], w=["A8"])
                            P.dve(lambda e: e.tensor_tensor(out=A16[:, 8:137], in0=A8[:, 4:133], in1=A8[:, 12:141], op=ALU.add), r=["A8"], w=["A16"])
                            lo, hi = A8, A16
                        for (p0, p1, A) in ((0, 64, lo), (64, 128, hi)):
                            P.dve((lambda p0, p1, A, j: lambda e: e.tensor_tensor(out=ptmp[p0:p1, :], in0=A[p0:p1, 8:136], in1=invcnt[p0:p1, j * 3 + edge, :], op=ALU.mult))(p0, p1, A, j),
                                  r=["A1", "A4", "A8", "A16", "invcnt"], w=["ptmp"])
                        P.dve((lambda u: lambda e: e.tensor_tensor(out=pooled, in0=ptmp, in1=u[:, 8:136], op=ALU.subtract))(u), r=["ptmp"] + wr, w=["pooled"])
                        P.pe(MM(ps[5][:, 0:128], poolw[:, j, :], pooled, True, True), r=["pooled", "poolw%d" % j], w=["ps5"])
                        P.act((lambda j: lambda e: e.activation(out=cp[:, 2 + j, :], in_=ps[5][:, 0:128], func=AF.Copy, scale=pscale[:, j:j + 1]))(j),
                              r=["ps5", "pscale"], w=["cpT%d" % ws])
                    if m == 0:
                        tap("cp%d" % l, cp, [128, 4, 128], ["cpT%d" % ws])

                def stepD(m):
                    qs = m % 2
                    blocks = [j for j in (m - 1, m, m + 1) if 0 <= j < NT]
                    pslot = [0]
                    nmm = 2 * len(blocks)
                    imm = 0
                    for g in range(2):
                        for bi, j in enumerate(blocks):
                            ks = j % 4
                            P.pe(MM(ps[5][:, :], kT[ks][:, g, :], qT[qs][:, g * 4:(g + 1) * 4, :], True, True),
                                 r=["kT%d" % ks, "qT%d" % qs], w=["ps5"])
                            s = pslot[0] % 2
                            pslot[0] += 1
                            P.act((lambda s: lambda e: e.activation(out=PT[s], in_=ps[5][:, :], func=AF.Exp))(s), r=["ps5"], w=["PT%d" % s])
                            if j != m:
                                mk = mlo if j < m else mhi
                                P.dve((lambda s, mk: lambda e: e.tensor_tensor(
                                    out=PT[s].rearrange("p (h t) -> p h t", t=128), in0=PT[s].rearrange("p (h t) -> p h t", t=128),
                                    in1=mk[:].unsqueeze(1).broadcast_to([128, 4, 128]), op=ALU.mult))(s, mk),
                                    r=["PT%d" % s, "mlo", "mhi"], w=["PT%d" % s])
                            P.pe(MM(ps[6][:, :], VP[ks][:, g, :], PT[s], imm == 0, imm == nmm - 1), r=["vR%d" % ks, "VP%dz" % ks, "PT%d" % s], w=["ps6"])
                            P.pe(MM(ps[7][:, :], onesg[:, g, :], PT[s], imm == 0, imm == nmm - 1), r=["onesg", "PT%d" % s], w=["ps7"])
                            imm += 1
                    P.dve(lambda e: e.tensor_tensor(
                        out=den.rearrange("p (h t) -> p h t", t=128), in0=ps[7][:, :].rearrange("p (h t) -> p h t", t=128),
                        in1=esink2[:].unsqueeze(2).broadcast_to([128, 4, 128]), op=ALU.add),
                        r=["ps7", "esink2"], w=["den"])
                    P.dve(lambda e: e.reciprocal(out=den, in_=den), r=["den"], w=["den"])
                    P.dve(lambda e: e.tensor_tensor(
                        out=attnT[:, 0:4, :], in0=ps[6][:, :].rearrange("p (h t) -> p h t", t=128),
                        in1=den.rearrange("p (h t) -> p h t", t=128), op=ALU.mult),
                        r=["ps6", "den"], w=["attnT"])
                    cp = cpT[m % 2]
                    npieces = 8
                    for pc in range(npieces):
                        s = wo_n[0] % 2
                        wo_n[0] += 1
                        if pc < 4:
                            P.dma((lambda s, pc: lambda e: e.dma_start(out=wo[s][0:64, :], in_=wout_d[l, pc * 64:(pc + 1) * 64, :]))(s, pc),
                                  w=["wo%dlo" % s], chan="wo%d" % s)
                            P.dma((lambda s, pc: lambda e: e.dma_start(out=wo[s][64:128, :], in_=wout_d[l, (4 + pc) * 64:(5 + pc) * 64, :]))(s, pc),
                                  w=["wo%dhi" % s], chan="wo%d" % s)
                            lhs = attnT[:, pc, :]
                            rw = wo[s]
                            rr = ["attnT"]
                        else:
                            c = pc - 4
                            P.dma((lambda s, c: lambda e: e.dma_start(out=wo[s], in_=wout_d[l, 512 + c * 128:512 + (c + 1) * 128, :]))(s, c),
                                  w=["wo%dlo" % s, "wo%dhi" % s], chan="wo%d" % s)
                            lhs = cp[:, c, :]
                            rw = wo[s]
                            rr = ["cpT%d" % (m % 2)]
                        for b in range(2):
                            P.pe(MM(ps[b][:, :], lhs, rw[:, b * 512:(b + 1) * 512], pc == 0, pc == npieces - 1), r=rr + ["wo%dlo" % s, "wo%dhi" % s], w=["ps%d" % b])
                    for b in range(2):
                        P.dve((lambda b: lambda e: e.tensor_tensor(out=otmp[:, b * 512:(b + 1) * 512], in0=ps[b][:, :], in1=GATE1[:, b * 512:(b + 1) * 512], op=ALU.mult))(b),
                              r=["ps%d" % b, "GATE0"], w=["junk"])
                    P.dve(lambda e: e.tensor_tensor(out=X[:, m, :], in0=X[:, m, :], in1=otmp, op=ALU.add), r=["junk", "X%d" % m], w=["X%d" % m])

                for n in range(NT):
                    if stage in ('A', 'AC', 'all'):
                        stepA(n)
                    if n >= 1:
                        if stage in ('AC', 'all'):
                            stepC(n - 1)
                        if stage == 'all':
                            stepD(n - 1)
                if stage in ('AC', 'all'):
                    stepC(NT - 1)
                if stage == 'all':
                    stepD(NT - 1)
                tap("xmid%d" % l, X[:, 0, :], [128, 1024], ["X0"])

            def peer():
                P.barrier()
                cv = Carver()
                NWQ = 2
                wqr = [cv.get(1024) for _ in range(NWQ)]
                xn = cv.get(1024)
                h2T = cv.get(1024).rearrange("p (c t) -> p c t", t=128)
                h2 = [cv.get(1024) for _ in range(2)]
                pqT = cv.get(2048).rearrange("p (c t) -> p c t", t=128)
                sc = cv.get(256)
                scr = cv.get(256)
                vv = cv.get(32)
                ii = cv.get(32).bitcast(U32)
                iif = cv.get(32)
                cand = cv.get(256)
                cand2 = cv.get(256)
                cidx = cv.get(256)
                top = cv.get(16)
                pos = cv.get(16).bitcast(U32)
                posff = cv.get(16)
                junk2 = cv.get(256)
                ex = cv.get(16)
                gate = [cv.get(128) for _ in range(2)]
                eidxf = cv.get(128)
                eidxi = [cv.get(128).bitcast(I32) for _ in range(2)]
                actv = cv.get(128)
                ag = cv.get(128)
                wgt = cv.get(128)
                NU = NU_RING
                ub = [cv.get(1024) for _ in range(NU)]
                dg = [cv.get(128) for _ in range(3)]
                dg_n = [0]
                yacc = cv.get(1024)
                djunk = cv.get(1024)
                ss = st8[:, 0:1]
                rstd = st8[:, 1:2]
                negm = st8[:, 2:3]
                gs = st8[:, 3:4]
                wq_n = [0]
                ub_n = [0]

                def route(n):
                    p = n % 2
                    Xn = X[:, n, :]
                    xr = "X%d" % n
                    P.dve(lambda e: e.memset(ss, 0.0), w=["ss"])
                    P.act(lambda e: e.activation(out=xn, in_=Xn, func=AF.Square, accum_out=ss), r=[xr, "ss"], w=["xn", "ss"])
                    rstd_from_ss(ss, rstd, 1024, "ss", "rstd")
                    P.dve(lambda e: e.tensor_scalar(out=xn, in0=Xn, scalar1=rstd, scalar2=None, op0=ALU.mult), r=[xr, "rstd"], w=["xn"])
                    yield
                    for half in range(2):
                        for c4 in range(4):
                            kc = half * 4 + c4
                            P.pe(TR(ps[4][:, c4 * 128:(c4 + 1) * 128], xn[:, kc * 128:(kc + 1) * 128], ident[:]), r=["xn", "ident"], w=["ps4"])
                        for c4 in range(4):
                            kc = half * 4 + c4
                            P.dve((lambda kc, c4: lambda e: e.tensor_scalar(
                                out=h2T[:, kc, :], in0=ps[4][:, c4 * 128:(c4 + 1) * 128], scalar1=G2s[:, kc:kc + 1], scalar2=SH2[:, kc:kc + 1],
                                op0=ALU.mult, op1=ALU.add))(kc, c4), r=["ps4", "der", "modT"], w=["h2T"])
                        yield
                    for half in range(2):
                        for c4 in range(4):
                            kc = half * 4 + c4
                            P.pe(TR(ps[4][:, c4 * 128:(c4 + 1) * 128], h2T[:, kc, :], ident[:]), r=["h2T", "ident"], w=["ps4"])
                        P.act((lambda half: lambda e: e.copy(out=h2[p][:, half * 512:(half + 1) * 512], in_=ps[4][:, :]))(half), r=["ps4"], w=["h2_%d" % p])
                        yield
                    if n == 0:
                        tap("h2_%d" % l, h2[p], [128, 1024], ["h2_%d" % p])
                    for kc in range(8):
                        for hf in range(2):
                            s = wq_n[0] % NWQ
                            wq_n[0] += 1
                            P.dma((lambda s, kc, hf: lambda e: e.dma_start(out=wqr[s], in_=wq_d[l, kc * 128:(kc + 1) * 128, hf * 1024:(hf + 1) * 1024]))(s, kc, hf),
                                  w=["wq%d" % s], chan="wq%d" % s)
                            for j in range(8):
                                c = hf * 8 + j
                                b = c // 4
                                P.pe(MM(ps[b][:, (c % 4) * 128:(c % 4 + 1) * 128], wqr[s][:, j * 128:(j + 1) * 128], h2T[:, kc, :],
                                        kc == 0 and c % 4 == 0, kc == 7), r=["wq%d" % s, "h2T"], w=["ps%d" % b])
                            yield
                    for b in range(4):
                        src = ps[b][:, :].rearrange("p (c t) -> p c t", t=128)
                        if b % 2 == 0:
                            P.act((lambda b, src: lambda e: e.copy(out=pqT[:, b * 4:(b + 1) * 4, :], in_=src))(b, src), r=["ps%d" % b], w=["pqT%d" % b])
                        else:
                            P.dve((lambda b, src: lambda e: e.tensor_copy(out=pqT[:, b * 4:(b + 1) * 4, :], in_=src))(b, src), r=["ps%d" % b], w=["pqT%d" % b])
                    pq_all = ["pqT0", "pqT1", "pqT2", "pqT3"]
                    P.dve(lambda e: e.memset(eidxf, 0.0), w=["eidxf"])
                    yield
                    for h in range(8):
                        for sd in range(2):
                            P.pe(MM(ps[5][:, sd * 128:(sd + 1) * 128], pqT[:, 2 * h + sd, :], keysT[:, sd, :], True, True),
                                 r=pq_all + ["keysT%d" % sd], w=["ps5"])
                        P.act(lambda e: e.copy(out=sc, in_=ps[5][:, 0:256]), r=["ps5"], w=["sc"])
                        if n == 0 and h == 0:
                            tap("sc%d" % l, sc, [128, 256], ["sc"])
                        for sd in range(2):
                            src = sc[:, sd * 128:(sd + 1) * 128]
                            rep = scr[:, sd * 128:(sd + 1) * 128]
                            v = vv[:, sd * 16:(sd + 1) * 16]
                            ix = ii[:, sd * 16:(sd + 1) * 16]
                            P.dve((lambda v, src: lambda e: e.max(out=v[:, 0:8], in_=src))(v, src), r=["sc"], w=["vv"])
                            P.dve((lambda v, src, ix: lambda e: e.max_index(out=ix[:, 0:8], in_max=v[:, 0:8], in_values=src))(v, src, ix), r=["sc", "vv"], w=["ii"])
                            P.dve((lambda v, src, rep: lambda e: e.match_replace(out=rep, in_to_replace=v[:, 0:8], in_values=src, imm_value=-1e30))(v, src, rep),
                                  r=["sc", "vv"], w=["scr"])
                            P.dve((lambda v, rep: lambda e: e.max(out=v[:, 8:16], in_=rep))(v, rep), r=["scr"], w=["vv"])
                            P.dve((lambda v, rep, ix: lambda e: e.max_index(out=ix[:, 8:16], in_max=v[:, 8:16], in_values=rep))(v, rep, ix), r=["scr", "vv"], w=["ii"])
                        yield
                        P.dve(lambda e: e.tensor_copy(out=iif, in_=ii), r=["ii"], w=["iif"])
                        P.dve(lambda e: e.tensor_scalar(out=iif[:, 0:16], in0=iif[:, 0:16], scalar1=128.0, scalar2=None, op0=ALU.mult), r=["iif"], w=["iif"])
                        off = 0
                        for (a0, a1, nb) in ((0, 2, 16), (2, 4, 5), (4, 8, 3), (8, 16, 1)):
                            na = a1 - a0
                            if nb == 1:
                                P.dve((lambda off, a0, a1: lambda e: e.tensor_scalar(out=cand[:, off:off + (a1 - a0)], in0=vv[:, a0:a1], scalar1=vv[:, 16:17], scalar2=None, op0=ALU.add))(off, a0, a1),
                                      r=["vv"], w=["cand"])
                                P.dve((lambda off, a0, a1: lambda e: e.tensor_scalar(out=cidx[:, off:off + (a1 - a0)], in0=iif[:, a0:a1], scalar1=iif[:, 16:17], scalar2=None, op0=ALU.add))(off, a0, a1),
                                      r=["iif"], w=["cidx"])
                                off += na
                                continue
                            segc = cand[:, off:off + na * nb].rearrange("p (a b) -> p a b", b=nb)
                            segx = cidx[:, off:off + na * nb].rearrange("p (a b) -> p a b", b=nb)
                            P.dve((lambda segc, a0, a1, na, nb: lambda e: e.tensor_tensor(
                                out=segc, in0=vv[:, a0:a1].unsqueeze(2).broadcast_to([128, na, nb]),
                                in1=vv[:, 16:16 + nb].unsqueeze(1).broadcast_to([128, na, nb]), op=ALU.add))(segc, a0, a1, na, nb), r=["vv"], w=["cand"])
                            P.dve((lambda segx, a0, a1, na, nb: lambda e: e.tensor_tensor(
                                out=segx, in0=iif[:, a0:a1].unsqueeze(2).broadcast_to([128, na, nb]),
                                in1=iif[:, 16:16 + nb].unsqueeze(1).broadcast_to([128, na, nb]), op=ALU.add))(segx, a0, a1, na, nb), r=["iif"], w=["cidx"])
                            off += na * nb
                        NC = off
                        candv = cand[:, 0:NC]
                        cand2v = cand2[:, 0:NC]
                        P.dve(lambda e: e.max(out=top[:, 0:8], in_=candv), r=["cand"], w=["top"])
                        P.dve(lambda e: e.max_index(out=pos[:, 0:8], in_max=top[:, 0:8], in_values=candv), r=["cand", "top"], w=["pos"])
                        P.dve(lambda e: e.match_replace(out=cand2v, in_to_replace=top[:, 0:8], in_values=candv, imm_value=-1e30), r=["cand", "top"], w=["cand2"])
                        P.dve(lambda e: e.max(out=top[:, 8:16], in_=cand2v), r=["cand2"], w=["top"])
                        P.dve(lambda e: e.max_index(out=pos[:, 8:16], in_max=top[:, 8:16], in_values=cand2v), r=["cand2", "top"], w=["pos"])
                        yield
                        P.dve(lambda e: e.tensor_scalar(out=negm, in0=top[:, 0:1], scalar1=-1.0, scalar2=None, op0=ALU.mult), r=["top"], w=["negm"])
                        P.dve(lambda e: e.memset(gs, 0.0), w=["gs"])
                        P.act(lambda e: e.activation(out=ex, in_=top, func=AF.Exp, bias=negm, accum_out=gs), r=["top", "negm", "gs"], w=["ex", "gs"])
                        P.dve(lambda e: e.reciprocal(out=gs, in_=gs), r=["gs"], w=["gs"])
                        P.dve((lambda h: lambda e: e.tensor_scalar(out=gate[p][:, h * 16:(h + 1) * 16], in0=ex, scalar1=gs, scalar2=None, op0=ALU.mult))(h),
                              r=["ex", "gs"], w=["gate_%d" % p])
                        P.dve(lambda e: e.tensor_copy(out=posff, in_=pos), r=["pos"], w=["posff"])
                        for k in range(16):
                            P.dve((lambda h, k: lambda e: e.scalar_tensor_tensor(
                                out=junk2[:, 0:NC], in0=iota[:, 0:NC], scalar=posff[:, k:k + 1], in1=cidx[:, 0:NC], op0=ALU.is_equal, op1=ALU.mult,
                                accum_out=eidxf[:, h * 16 + k:h * 16 + k + 1]))(h, k), r=["iota", "posff", "cidx", "eidxf"], w=["junk2", "eidxf"])
                            if k % 8 == 7:
                                yield
                    P.dve(lambda e: e.tensor_copy(out=eidxi[p], in_=eidxf), r=["eidxf"], w=["eidxi_%d" % p])
                    if n == 0:
                        tap("eidx%d" % l, eidxi[p], [128, 128], ["eidxi_%d" % p], dt=I32)
                        tap("gate%d" % l, gate[p], [128, 128], ["gate_%d" % p])

                def gather(n, nxt):
                    p = n % 2
                    Xn = X[:, n, :]
                    xr = "X%d" % n
                    if not do_gather:
                        for _ in nxt:
                            pass
                        return
                    P.dve(lambda e: e.memset(actv, 0.0), w=["actv"])
                    for hk in range(128):
                        s = ub_n[0] % NU
                        ub_n[0] += 1
                        P.dma((lambda s, hk: lambda e: e.indirect_dma_start(
                            out=ub[s], out_offset=None, in_=pu_d[l], in_offset=bass.IndirectOffsetOnAxis(ap=eidxi[p][:, hk:hk + 1], axis=0)))(s, hk),
                            r=["eidxi_%d" % p], w=["ub%d" % s], chan="ub%d" % s, eng="pool")
                        P.dve((lambda s, hk: lambda e: e.scalar_tensor_tensor(
                            out=djunk, in0=ub[s], scalar=1.0, in1=h2[p], op0=ALU.mult, op1=ALU.mult, accum_out=actv[:, hk:hk + 1]))(s, hk),
                            r=["ub%d" % s, "h2_%d" % p, "actv"], w=["djunk", "actv"])
                        if hk % 4 == 3:
                            next(nxt, None)
                    P.act(lambda e: e.activation(out=ag, in_=actv, func=AF.Gelu), r=["actv"], w=["ag"])
                    P.dve(lambda e: e.tensor_tensor(out=wgt, in0=ag, in1=gate[p], op=ALU.mult), r=["ag", "gate_%d" % p], w=["wgt"])
                    if PE_EVERY == 34:
                        pe_hks = [hk for hk in range(128) if hk % 4 != 0]
                    elif PE_EVERY == 23:
                        pe_hks = [hk for hk in range(128) if hk % 3 != 0]
                    else:
                        pe_hks = [hk for hk in range(128) if PE_EVERY and hk % PE_EVERY == PE_EVERY - 1]
                    first_dve = True
                    for hk in range(128):
                        s = ub_n[0] % NU
                        ub_n[0] += 1
                        P.dma((lambda s, hk: lambda e: e.indirect_dma_start(
                            out=ub[s], out_offset=None, in_=pv_d[l], in_offset=bass.IndirectOffsetOnAxis(ap=eidxi[p][:, hk:hk + 1], axis=0)))(s, hk),
                            r=["eidxi_%d" % p], w=["ub%d" % s], chan="ub%d" % s, eng="pool")
                        if hk in pe_hks:
                            d = dg_n[0] % 3
                            dg_n[0] += 1
                            P.act((lambda d, hk: lambda e: e.activation(out=dg[d], in_=ident[:], func=AF.Copy, scale=wgt[:, hk:hk + 1]))(d, hk),
                                  r=["ident", "wgt"], w=["dg%d" % d])
                            for b in range(2):
                                P.pe(MM(ps[6 + b][:, :], dg[d], ub[s][:, b * 512:(b + 1) * 512], hk == pe_hks[0], hk == pe_hks[-1]),
                                     r=["dg%d" % d, "ub%d" % s], w=["ps%d" % (6 + b)])
                        elif first_dve:
                            first_dve = False
                            P.dve((lambda s, hk: lambda e: e.tensor_scalar(out=yacc, in0=ub[s], scalar1=wgt[:, hk:hk + 1], scalar2=None, op0=ALU.mult))(s, hk),
                                  r=["ub%d" % s, "wgt"], w=["yacc"])
                        else:
                            P.dve((lambda s, hk: lambda e: e.scalar_tensor_tensor(
                                out=yacc, in0=ub[s], scalar=wgt[:, hk:hk + 1], in1=yacc, op0=ALU.mult, op1=ALU.add))(s, hk),
                                r=["ub%d" % s, "wgt", "yacc"], w=["yacc"])
                        if hk % 4 == 3:
                            next(nxt, None)
                    for _ in nxt:
                        pass
                    if n == 0:
                        tap("wgt%d" % l, wgt, [128, 128], ["wgt"])
                    if PE_EVERY:
                        for b in range(2):
                            P.dve((lambda b: lambda e: e.tensor_tensor(out=yacc[:, b * 512:(b + 1) * 512], in0=ps[6 + b][:, :], in1=yacc[:, b * 512:(b + 1) * 512], op=ALU.add))(b),
                                  r=["ps%d" % (6 + b), "yacc"], w=["yacc"])
                    P.dve(lambda e: e.tensor_tensor(out=yacc, in0=yacc, in1=GATE2[:], op=ALU.mult), r=["yacc", "GATE1"], w=["yacc"])
                    P.dve(lambda e: e.tensor_tensor(out=Xn, in0=Xn, in1=yacc, op=ALU.add), r=["yacc", xr], w=[xr])

                if do_peer:
                    for _ in route(0):
                        pass
                    for n in range(NT):
                        nxt = route(n + 1) if n + 1 < NT else iter(())
                        gather(n, nxt)

            prologue()
            mixing()
            peer()

        for l in range(L):
            layer(l)

        for n in range(NT):
            P.dma((lambda n: lambda e: e.dma_start(out=y_d[n * 128:(n + 1) * 128, :], in_=X[:, n, :]))(n), r=["X%d" % n], chan="out")
        fw = ["out"] + (["tap"] if tap_out else [])
        counts = P.emit(final_wait_chans=fw)
    return nc, counts


def host_consts(NT):
    S = NT * 128
    ar = np.arange(128)
    mlo = (ar[None, :] <= ar[:, None]).astype(np.float32)
    mhi = (ar[:, None] <= ar[None, :]).astype(np.float32)
    invcnt = np.zeros((128, 6, 128), np.float32)
    for j in range(2):
        for e in range(3):
            t = ar + (0 if e == 0 else (S - 128 if e == 2 else 256))
            for p in range(128):
                w = (2, 4, 8, 16)[2 * j + (p // 64)]
                if e == 1:
                    cnt = np.full(128, w)
                else:
                    cnt = np.minimum(t + w // 2, S) - np.maximum(t - w // 2, 0)
                invcnt[p, j * 3 + e, :] = 1.0 / cnt.astype(np.float32)
    onesg = np.zeros((128, 2, 128), np.float32)
    onesg[:, 0, 0:64] = 1.0
    onesg[:, 1, 64:128] = 1.0
    invf = (500000.0 ** (-np.arange(0, 16, 2, dtype=np.float32) / 16)).astype(np.float32)
    return dict(
        ident=np.eye(128, dtype=np.float32), ones=np.ones((128, 128), np.float32), mlo=mlo, mhi=mhi,
        iota=np.broadcast_to(np.arange(256, dtype=np.float32), (128, 256)).copy(),
        onesg=onesg, invcnt=invcnt, invfreq=np.broadcast_to(invf, (128, 8)).copy())


def host_weights(L, norm1_g, norm2_g, w_ada, b_ada, w_in, q_norm_g, k_norm_g, attn_sink, conv_w, pool_w,
                 pool_scale, w_out, peer_wq, peer_keys, peer_u, peer_v):
    f = lambda a: np.ascontiguousarray(np.asarray(a, dtype=np.float32))
    d = {}
    d["n1gT"] = f(np.asarray(norm1_g).reshape(L, 8, 128).transpose(0, 2, 1))
    d["n2gT"] = f(np.asarray(norm2_g).reshape(L, 8, 128).transpose(0, 2, 1))
    d["badaT"] = f(np.asarray(b_ada).reshape(L, 48, 128).transpose(0, 2, 1))
    d["wada"] = f(w_ada)
    d["win"] = f(w_in)
    d["wout"] = f(w_out)
    d["wq"] = f(peer_wq)
    gq = np.concatenate([np.tile(np.asarray(q_norm_g), (1, 8)), np.tile(np.asarray(k_norm_g), (1, 2))], axis=1)
    d["gqk"] = f(np.broadcast_to(gq[:, None, :], (L, 128, 640)))
    d["sink"] = f(np.broadcast_to(np.asarray(attn_sink)[:, None, :], (L, 128, 8)))
    sk = np.asarray(attn_sink, dtype=np.float32)
    s2 = np.zeros((L, 128, 4), np.float32)
    s2[:, 0:64, :] = sk[:, None, 0:4]
    s2[:, 64:128, :] = sk[:, None, 4:8]
    d["sink2"] = s2
    cw = np.asarray(conv_w).reshape(L, 3, 2, 128).transpose(0, 3, 2, 1)
    d["convw"] = f(cw.reshape(L, 128, 6))
    pw = np.zeros((L, 2, 128, 128), np.float32)
    pwa = np.asarray(pool_w)
    for j in range(2):
        pw[:, j, 0:64, 0:64] = pwa[:, 2 * j]
        pw[:, j, 64:128, 64:128] = pwa[:, 2 * j + 1]
    d["poolw"] = pw
    d["pscale"] = f(np.asarray(pool_scale).reshape(L, 2, 128).transpose(0, 2, 1))
    d["keysT"] = f(np.asarray(peer_keys).transpose(0, 1, 3, 2))
    pu = np.asarray(peer_u, dtype=np.float32)
    pv = np.asarray(peer_v, dtype=np.float32)
    for l in range(L):
        d["pu%d" % l] = np.ascontiguousarray(pu[l])
        d["pv%d" % l] = np.ascontiguousarray(pv[l])
    return d


_CACHE = {}


def kernel(x, c, positions, norm1_g, norm2_g, w_ada, b_ada, w_in, q_norm_g, k_norm_g, attn_sink, conv_w,
           pool_w, pool_scale, w_out, peer_wq, peer_keys, peer_u, peer_v):
    x = np.asarray(x, dtype=np.float32)
    c = np.asarray(c, dtype=np.float32)
    positions = np.asarray(positions).astype(np.int32)
    B, S, D = x.shape
    L = np.asarray(w_in).shape[0]
    NT = S // 128
    key = (NT, L)
    if key not in _CACHE:
        _CACHE[key] = build(NT, L)[0]
    nc = _CACHE[key]
    shared = host_weights(L, norm1_g, norm2_g, w_ada, b_ada, w_in, q_norm_g, k_norm_g, attn_sink, conv_w,
                          pool_w, pool_scale, w_out, peer_wq, peer_keys, peer_u, peer_v)
    shared.update(host_consts(NT))
    in_maps = []
    for b in range(B):
        m = dict(shared)
        m["x"] = np.ascontiguousarray(x[b])
        m["cT"] = np.ascontiguousarray(c[b].reshape(8, 128).T)
        m["posT"] = np.ascontiguousarray(positions[b].reshape(NT, 128).T)
        in_maps.append(m)
    res = run_bass_kernel_spmd(nc, in_maps, core_ids=list(range(B)))
    return np.stack([np.asarray(r["y"]) for r in res.results], axis=0).astype(np.float32)
```
